# Optimizing a Trainium2 kernel written in Bass

```python
import math
import jax, jax.numpy as jnp
from jax import lax
import numpy as np

D_MODEL = 1024
BATCH = 32
SEQ = 2048
DEPTH = 1

HEAD_DIM = 64
NSA_HEADS = D_MODEL // 2 // HEAD_DIM
NSA_KV_HEADS = 2
NSA_GROUP = NSA_HEADS // NSA_KV_HEADS
CMP_LEN = 32
CMP_STRIDE = 16
CMP_HIDDEN = 256
SLC_LEN = 64
SLC_TOPK = 6
SLC_Q_CHUNK = 32
WIN = 256
DIFF_QK_DIM = 32
DIFF_V_DIM = 64
DIFF_HEADS = D_MODEL // 2 // DIFF_V_DIM
ROPE_THETA = 500000.0
Q_BLOCK = 128
IN_COLS = (NSA_HEADS * HEAD_DIM + 6 * NSA_KV_HEADS * HEAD_DIM + 3 * NSA_HEADS
           + 4 * DIFF_HEADS * DIFF_QK_DIM + DIFF_HEADS * DIFF_V_DIM)
N_EXPERT_GROUPS = 4
EXPERTS_PER_GROUP = 8
N_EXPERTS = N_EXPERT_GROUPS * EXPERTS_PER_GROUP
EXPERT_TOPK = 2
EXPERT_HIDDEN = D_MODEL // 2
MOE_BLOCK = 512
EPS = 1e-6
NEG = -1e30
FORCE_BONUS = 1e4

kernel_name = "hymba_nsa_diffattn_hmoe_layer"


def rms_norm(x, g):
    x32 = x.astype(jnp.float32)
    y = x32 * lax.rsqrt(jnp.mean(x32 * x32, axis=-1, keepdims=True) + EPS)
    return (y * g.astype(jnp.float32)).astype(x.dtype)


def rope_tables(seq, rot_dim):
    inv = ROPE_THETA ** (-jnp.arange(0, rot_dim, 2, dtype=jnp.float32) / rot_dim)
    ang = jnp.arange(seq, dtype=jnp.float32)[:, None] * inv[None, :]
    return jnp.cos(ang), jnp.sin(ang)


def apply_rope(x, cos, sin):
    half = cos.shape[-1]
    x1, x2, xp = x[..., :half], x[..., half:2 * half], x[..., 2 * half:]
    c, s = cos.astype(x.dtype), sin.astype(x.dtype)
    return jnp.concatenate([x1 * c - x2 * s, x1 * s + x2 * c, xp], axis=-1)


def heads(t, n, d):
    b, s, _ = t.shape
    return t.reshape(b, s, n, d).transpose(0, 2, 1, 3)


def compress_blocks(t, pe, w1, w2):
    B, G, S, Dh = t.shape
    n_cmp = (S - CMP_LEN) // CMP_STRIDE + 1
    idx = jnp.arange(n_cmp)[:, None] * CMP_STRIDE + jnp.arange(CMP_LEN)[None, :]
    blocks = (t[:, :, idx, :] + pe).reshape(B, G, n_cmp, CMP_LEN * Dh)
    return jax.nn.gelu(blocks @ w1) @ w2


def selected_attention(q, k, v, sel, scale):
    B, G, NG, S, Dh = q.shape
    K = sel.shape[-1]
    n_chunk = S // SLC_Q_CHUNK
    kb = k.reshape(B, G, S // SLC_LEN, SLC_LEN, Dh)
    vb = v.reshape(B, G, S // SLC_LEN, SLC_LEN, Dh)
    gather = jax.vmap(jax.vmap(lambda blocks, idx: blocks[idx]))
    q_c = jnp.moveaxis(q.reshape(B, G, NG, n_chunk, SLC_Q_CHUNK, Dh), 3, 0)
    sel_c = jnp.moveaxis(sel.reshape(B, G, n_chunk, SLC_Q_CHUNK, K), 2, 0)
    t_c = jnp.arange(S).reshape(n_chunk, SLC_Q_CHUNK)
    offs = jnp.arange(SLC_LEN)

    def chunk(args):
        qc, ic, tc = args
        kg = gather(kb, ic).reshape(B, G, SLC_Q_CHUNK, K * SLC_LEN, Dh)
        vg = gather(vb, ic).reshape(B, G, SLC_Q_CHUNK, K * SLC_LEN, Dh)
        kpos = (ic[..., None] * SLC_LEN + offs).reshape(B, G, SLC_Q_CHUNK, K * SLC_LEN)
        mask = kpos <= tc[:, None]
        s = jnp.einsum('bgntd,bgtkd->bgntk', qc, kg).astype(jnp.float32) * scale
        p = jax.nn.softmax(jnp.where(mask[:, :, None], s, NEG), axis=-1)
        return jnp.einsum('bgntk,bgtkd->bgntd', p.astype(vg.dtype), vg)

    o = lax.map(chunk, (q_c, sel_c, t_c))
    return jnp.moveaxis(o, 0, 3).reshape(B, G, NG, S, Dh)


def window_attention(q, k, v, scale):
    B, G, NG, S, Dh = q.shape
    nq = S // Q_BLOCK
    nprev = WIN // Q_BLOCK
    pad = nprev * Q_BLOCK

    def band(t):
        tp = jnp.pad(t, ((0, 0), (0, 0), (pad, 0), (0, 0))).reshape(B, G, nq + nprev, Q_BLOCK, Dh)
        return jnp.concatenate([tp[:, :, i:i + nq] for i in range(nprev + 1)], axis=3)

    kb, vb = band(k), band(v)
    qb = q.reshape(B, G, NG, nq, Q_BLOCK, Dh)
    tq = jnp.arange(nq)[:, None] * Q_BLOCK + jnp.arange(Q_BLOCK)[None, :]
    tk = jnp.arange(nq)[:, None] * Q_BLOCK - pad + jnp.arange((nprev + 1) * Q_BLOCK)[None, :]
    tq3, tk3 = tq[:, :, None], tk[:, None, :]
    mask = (tk3 <= tq3) & (tk3 > tq3 - WIN) & (tk3 >= 0)
    s = jnp.einsum('bgnqid,bgqjd->bgnqij', qb, kb).astype(jnp.float32) * scale
    p = jax.nn.softmax(jnp.where(mask, s, NEG), axis=-1)
    o = jnp.einsum('bgnqij,bgqjd->bgnqid', p.astype(vb.dtype), vb)
    return o.reshape(B, G, NG, S, Dh)


def nsa_mixer(q, kv, gate_logits, pe_cmp, w_cmp1, w_cmp2, g_q, g_k, cos, sin):
    B, S, _ = q.shape
    dt = q.dtype
    scale = HEAD_DIM ** -0.5
    qh = rms_norm(heads(q, NSA_HEADS, HEAD_DIM), g_q)
    q_nope = qh.reshape(B, NSA_KV_HEADS, NSA_GROUP, S, HEAD_DIM)
    q_rope = apply_rope(qh, cos, sin).reshape(B, NSA_KV_HEADS, NSA_GROUP, S, HEAD_DIM)
    kc, vc, ks, vs, kw, vw = [heads(t, NSA_KV_HEADS, HEAD_DIM) for t in jnp.split(kv, 6, axis=-1)]
    t_pos = jnp.arange(S)

    k_cmp = rms_norm(compress_blocks(kc, pe_cmp[0], w_cmp1[0], w_cmp2[0]), g_k[0])
    v_cmp = compress_blocks(vc, pe_cmp[1], w_cmp1[1], w_cmp2[1])
    n_cmp = k_cmp.shape[2]
    cmp_end = jnp.arange(n_cmp) * CMP_STRIDE + CMP_LEN - 1
    cmp_mask = cmp_end[None, :] <= t_pos[:, None]
    cmp_any = jnp.any(cmp_mask, axis=-1)[:, None].astype(jnp.float32)
    s_cmp = jnp.einsum('bgnsd,bgcd->bgnsc', q_nope, k_cmp).astype(jnp.float32) * scale
    p_cmp = jax.nn.softmax(jnp.where(cmp_mask, s_cmp, NEG), axis=-1) * cmp_any
    o_cmp = jnp.einsum('bgnsc,bgcd->bgnsd', p_cmp.astype(dt), v_cmp)

    n_slc = S // SLC_LEN
    c_start = jnp.arange(n_cmp) * CMP_STRIDE
    s_start = jnp.arange(n_slc) * SLC_LEN
    overlap = jnp.clip(jnp.minimum(c_start[:, None] + CMP_LEN, s_start[None, :] + SLC_LEN)
                       - jnp.maximum(c_start[:, None], s_start[None, :]), 0, None).astype(jnp.float32) / CMP_LEN
    imp = jnp.einsum('bgnsc,cj->bgsj', p_cmp, overlap)
    blk = jnp.arange(n_slc)[None, :]
    cur = (t_pos // SLC_LEN)[:, None]
    valid = blk <= cur
    forced = (blk == 0) | (blk == cur) | (blk == cur - 1)
    score = jnp.where(valid, imp + jnp.where(forced, FORCE_BONUS, 0.0), NEG)
    _, sel = lax.top_k(score, min(SLC_TOPK, n_slc))
    k_s = apply_rope(rms_norm(ks, g_k[1]), cos, sin)
    o_slc = selected_attention(q_rope, k_s, vs, sel, scale)

    k_w = apply_rope(rms_norm(kw, g_k[2]), cos, sin)
    o_win = window_attention(q_rope, k_w, vw, scale)

    g = jax.nn.sigmoid(gate_logits.astype(jnp.float32)).astype(dt)
    g = g.reshape(B, S, NSA_HEADS, 3).transpose(0, 2, 1, 3).reshape(B, NSA_KV_HEADS, NSA_GROUP, S, 3)
    o = g[..., 0:1] * o_cmp + g[..., 1:2] * o_slc + g[..., 2:3] * o_win
    return o.reshape(B, NSA_HEADS, S, HEAD_DIM).transpose(0, 2, 1, 3).reshape(B, S, NSA_HEADS * HEAD_DIM)


def diff_mixer(q, k, v, lam_q1, lam_k1, lam_q2, lam_k2, g_q, g_k, g_o, lambda_init, cos, sin):
    B, S, _ = q.shape

    def qk_heads(t, g):
        t = t.reshape(B, S, DIFF_HEADS, 2, DIFF_QK_DIM).transpose(0, 2, 3, 1, 4)
        return apply_rope(rms_norm(t, g), cos, sin)

    qh, kh = qk_heads(q, g_q), qk_heads(k, g_k)
    vh = heads(v, DIFF_HEADS, DIFF_V_DIM)
    f32 = jnp.float32
    lam = (jnp.exp(jnp.sum(lam_q1.astype(f32) * lam_k1.astype(f32)))
           - jnp.exp(jnp.sum(lam_q2.astype(f32) * lam_k2.astype(f32))) + lambda_init)
    scale = DIFF_QK_DIM ** -0.5
    outs = []
    for i in range(S // Q_BLOCK):
        lo, hi = i * Q_BLOCK, (i + 1) * Q_BLOCK
        s = jnp.einsum('bhmqd,bhmkd->bhmqk', qh[:, :, :, lo:hi], kh[:, :, :, :hi]).astype(f32) * scale
        mask = jnp.arange(hi)[None, :] <= jnp.arange(lo, hi)[:, None]
        p = jax.nn.softmax(jnp.where(mask, s, NEG), axis=-1)
        a = p[:, :, 0] - lam * p[:, :, 1]
        outs.append(jnp.einsum('bhqk,bhkd->bhqd', a.astype(vh.dtype), vh[:, :, :hi]))
    o = jnp.concatenate(outs, axis=2)
    o = rms_norm(o, g_o) * (1.0 - lambda_init)
    return o.transpose(0, 2, 1, 3).reshape(B, S, DIFF_HEADS * DIFF_V_DIM)


def hier_moe(h, w_rg, b_rg, w_re, b_re, w_g, w_u, w_d):
    T, D = h.shape
    f32 = jnp.float32
    lg = (h @ w_rg).astype(f32) + b_rg.astype(f32)
    pg = jax.nn.softmax(lg, axis=-1)
    _, grp = lax.top_k(lg, 1)
    p_grp = jnp.take_along_axis(pg, grp, axis=-1)
    le = ((h @ w_re).astype(f32) + b_re.astype(f32)).reshape(T, N_EXPERT_GROUPS, EXPERTS_PER_GROUP)
    le_sel = jnp.take_along_axis(le, grp[:, :, None], axis=1)[:, 0]
    top_v, top_i = lax.top_k(le_sel, EXPERT_TOPK)
    weight = p_grp * jax.nn.softmax(top_v, axis=-1)
    expert = grp * EXPERTS_PER_GROUP + top_i

    e_flat = expert.reshape(-1)
    tok = jnp.repeat(jnp.arange(T), EXPERT_TOPK)
    w_flat = weight.reshape(-1)
    order = jnp.argsort(e_flat)
    e_s, tok_s, w_s = e_flat[order], tok[order], w_flat[order]
    counts = jnp.bincount(e_flat, length=N_EXPERTS)
    starts = jnp.cumsum(counts) - counts
    padded = (counts + MOE_BLOCK - 1) // MOE_BLOCK * MOE_BLOCK
    pend = jnp.cumsum(padded)
    pstarts = pend - padded
    dest = pstarts[e_s] + (jnp.arange(T * EXPERT_TOPK) - starts[e_s])
    n_blk = (T * EXPERT_TOPK + MOE_BLOCK - 1) // MOE_BLOCK + N_EXPERTS
    rows = n_blk * MOE_BLOCK
    buf = jnp.zeros((rows, D), h.dtype).at[dest].set(h[tok_s])
    blk_e = jnp.minimum(jnp.searchsorted(pend, jnp.arange(n_blk) * MOE_BLOCK, side='right'), N_EXPERTS - 1)

    def expert_block(args):
        xb, e = args
        return (jax.nn.silu(xb @ w_g[e]) * (xb @ w_u[e])) @ w_d[e]

    yb = lax.map(expert_block, (buf.reshape(n_blk, MOE_BLOCK, D), blk_e)).reshape(rows, D)
    return jnp.zeros((T, D), h.dtype).at[tok_s].add(yb[dest] * w_s[:, None].astype(h.dtype))


def setup_inputs(seed: int = 0) -> dict:
    key = jax.random.key(seed)
    ks = jax.random.split(key, 32)
    f32 = jnp.float32

    def nrm(k, shape, scale):
        return jax.random.normal(k, shape, f32) * scale

    D, L = D_MODEL, DEPTH
    return {
        'x': nrm(ks[0], (BATCH, SEQ, D), 1.0),
        'c': nrm(ks[1], (BATCH, D), 1.0),
        'w_ada': nrm(ks[2], (L, D, 6 * D), 0.3 * D ** -0.5),
        'b_ada': nrm(ks[3], (L, 6 * D), 0.01),
        'g_norm_mix': 1.0 + nrm(ks[4], (L, D), 0.05),
        'g_norm_ffn': 1.0 + nrm(ks[5], (L, D), 0.05),
        'w_in': nrm(ks[6], (L, D, IN_COLS), D ** -0.5),
        'g_nsa_q': 1.0 + nrm(ks[7], (L, HEAD_DIM), 0.05),
        'g_nsa_k': 1.0 + nrm(ks[8], (L, 3, HEAD_DIM), 0.05),
        'pe_cmp': nrm(ks[9], (L, 2, CMP_LEN, HEAD_DIM), 0.1),
        'w_cmp1': nrm(ks[10], (L, 2, CMP_LEN * HEAD_DIM, CMP_HIDDEN), (CMP_LEN * HEAD_DIM) ** -0.5),
        'w_cmp2': nrm(ks[11], (L, 2, CMP_HIDDEN, HEAD_DIM), CMP_HIDDEN ** -0.5),
        'g_diff_q': 1.0 + nrm(ks[12], (L, DIFF_QK_DIM), 0.05),
        'g_diff_k': 1.0 + nrm(ks[13], (L, DIFF_QK_DIM), 0.05),
        'lam_q1': nrm(ks[14], (L, DIFF_QK_DIM), 0.1),
        'lam_k1': nrm(ks[15], (L, DIFF_QK_DIM), 0.1),
        'lam_q2': nrm(ks[16], (L, DIFF_QK_DIM), 0.1),
        'lam_k2': nrm(ks[17], (L, DIFF_QK_DIM), 0.1),
        'g_diff_out': 1.0 + nrm(ks[18], (L, DIFF_V_DIM), 0.05),
        'w_out': nrm(ks[19], (L, D, D), D ** -0.5),
        'w_router_group': nrm(ks[20], (L, D, N_EXPERT_GROUPS), D ** -0.5),
        'b_router_group': nrm(ks[21], (L, N_EXPERT_GROUPS), 0.01),
        'w_router_expert': nrm(ks[22], (L, D, N_EXPERTS), D ** -0.5),
        'b_router_expert': nrm(ks[23], (L, N_EXPERTS), 0.01),
        'w_exp_gate': nrm(ks[24], (L, N_EXPERTS, D, EXPERT_HIDDEN), D ** -0.5),
        'w_exp_up': nrm(ks[25], (L, N_EXPERTS, D, EXPERT_HIDDEN), D ** -0.5),
        'w_exp_down': nrm(ks[26], (L, N_EXPERTS, EXPERT_HIDDEN, D), EXPERT_HIDDEN ** -0.5),
    }


def reference(x, c, w_ada, b_ada, g_norm_mix, g_norm_ffn, w_in, g_nsa_q, g_nsa_k, pe_cmp,
              w_cmp1, w_cmp2, g_diff_q, g_diff_k, lam_q1, lam_k1, lam_q2, lam_k2, g_diff_out,
              w_out, w_router_group, b_router_group, w_router_expert, b_router_expert,
              w_exp_gate, w_exp_up, w_exp_down):
    B, S, D = x.shape
    cos_n, sin_n = rope_tables(S, HEAD_DIM // 4)
    cos_d, sin_d = rope_tables(S, DIFF_QK_DIM // 4)
    sizes = (NSA_HEADS * HEAD_DIM, 6 * NSA_KV_HEADS * HEAD_DIM, 3 * NSA_HEADS,
             2 * DIFF_HEADS * DIFF_QK_DIM, 2 * DIFF_HEADS * DIFF_QK_DIM, DIFF_HEADS * DIFF_V_DIM)
    cuts = [int(v) for v in np.cumsum(sizes)[:-1]]
    for l in range(DEPTH):
        lambda_init = 0.8 - 0.6 * math.exp(-0.3 * l)
        mod = (c @ w_ada[l] + b_ada[l])[:, None, :]
        shift1, scale1, gate1, shift2, scale2, gate2 = jnp.split(mod, 6, axis=-1)

        h = rms_norm(x, g_norm_mix[l]) * (1.0 + scale1) + shift1
        proj = h @ w_in[l]
        q_n, kv_n, gate_n, q_d, k_d, v_d = jnp.split(proj, cuts, axis=-1)
        o_nsa = nsa_mixer(q_n, kv_n, gate_n, pe_cmp[l], w_cmp1[l], w_cmp2[l],
                          g_nsa_q[l], g_nsa_k[l], cos_n, sin_n)
        o_diff = diff_mixer(q_d, k_d, v_d, lam_q1[l], lam_k1[l], lam_q2[l], lam_k2[l],
                            g_diff_q[l], g_diff_k[l], g_diff_out[l], lambda_init, cos_d, sin_d)
        x = x + gate1 * (jnp.concatenate([o_nsa, o_diff], axis=-1) @ w_out[l])

        h = rms_norm(x, g_norm_ffn[l]) * (1.0 + scale2) + shift2
        y = hier_moe(h.reshape(B * S, D), w_router_group[l], b_router_group[l],
                     w_router_expert[l], b_router_expert[l],
                     w_exp_gate[l], w_exp_up[l], w_exp_down[l])
        x = x + gate2 * y.reshape(B, S, D)
    return x
```

```python
import math
import numpy as np
import ml_dtypes
import concourse.bass as bass
import concourse.mybir as mybir
from concourse.bass_utils import run_bass_kernel_spmd

F32 = mybir.dt.float32
BF16 = mybir.dt.bfloat16
I32 = mybir.dt.int32
U32 = mybir.dt.uint32
AF = mybir.ActivationFunctionType
ALU = mybir.AluOpType
AX = mybir.AxisListType

S = 2048
D = 1024
NT = S // 128
TCH = 512
NCH = S // TCH
EPS = 1e-6
NEGM = -30000.0
N_CMP = 127
NEXP = 32
CAP = 1152
LAMBDA_INIT = 0.8 - 0.6 * math.exp(-0.3 * 0)

FM_CHUNKS = ["qn0", "qn1", "qn2", "qn3", "ks0", "ks1", "kw0", "kw1",
             "kc0", "kc1", "vc0", "vc1", "qd0", "qd1", "qd2", "qd3",
             "kd0", "kd1", "kd2", "kd3"]
NFM = len(FM_CHUNKS)
TM_A = 280
TM_B = 512
NCOLS = NFM * 128 + TM_A + TM_B


class Buf:
    def __init__(self, name, excl=False):
        self.name = name
        self.excl = excl
        self.w = []
        self.r = []
        self.dsem = None
        self.dval = 0


class Sync:
    ENG = ("pe", "act", "dve", "pool", "sp")

    def __init__(self, nc):
        self.nc = nc
        self.eng = {"pe": nc.tensor, "act": nc.scalar, "dve": nc.vector,
                    "pool": nc.gpsimd, "sp": nc.sync}
        self._ctx = nc.cleanup_on_exit()
        self._ctx.__enter__()
        self.sem = {e: nc.alloc_semaphore("sem_" + e) for e in self.ENG}
        self.dpool = [nc.alloc_semaphore("dsem%d" % i) for i in range(80)]
        nc.all_engine_barrier()
        for s_ in list(self.sem.values()) + self.dpool:
            nc.gpsimd.sem_clear(s_)
        nc.all_engine_barrier()
        self.cnt = {e: 0 for e in self.ENG}
        self.known = {e: {} for e in self.ENG}
        self.dma_bufs = []
        self.self_sync = True

    def _wait(self, e, toks):
        best = {}
        for (sem, val, owner) in toks:
            if owner == e and (e == "pe" or not self.self_sync):
                continue
            k = id(sem)
            if self.known[e].get(k, 0) >= val:
                continue
            if k not in best or best[k][1] < val:
                best[k] = (sem, val)
        for k, (sem, val) in best.items():
            self.eng[e].wait_ge(sem, val)
            self.known[e][k] = val

    def _deps(self, reads, writes, e=None):
        toks = []
        for b in reads:
            toks += b.w
            if b.excl:
                toks += [t for t in b.r if t[2] != e]
        for b in writes:
            toks += b.w + b.r
        return toks

    def op(self, e, fn, reads=(), writes=(), inc=True):
        self._wait(e, self._deps(reads, writes, e))
        ins = fn()
        if inc:
            self.cnt[e] += 1
            ins.then_inc(self.sem[e], 1)
            tok = (self.sem[e], self.cnt[e], e)
            for b in reads:
                b.r.append(tok)
                if len(b.r) > 12:
                    b.r = b.r[-12:] if False else b.r
            for b in writes:
                b.w = [tok]
                b.r = []
        return ins

    def group(self, e, fns, reads=(), writes=()):
        self._wait(e, self._deps(reads, writes, e))
        ins = None
        for fn in fns:
            ins = fn()
        self.cnt[e] += 1
        ins.then_inc(self.sem[e], 1)
        tok = (self.sem[e], self.cnt[e], e)
        for b in reads:
            b.r.append(tok)
        for b in writes:
            b.w = [tok]
            b.r = []

    def dma(self, q, out, in_, sb, reads=(), writes=(), add=False):
        self._wait(q, self._deps(reads, writes))
        if sb.dsem is None:
            sb.dsem = self.dpool.pop()
            self.dma_bufs.append(sb)
        sb.dval += 16
        self.eng[q].dma_start(out=out, in_=in_).then_inc(sb.dsem, 16)
        tok = (sb.dsem, sb.dval, "dma")
        for b in reads:
            b.r.append(tok)
        for b in writes:
            if add:
                b.w.append(tok)
            else:
                b.w = [tok]
                b.r = []
        return tok

    def dma_fn(self, q, fn, sb, reads=(), writes=()):
        self._wait(q, self._deps(reads, writes, q))
        if sb.dsem is None:
            sb.dsem = self.dpool.pop()
            self.dma_bufs.append(sb)
        sb.dval += 16
        fn().then_inc(sb.dsem, 16)
        tok = (sb.dsem, sb.dval, "dma")
        for b in reads:
            b.r.append(tok)
        for b in writes:
            b.w = [tok]
            b.r = []
        return tok

    def finish(self):
        self.barrier()
        self._ctx.__exit__(None, None, None)

    def barrier(self, engines=None):
        engines = engines or self.ENG
        toks = [(self.sem[f], self.cnt[f], f) for f in self.ENG if self.cnt[f] > 0]
        toks += [(b.dsem, b.dval, "dma") for b in self.dma_bufs]
        for e in engines:
            ss = self.self_sync
            self.self_sync = True
            self._wait(e, [t for t in toks if t[2] != e])
            self.self_sync = ss


def _fm(v, nchunk):
    return np.ascontiguousarray(v.reshape(nchunk, 128).T)


def _rope_tables():
    t = np.arange(S, dtype=np.float32)

    def tab(rot_dim, hd):
        inv = (500000.0 ** (-np.arange(0, rot_dim, 2, dtype=np.float32) / rot_dim)).astype(np.float32)
        ang = t[:, None] * inv[None, :]
        cos, sin = np.cos(ang).astype(np.float32), np.sin(ang).astype(np.float32)
        half = rot_dim // 2
        C = np.ones((128, S), np.float32)
        Sg = np.zeros((128, S), np.float32)
        for p in range(128):
            d = p % hd
            if d < half:
                C[p] = cos[:, d]
                Sg[p] = -sin[:, d]
            elif d < 2 * half:
                C[p] = cos[:, d - half]
                Sg[p] = sin[:, d - half]
        return C, Sg

    cn, sn = tab(16, 64)
    cd, sd = tab(8, 32)
    return cn, sn, cd, sd


def _const_mats():
    def blk(hd):
        m = np.zeros((128, 128), np.float32)
        for p in range(128):
            b = p // hd
            m[p, b * hd:(b + 1) * hd] = 1.0 / hd
        return m

    def perm(hd, half):
        m = np.zeros((128, 128), np.float32)
        for p in range(128):
            d = p % hd
            if d < half:
                m[p + half, p] = 1.0
            elif d < 2 * half:
                m[p - half, p] = 1.0
        return m

    ident = np.eye(128, dtype=np.float32)
    k = np.arange(128)[:, None]
    q = np.arange(128)[None, :]
    negc = np.where(k > q, NEGM, 0.0).astype(np.float32)
    negw = np.where(k <= q, NEGM, 0.0).astype(np.float32)
    negcmp = np.zeros((128, NT, 128), np.float32)
    for qt in range(NT):
        t = qt * 128 + np.arange(128)[None, :]
        c = np.arange(128)[:, None]
        negcmp[:, qt, :] = np.where(16 * c + 31 <= t, 0.0, NEGM)
    esel = np.zeros((32, NT, 128), np.float32)
    for kt in range(NT):
        for key in range(128):
            esel[(kt * 128 + key) // 64, kt, key] = 1.0
    ov = np.zeros((128, 32), np.float32)
    for c in range(N_CMP):
        for j in range(32):
            o = min(16 * c + 32, 64 * j + 64) - max(16 * c, 64 * j)
            ov[c, j] = max(o, 0) / 32.0
    forced = np.zeros((128, NT, 32), np.float32)
    invalid = np.zeros((128, NT, 32), np.float32)
    for qt in range(NT):
        for p in range(128):
            t = qt * 128 + p
            cur = t // 64
            for j in range(32):
                if j > cur:
                    invalid[p, qt, j] = 1.0
                elif j == 0 or j == cur or j == cur - 1:
                    forced[p, qt, j] = 1.0
    ltri = (np.arange(128)[:, None] < np.arange(128)[None, :]).astype(np.float32)
    return dict(blk64=blk(64), blk32=blk(32), perm64=perm(64, 8), perm32=perm(32, 4), ident=ident,
                negc=negc, negw=negw, negcmp=negcmp.reshape(128, NT * 128), esel=esel.reshape(32, NT * 128),
                ov=ov, forced=forced.reshape(128, NT * 32), invalid=invalid.reshape(128, NT * 32), ltri=ltri)


def _win_cols(w_in):
    o_q, o_kv, o_g, o_qd, o_kd, o_vd = 0, 512, 1280, 1304, 1816, 2328
    cols = []

    def kv(i, g):
        base = o_kv + i * 128 + g * 64
        return list(range(base, base + 64))

    for j in range(4):
        cols += list(range(o_q + j * 128, o_q + (j + 1) * 128))
    for g in range(2):
        cols += kv(2, g) + kv(2, g)
    for g in range(2):
        cols += kv(4, g) + kv(4, g)
    for g in range(2):
        cols += kv(0, g) + kv(0, g)
    for g in range(2):
        cols += kv(1, g) + kv(1, g)
    for j in range(4):
        cols += list(range(o_qd + j * 128, o_qd + (j + 1) * 128))
    for j in range(4):
        cols += list(range(o_kd + j * 128, o_kd + (j + 1) * 128))
    cols += kv(3, 0) + kv(3, 1) + kv(5, 0) + kv(5, 1)
    cols += list(range(o_g, o_g + 24))
    cols += list(range(o_vd, o_vd + 512))
    assert len(cols) == NCOLS
    return np.ascontiguousarray(w_in[:, cols])


_CACHE = {}


def _host_consts():
    if "c" not in _CACHE:
        cn, sn, cd, sd = _rope_tables()
        cm = _const_mats()
        _CACHE["c"] = dict(cn=cn, sn=sn, cd=cd, sd=sd, **cm)
    return _CACHE["c"]


def build(nseq, stage="full", dbg=False):
    nc = bass.Bass("TRN2", target_bir_lowering=False)
    sy = Sync(nc)
    eng = sy.eng
    PE, ACT, DVE, POOL = eng["pe"], eng["act"], eng["dve"], eng["pool"]

    used_inputs = []

    def din(name, shape, dt=F32):
        used_inputs.append(name)
        return nc.dram_tensor(name, list(shape), dt, kind="ExternalInput").ap()

    x_d = din("x", [nseq, S, D])
    cT_d = din("cT", [128, 8, nseq])
    wada_d = din("w_ada", [D, 6 * D])
    bada_fm_d = din("b_ada_fm", [128, 48])
    bada_row_d = din("b_ada_row", [1, 6 * D])
    gmix_d = din("gmix_fm", [128, 8])
    gffn_d = din("gffn_fm", [128, 8])
    win_d = din("w_inp", [D, NCOLS])
    rowgain_d = din("rowgain", [128, 5])
    gk0_d = din("gk0_row", [1, 128])
    go_d = din("go_row", [1, 64])
    lamv_d = din("lamv", [1, 128])
    pefm_d = din("pe_fm", [128, 32])
    wc1_d = din("w_cmp1", [2, 2048, 256])
    wc2k_d = din("w_cmp2k", [256, 128])
    wc2v_d = din("w_cmp2v", [256, 64])
    wout_d = din("w_out", [D, D])
    cn_d = din("cn", [128, S]); sn_d = din("sn", [128, S])
    cd_d = din("cd", [128, S]); sd_d = din("sd", [128, S])
    mats_d = {k: din("m_" + k, shp) for k, shp in [
        ("blk64", [128, 128]), ("blk32", [128, 128]), ("perm64", [128, 128]), ("perm32", [128, 128]),
        ("ident", [128, 128]), ("negc", [128, 128]), ("negw", [128, 128]), ("negcmp", [128, S]),
        ("esel", [32, S]), ("ov", [128, 32]), ("forced", [128, NT * 32]), ("invalid", [128, NT * 32]),
        ("ltri", [128, 128])]}
    ecap_d = din("ecap", [128, 32])
    wr_d = din("w_r", [D, 36])
    br_d = din("b_r", [1, 36])
    wg_d = din("w_g", [NEXP, D, 512])
    wu_d = din("w_u", [NEXP, D, 512])
    wd_d = din("w_d", [NEXP, 512, D])
    out_d = nc.dram_tensor("out", [nseq, S, D], F32, kind="ExternalOutput").ap()
    modrow_d = nc.dram_tensor("modrow", [nseq, 6 * D], F32, kind="Internal").ap()
    x1_d = nc.dram_tensor("x1s", [nseq, S, D], F32, kind="Internal").ap()
    dbg_d = {}

    def sb(name, shape, dt=F32):
        return nc.alloc_sbuf_tensor(name, list(shape), dt)

    banks = [nc.alloc_psum_tensor("bank%d" % i, [128, 512], F32) for i in range(8)]
    bbuf = [Buf("bank%d" % i, excl=True) for i in range(8)]

    class Rot:
        def __init__(self, idx):
            self.idx = idx; self.i = 0

        def next(self):
            k = self.idx[self.i % len(self.idx)]
            self.i += 1
            return banks[k], bbuf[k]

    rotP = Rot([0, 1])
    rotS = Rot([2, 3, 4])
    rotO = Rot([5, 6, 7])

    cb = {}

    def const_load(name, d_ap, shape, dt, q="pool"):
        t = sb("c_" + name, shape, dt)
        b = Buf("c_" + name)
        sy.dma(q, t[:], d_ap, b, writes=[b])
        cb[name] = (t, b)
        return t, b

    for k in ["blk64", "blk32", "perm64", "perm32", "ident", "negc", "negw"]:
        const_load(k, mats_d[k][:, :], [128, 128], BF16)
    const_load("negcmp", mats_d["negcmp"][:, :], [128, S], BF16)
    esel_t = sb("c_esel", [128, S], BF16); esel_b = Buf("c_esel")
    sy.op("pool", lambda: POOL.memset(esel_t[:], 0.0), writes=[esel_b])
    sy.dma("pool", esel_t[0:32, :], mats_d["esel"][:, :], esel_b, writes=[esel_b])
    cb["esel"] = (esel_t, esel_b)
    const_load("forced", mats_d["forced"][:, :], [128, NT * 32], F32, q="sp")
    const_load("invalid", mats_d["invalid"][:, :], [128, NT * 32], F32, q="sp")
    const_load("identf", mats_d["ident"][:, :], [128, 128], F32, q="sp")
    const_load("rowgain", rowgain_d[:, :], [128, 5], F32, q="sp")
    const_load("gmix", gmix_d[:, :], [128, 8], F32, q="sp")
    const_load("gffn", gffn_d[:, :], [128, 8], F32, q="sp")
    const_load("bada_fm", bada_fm_d[:, :], [128, 48], F32, q="sp")
    const_load("cT", cT_d[:, :, :], [128, 8, nseq], BF16)
    const_load("gk0", gk0_d[0:1, :].partition_broadcast(128), [128, 128], F32)
    const_load("go", go_d[0:1, :].partition_broadcast(128), [128, 64], F32)
    const_load("lamv", lamv_d[0:1, :].partition_broadcast(128), [128, 128], F32)
    const_load("pefm", pefm_d[:, :], [128, 32], BF16)
    const_load("wout", wout_d.rearrange("(k p) n -> p k n", p=128), [128, 8, D], BF16)
    const_load("wc1k", wc1_d[0].rearrange("(j p) n -> p j n", p=128), [128, 16, 256], BF16)
    const_load("wc1v", wc1_d[1].rearrange("(j p) n -> p j n", p=128), [128, 16, 256], BF16)
    const_load("wc2k", wc2k_d.rearrange("(m p) n -> p m n", p=128), [128, 2, 128], BF16)
    const_load("wc2v", wc2v_d.rearrange("(m p) n -> p m n", p=128), [128, 2, 64], BF16)

    if stage == "consts":
        sy.finish()
        return nc, sy, locals()

    def C(name):
        return cb[name][0]

    def CB(name):
        return cb[name][1]

    epsT = sb("epsT", [128, 1]); epsB = Buf("epsT")
    sy.op("dve", lambda: DVE.memset(epsT[:], EPS), writes=[epsB])

    modT = sb("modT", [128, 48, nseq]); modTB = Buf("modT")
    gs1 = sb("gs1", [128, 8, nseq]); gs1B = Buf("gs1")
    gs2 = sb("gs2", [128, 8, nseq]); gs2B = Buf("gs2")
    neglam = sb("neglam", [128, 1]); neglamB = Buf("neglam")
    wst = [sb("wst%d" % i, [128, 8, 512], BF16) for i in range(2)]
    wstB = [Buf("wst%d" % i) for i in range(2)]
    stg = [sb("stg%d" % i, [128, 1024]) for i in range(2)]
    stgB = [Buf("stg%d" % i) for i in range(2)]
    wi = [0]

    def wload(d_ap, ncols):
        i = wi[0] % 2
        wi[0] += 1
        sy.dma("pool", wst[i][:, :, 0:ncols], d_ap, wstB[i], writes=[wstB[i]])
        return wst[i], wstB[i]

    wada_v = wada_d.rearrange("(k p) n -> p k n", p=128)
    for gi in range(12):
        wt, wB = wload(wada_v[:, :, gi * 512:(gi + 1) * 512], 512)
        if True:
            pt, pB = rotP.next()
            fns = []
            for mc in range(4):
                for k in range(8):
                    fns.append(lambda mc=mc, k=k: PE.matmul(
                        pt[:, mc * nseq:(mc + 1) * nseq], lhsT=wt[:, k, mc * 128:(mc + 1) * 128],
                        rhs=C("cT")[:, k, :], start=(k == 0), stop=(k == 7)))
            sy.group("pe", fns, reads=[wB, CB("cT")], writes=[pB])
            for mc in range(4):
                j = gi * 4 + mc
                sy.op("dve", lambda mc=mc, j=j: DVE.tensor_scalar(
                    out=modT[:, j, :], in0=pt[:, mc * nseq:(mc + 1) * nseq],
                    scalar1=C("bada_fm")[:, j:j + 1], scalar2=None, op0=ALU.add),
                    reads=[pB, CB("bada_fm")], writes=[modTB])
    for k in range(8):
        sy.op("dve", lambda k=k: DVE.tensor_scalar(out=gs1[:, k, :], in0=modT[:, 8 + k, :], scalar1=1.0,
                                                    scalar2=C("gmix")[:, k:k + 1], op0=ALU.add, op1=ALU.mult),
              reads=[modTB, CB("gmix")], writes=[gs1B])
        sy.op("dve", lambda k=k: DVE.tensor_scalar(out=gs2[:, k, :], in0=modT[:, 32 + k, :], scalar1=1.0,
                                                    scalar2=C("gffn")[:, k:k + 1], op0=ALU.add, op1=ALU.mult),
              reads=[modTB, CB("gffn")], writes=[gs2B])
    if stage == "ada":
        sy.finish()
        return nc, sy, locals()
    lt = sb("lamtmp", [128, 8]); ltB = Buf("lamtmp")
    lv = C("lamv")
    lprod = sb("lamprod", [128, 64])
    sy.op("dve", lambda: DVE.tensor_tensor(out=lprod[:, 0:32], in0=lv[:, 0:32], in1=lv[:, 32:64], op=ALU.mult),
          reads=[CB("lamv")], writes=[ltB])
    sy.op("dve", lambda: DVE.tensor_tensor(out=lprod[:, 32:64], in0=lv[:, 64:96], in1=lv[:, 96:128], op=ALU.mult),
          reads=[CB("lamv")], writes=[ltB])
    sy.op("dve", lambda: DVE.tensor_reduce(out=lt[:, 0:2], in_=lprod[:].rearrange("p (a b) -> p a b", a=2),
                                            axis=AX.X, op=ALU.add), reads=[ltB], writes=[ltB])
    lt2 = sb("lamtmp2", [128, 2]); lt2B = Buf("lamtmp2")
    sy.op("act", lambda: ACT.activation(out=lt2[:, 0:2], in_=lt[:, 0:2], func=AF.Exp), reads=[ltB], writes=[lt2B])
    sy.op("dve", lambda: DVE.tensor_tensor(out=lt[:, 4:5], in0=lt2[:, 1:2], in1=lt2[:, 0:1], op=ALU.subtract),
          reads=[lt2B], writes=[ltB])
    sy.op("dve", lambda: DVE.tensor_scalar(out=neglam[:, 0:1], in0=lt[:, 4:5], scalar1=-LAMBDA_INIT, scalar2=None,
                                            op0=ALU.add), reads=[ltB], writes=[neglamB])

    peb = sb("peb", [128, 4]); pebB = Buf("peb")
    pt, pB = rotP.next()
    fns = []
    for kv in range(2):
        w1 = C("wc1k") if kv == 0 else C("wc1v")
        for mc in range(2):
            for j in range(16):
                fns.append(lambda kv=kv, mc=mc, j=j, w1=w1: PE.matmul(
                    pt[:, (kv * 2 + mc) * 2:(kv * 2 + mc) * 2 + 1], lhsT=w1[:, j, mc * 128:(mc + 1) * 128],
                    rhs=C("pefm")[:, kv * 16 + j:kv * 16 + j + 1], start=(j == 0), stop=(j == 15)))
    sy.group("pe", fns, reads=[CB("wc1k"), CB("wc1v"), CB("pefm")], writes=[pB])
    sy.op("dve", lambda: DVE.tensor_copy(out=peb[:, 0:4], in_=pt[:, 0:8:2]), reads=[pB], writes=[pebB])

    if stage == "setup":
        sy.finish()
        return nc, sy, locals()
    return _build_rest(nc, sy, locals(), nseq, stage, dbg)


class _Stop(Exception):
    pass


class Arena:
    def __init__(self, nc, name, nbytes):
        self.t = nc.alloc_sbuf_tensor(name, [128, nbytes // 2], BF16)
        self.cap = nbytes // 2
        self.off = 0

    def reset(self):
        self.off = 0

    def get(self, shape, dt):
        n = 1
        for s_ in shape[1:]:
            n *= s_
        esz = 4 if dt in (F32, I32, U32) else 2
        n2 = n * esz // 2
        self.off = (self.off + 15) // 16 * 16
        assert self.off + n2 <= self.cap, ("arena overflow", self.off, n2, self.cap)
        ap = self.t[0:shape[0], self.off:self.off + n2]
        self.off += n2
        if esz == 4:
            ap = ap.bitcast(dt)
        if len(shape) == 3:
            ap = ap.rearrange("p (a b) -> p a b", a=shape[1])
        elif len(shape) == 4:
            ap = ap.rearrange("p (a b c) -> p a b c", a=shape[1], b=shape[2])
        return ap


def _build_rest(nc, sy, L, nseq, stage, dbg):
    try:
        return _build_rest2(nc, sy, L, nseq, stage, dbg)
    except _Stop:
        sy.finish()
        return nc, sy, L


def _build_rest2(nc, sy, L, nseq, stage, dbg):
    g = dict(L)

    def stop_if(name):
        if stage == name:
            raise _Stop()

    used_inputs = g["used_inputs"]
    PE, ACT, DVE, POOL = g["PE"], g["ACT"], g["DVE"], g["POOL"]
    C, CB = g["C"], g["CB"]
    rotP, rotS, rotO, Rot = g["rotP"], g["rotS"], g["rotO"], g["Rot"]
    rotPP = Rot([0, 1, 7, 2, 3, 4, 5, 6])
    x_d, win_d, out_d, x1_d, modrow_d = g["x_d"], g["win_d"], g["out_d"], g["x1_d"], g["modrow_d"]
    gs1, gs1B, gs2, gs2B, modT, modTB = g["gs1"], g["gs1B"], g["gs2"], g["gs2B"], g["modT"], g["modTB"]
    neglam, neglamB, peb, pebB, epsT, epsB = g["neglam"], g["neglamB"], g["peb"], g["pebB"], g["epsT"], g["epsB"]
    wload, stg, stgB = g["wload"], g["stg"], g["stgB"]
    cn_d, sn_d, cd_d, sd_d = g["cn_d"], g["sn_d"], g["cd_d"], g["sd_d"]

    NROWS_ = NEXP * CAP + 128
    xbuf_d = nc.dram_tensor("xbuf", [NROWS_, D], BF16, kind="Internal").ap()
    ybuf_d = nc.dram_tensor("ybuf", [NROWS_, D], BF16, kind="Internal").ap()
    xbufB = Buf("xbuf")
    zf_done = [False]
    if stage not in ("attn",):
        ztile = nc.alloc_sbuf_tensor("ztile", [128, D], BF16); ztileB = Buf("ztile")
        sy.op("dve", lambda: DVE.memset(ztile[:], 0.0), writes=[ztileB])

    def emit_zero_fill():
        if zf_done[0] or stage in ("attn",):
            return
        zf_done[0] = True
        for r0 in range(0, NROWS_, 128):
            sy.dma("sp", xbuf_d[r0:r0 + 128, :], ztile[:], ztileB, reads=[ztileB], writes=[xbufB], add=True)
        sy.dma("sp", ybuf_d[NEXP * CAP:NEXP * CAP + 128, :], ztile[:], ztileB, reads=[ztileB])
    g["xbuf_d"], g["ybuf_d"], g["xbufB"] = xbuf_d, ybuf_d, xbufB
    arena = Arena(nc, "arena", 133248)
    A = arena.get
    hT = A([128, 8, TCH], BF16); hTB = Buf("hT")
    QNOPE = A([128, 8, TCH], BF16); QNOPEB = Buf("qnope")
    QROPE = A([128, 8, TCH], BF16); QROPEB = Buf("qrope")
    QD = A([128, 4, TCH], BF16); QDB = Buf("qd")
    KD = A([128, 4, S], BF16); KDB = Buf("kd")
    KS = A([128, 2, S], BF16); KSB = Buf("ks")
    KW = A([128, 2, S], BF16); KWB = Buf("kw")
    KC2 = A([128, 2, TCH + 32], BF16); KC2B = Buf("kc2")
    VC2 = A([128, 2, TCH + 32], BF16); VC2B = Buf("vc2")
    VS = A([128, NT, 2, 65], BF16); VSB = Buf("vs")
    VW = A([128, NT, 2, 65], BF16); VWB = Buf("vw")
    VD = A([128, NT, 8, 65], BF16); VDB = Buf("vd")
    HIDK = A([128, 2, 2, 128], BF16); HIDKB = Buf("hidk")
    HIDV = A([128, 2, 2, 128], BF16); HIDVB = Buf("hidv")
    KCMPT = A([128, 2, 128], BF16); KCMPTB = Buf("kcmpt")
    VCMP = A([128, 2, 97], BF16); VCMPB = Buf("vcmp")
    ropeT = A([128, 4, TCH], F32); ropeTB = Buf("ropeT")
    GATE = A([128, 4, 24], F32); GATEB = Buf("gate")
    G1 = A([128, D], F32); G1B = Buf("g1")
    xn = A([128, D], BF16); xnB = Buf("xn")
    PT = [A([128, 512], BF16) for _ in range(4)]; PTB = [Buf("pt%d" % i) for i in range(4)]
    sqb = [PT[0], PT[1]]; sqbB = [PTB[0], PTB[1]]
    lnb = A([128, 512], F32); lnbB = Buf("lnb")
    rstd = A([128, 512], F32); rstdB = Buf("rstd")
    qnb = [A([128, 512], BF16) for _ in range(3)]; qnbB = [Buf("qn%d" % i) for i in range(3)]
    t1b = A([128, 512], F32); t1bB = Buf("t1b")
    t2b = A([128, 512], F32); t2bB = Buf("t2b")
    small = A([128, 256], F32); smallB = Buf("small")
    onsa, onsaB = lnb, lnbB
    odif, odifB = t2b, t2bB
    tmpo, tmpoB = t1b, t1bB
    attn = A([128, D], BF16); attnB = Buf("attn")
    attn2 = [(attn, attnB), (A([128, D], BF16), Buf("attn_b"))]
    attnT = A([128, 8, 128], BF16); attnTB = Buf("attnT")
    score = A([128, 2, 32], F32); scoreB = Buf("score")
    negsel = A([128, 2, 32], BF16); negselB = Buf("negsel")
    NEGSELT = A([128, 2, 128], BF16); NEGSELTB = Buf("negselT")
    kcn = A([128, 128], BF16); kcnB = Buf("kcn")

    sy.op("pool", lambda: POOL.memset(HIDK, 0.0), writes=[HIDKB])
    sy.op("pool", lambda: POOL.memset(HIDV, 0.0), writes=[HIDVB])
    sy.op("pool", lambda: POOL.memset(KC2, 0.0), writes=[KC2B])
    sy.op("pool", lambda: POOL.memset(VC2, 0.0), writes=[VC2B])
    sy.op("pool", lambda: POOL.memset(VS, 1.0), writes=[VSB])
    sy.op("pool", lambda: POOL.memset(VW, 1.0), writes=[VWB])
    sy.op("pool", lambda: POOL.memset(VD, 1.0), writes=[VDB])
    sy.op("pool", lambda: POOL.memset(VCMP, 1.0), writes=[VCMPB])
    sy.op("pool", lambda: POOL.memset(NEGSELT, 0.0), writes=[NEGSELTB])
    NEGSELT_b = A([128, 2, 128], BF16); NEGSELT_bB = Buf("negselT_b")
    sy.op("pool", lambda: POOL.memset(NEGSELT_b, 0.0), writes=[NEGSELT_bB])
    onsa2 = [(onsa, onsaB), (rstd, rstdB)]
    score2 = [(score, scoreB), (A([128, 2, 32], F32), Buf("score_b"))]
    negsel2 = [(negsel, negselB), (A([128, 2, 32], BF16), Buf("negsel_b"))]
    NEGSELT2 = [(NEGSELT, NEGSELTB), (NEGSELT_b, NEGSELT_bB)]
    sy.op("pool", lambda: POOL.memset(QNOPE, 0.0), writes=[QNOPEB])
    sy.op("pool", lambda: POOL.memset(QROPE, 0.0), writes=[QROPEB])
    goS = nc.alloc_sbuf_tensor("goS", [128, 64], F32); goSB = Buf("goS")
    sy.op("dve", lambda: DVE.tensor_scalar(out=goS[:], in0=C("go")[:], scalar1=1.0 - LAMBDA_INIT, scalar2=None, op0=ALU.mult),
          reads=[CB("go")], writes=[goSB])
    causal01 = nc.alloc_sbuf_tensor("causal01", [128, 128], BF16); causal01B = Buf("causal01")
    sy.op("dve", lambda: DVE.tensor_scalar(out=causal01[:], in0=C("negc")[:], scalar1=-0.5, scalar2=None, op0=ALU.is_gt),
          reads=[CB("negc")], writes=[causal01B])
    ovt = sb_tmp = nc.alloc_sbuf_tensor("ovt", [128, 32], BF16)
    ovB = Buf("ovt")
    sy.dma("pool", ovt[:], g["mats_d"]["ov"][:, :], ovB, writes=[ovB])
    for gq in range(2):
        sy.op("dve", lambda gq=gq: DVE.tensor_copy(out=VCMP[:, gq, 65:97], in_=ovt[:]), reads=[ovB], writes=[VCMPB])

    stop_if("init")
    ident = C("ident"); identB = CB("ident")
    rowgain = C("rowgain")
    cnt = {"sq": 0, "qn": 0, "pt": 0}

    def normrope(pt, pB, blkname, permname, gcol, ctab, stab, nope_dsts, nope_bufs, rope_dsts, rope_bufs, ncols=TCH):
        i = cnt["sq"] % 2; cnt["sq"] += 1
        sq, sqB = sqb[i], sqbB[i]
        sy.op("act", lambda: ACT.activation(out=sq[:, 0:ncols], in_=pt[:, 0:ncols], func=AF.Square),
              reads=[pB], writes=[sqB])
        p2, p2B = rotPP.next()
        sy.op("pe", lambda: PE.matmul(p2[:, 0:ncols], lhsT=C(blkname)[:], rhs=sq[:, 0:ncols], start=True, stop=True),
              reads=[sqB, CB(blkname)], writes=[p2B])
        sy.op("act", lambda: ACT.activation(out=lnb[:, 0:ncols], in_=p2[:, 0:ncols], func=AF.Ln, bias=epsT[:, 0:1]),
              reads=[p2B, epsB], writes=[lnbB])
        sy.op("act", lambda: ACT.activation(out=rstd[:, 0:ncols], in_=lnb[:, 0:ncols], func=AF.Exp, scale=-0.5),
              reads=[lnbB], writes=[rstdB])
        j = cnt["qn"] % 3; cnt["qn"] += 1
        qn, qnB = qnb[j][:, 0:ncols], qnbB[j]
        sy.op("dve", lambda: DVE.scalar_tensor_tensor(out=qn, in0=pt[:, 0:ncols], scalar=rowgain[:, gcol:gcol + 1],
                                                      in1=rstd[:, 0:ncols], op0=ALU.mult, op1=ALU.mult),
              reads=[pB, rstdB, CB("rowgain")], writes=[qnB])
        for (dst, lo, hi) in (nope_dsts or []):
            sy.op("act", lambda: ACT.activation(out=dst, in_=qn[lo:hi, :], func=AF.Copy), reads=[qnB], writes=nope_bufs)
        yield
        p3, p3B = rotPP.next()
        sy.op("pe", lambda: PE.matmul(p3[:, 0:ncols], lhsT=C(permname)[:], rhs=qn, start=True, stop=True),
              reads=[qnB, CB(permname)], writes=[p3B])
        sy.op("dve", lambda: DVE.tensor_tensor(out=t1b[:, 0:ncols], in0=qn, in1=ctab, op=ALU.mult),
              reads=[qnB, ropeTB], writes=[t1bB])
        sy.op("dve", lambda: DVE.tensor_tensor(out=t2b[:, 0:ncols], in0=p3[:, 0:ncols], in1=stab, op=ALU.mult),
              reads=[p3B, ropeTB], writes=[t2bB])
        for (dst, lo, hi) in rope_dsts:
            sy.op("dve", lambda: DVE.tensor_tensor(out=dst, in0=t1b[lo:hi, 0:ncols], in1=t2b[lo:hi, 0:ncols], op=ALU.add),
                  reads=[t1bB, t2bB], writes=rope_bufs)

    win_v = win_d.rearrange("(k p) n -> p k n", p=128)
    xi = [0]

    negc, negw, negcmp, esel = C("negc"), C("negw"), C("negcmp"), C("esel")
    ptc = [0]
    m8 = nc.alloc_sbuf_tensor("m8", [128, 16], F32); m8B = Buf("m8")

    def run_jobs(jobs):
        st = [None] * len(jobs)

        def issue_qk(n):
            bank, bB = rotS.next()
            st[n] = (bank, bB)
            jb = jobs[n]
            sy.group("pe", [(lambda f=f, bank=bank: f(bank)) for f in jb["qk"]], reads=jb["qk_reads"], writes=[bB])

        DEPTH = 2
        for n in range(min(DEPTH, len(jobs))):
            issue_qk(n)
        for n, jb in enumerate(jobs):
            if n + DEPTH < len(jobs):
                issue_qk(n + DEPTH)
            bank, bB = st[n]
            pi = ptc[0] % 4; ptc[0] += 1
            P, PB = PT[pi], PTB[pi]
            ncol = jb["ncol"]
            sy.op("act", lambda: ACT.activation(out=P[:, 0:ncol], in_=bank[:, 0:ncol], func=AF.Exp, scale=jb["scale"]),
                  reads=[bB], writes=[PB])
            if jb.get("mask01") is not None:
                c0 = jb["mask01"]
                sy.op("pool", lambda: POOL.tensor_tensor(out=P[:, c0:c0 + 128], in0=P[:, c0:c0 + 128], in1=causal01[:], op=ALU.mult),
                      reads=[PB, causal01B], writes=[PB])
            sy.group("pe", [(lambda f=f, P=P: f(P)) for f in jb["pv"]], reads=[PB] + jb["pv_reads"], writes=[jb["O"]])
            if jb.get("post") is not None:
                jb["post"]()

    def make_jobs(items, Oap, OB, scale, vreads, kreads, post=None, postmask=False):
        jobs = []
        nb = (len(items) + 3) // 4
        for bi in range(nb):
            blk = items[bi * 4:(bi + 1) * 4]
            qk, pv = [], []
            mask01 = None
            for n, (lk, rq, masks, va, tp) in enumerate(blk):
                gidx = bi * 4 + n
                if postmask and masks:
                    mask01 = n * 128
                    masks = []
                def fqk(bank, lk=lk, rq=rq, masks=masks, n=n, tp=tp):
                    out = bank[:, n * 128:(n + 1) * 128]
                    kw = {} if tp is None else {"tile_position": tp}
                    r = PE.matmul(out, lhsT=lk, rhs=rq, start=True, stop=(len(masks) == 0), **kw)
                    for mi, (ml, mr) in enumerate(masks):
                        r = PE.matmul(out, lhsT=ml, rhs=mr, start=False, stop=(mi == len(masks) - 1))
                    return r
                qk.append(fqk)
                def fpv(P, va=va, n=n, gidx=gidx):
                    return PE.matmul(Oap, lhsT=P[:, n * 128:(n + 1) * 128], rhs=va, start=(gidx == 0), stop=(gidx == len(items) - 1))
                pv.append(fpv)
            jobs.append(dict(qk=qk, qk_reads=kreads, ncol=len(blk) * 128, scale=scale, mask01=mask01, pv=pv,
                             pv_reads=vreads, O=OB, post=(post if bi == nb - 1 else None)))
        return jobs

    def id_masks(neg_ap):
        return [(ident[:], neg_ap)]

    def compress_A(c):
        i0 = 1 if c == 0 else 0
        nblk = 32 - i0
        blk0 = 32 * c - 1 + i0
        for kv in range(2):
            src_t, srcB = (KC2, KC2B) if kv == 0 else (VC2, VC2B)
            w1, w1B = (C("wc1k"), CB("wc1k")) if kv == 0 else (C("wc1v"), CB("wc1v"))
            HID, HIDB = (HIDK, HIDKB) if kv == 0 else (HIDV, HIDVB)
            for gq in range(2):
                pt, pB = rotP.next()
                fns = []
                for m in range(2):
                    for j in range(16):
                        st0 = 16 + 2 * j + 16 * i0
                        fns.append(lambda m=m, j=j, st0=st0: PE.matmul(
                            pt[:, m * 32 + i0:m * 32 + 32], lhsT=w1[:, j, m * 128:(m + 1) * 128],
                            rhs=src_t[:, gq, st0:st0 + 16 * (nblk - 1) + 1:16], start=(j == 0), stop=(j == 15)))
                sy.group("pe", fns, reads=[srcB, w1B], writes=[pB])
                hx = small[:, 64:128]
                for m in range(2):
                    sy.op("dve", lambda m=m: DVE.tensor_scalar(out=hx[:, m * 32:(m + 1) * 32], in0=pt[:, m * 32:(m + 1) * 32],
                                                               scalar1=peb[:, kv * 2 + m:kv * 2 + m + 1], scalar2=None, op0=ALU.add),
                          reads=[pB, pebB], writes=[smallB])
                h2 = small[:, 128:192]
                sy.op("dve", lambda: DVE.tensor_tensor(out=h2, in0=hx, in1=hx, op=ALU.mult), reads=[smallB], writes=[smallB])
                sy.op("dve", lambda: DVE.tensor_scalar(out=h2, in0=h2, scalar1=0.044715, scalar2=1.0, op0=ALU.mult, op1=ALU.add),
                      reads=[smallB], writes=[smallB])
                sy.op("dve", lambda: DVE.tensor_tensor(out=h2, in0=h2, in1=hx, op=ALU.mult), reads=[smallB], writes=[smallB])
                h3 = small[:, 192:256]
                sy.op("act", lambda: ACT.activation(out=h3, in_=h2, func=AF.Exp, scale=-1.5957691216057308),
                      reads=[smallB], writes=[smallB])
                sy.op("dve", lambda: DVE.tensor_scalar(out=h3, in0=h3, scalar1=1.0, scalar2=None, op0=ALU.add),
                      reads=[smallB], writes=[smallB])
                sy.op("dve", lambda: DVE.reciprocal(out=h2, in_=h3), reads=[smallB], writes=[smallB])
                for m in range(2):
                    sy.op("dve", lambda m=m: DVE.tensor_tensor(out=HID[:, gq, m, blk0:blk0 + nblk], in0=hx[:, m * 32 + i0:m * 32 + 32],
                                                               in1=h2[:, m * 32 + i0:m * 32 + 32], op=ALU.mult),
                          reads=[smallB], writes=[HIDB])
        for t_, tB in ((KC2, KC2B), (VC2, VC2B)):
            sy.op("dve", lambda: DVE.tensor_copy(out=t_[:, :, 0:32], in_=t_[:, :, TCH:TCH + 32]), reads=[tB], writes=[tB])

    def compress_B(c):
        for gq in range(2):
            pt, pB = rotP.next()
            sy.group("pe", [(lambda m=m: PE.matmul(pt[:, 0:128], lhsT=HIDK[:, gq, m, :], rhs=C("wc2k")[:, m, :],
                                                   start=(m == 0), stop=(m == 1))) for m in range(2)],
                     reads=[HIDKB, CB("wc2k")], writes=[pB])
            sy.op("act", lambda: ACT.activation(out=t1b[:, 0:64], in_=pt[:, 0:64], func=AF.Square, accum_out=small[:, 5:6]),
                  reads=[pB], writes=[t1bB, smallB])
            sy.op("act", lambda: ACT.activation(out=small[:, 6:7], in_=small[:, 5:6], func=AF.Ln, bias=epsT[:, 0:1], scale=1.0 / 64),
                  reads=[smallB, epsB], writes=[smallB])
            sy.op("act", lambda: ACT.activation(out=small[:, 7:8], in_=small[:, 6:7], func=AF.Exp, scale=-0.5),
                  reads=[smallB], writes=[smallB])
            sy.op("dve", lambda: DVE.scalar_tensor_tensor(out=kcn, in0=pt[:, 0:128], scalar=small[:, 7:8], in1=C("gk0")[:],
                                                          op0=ALU.mult, op1=ALU.mult), reads=[pB, smallB, CB("gk0")], writes=[kcnB])
            p2, p2B = rotP.next()
            p2b = p2[:].bitcast(BF16)
            sy.op("pe", lambda: PE.transpose(p2b[:, 0:128], kcn, ident[:]), reads=[kcnB, identB], writes=[p2B])
            sy.op("dve", lambda: DVE.tensor_copy(out=KCMPT[:, gq, :], in_=p2b[:, 0:128]), reads=[p2B], writes=[KCMPTB])
            p3, p3B = rotP.next()
            sy.group("pe", [(lambda m=m: PE.matmul(p3[:, 0:64], lhsT=HIDV[:, gq, m, :], rhs=C("wc2v")[:, m, :],
                                                   start=(m == 0), stop=(m == 1))) for m in range(2)],
                     reads=[HIDVB, CB("wc2v")], writes=[p3B])
            sy.op("act", lambda: ACT.activation(out=VCMP[:, gq, 0:64], in_=p3[:, 0:64], func=AF.Copy), reads=[p3B], writes=[VCMPB])

    def x1_dst(b, qt):
        t_ = out_d if stage == "attn" else x1_d
        return t_[b, qt * 128:(qt + 1) * 128, :]

    bct = nc.alloc_sbuf_tensor("bct", [128, 128], F32); bctB = Buf("bct")

    def bcast_row(dst, dstB, j0, b):
        for k in range(8):
            sy.op("dve", lambda: DVE.tensor_copy(out=bct[:], in_=modT[:, j0 + k, b:b + 1].to_broadcast([128, 128])),
                  reads=[modTB], writes=[bctB])
            p2, p2B = rotP.next()
            sy.op("pe", lambda: PE.transpose(p2[:, 0:128], bct[:], C("identf")[:]), reads=[bctB, CB("identf")], writes=[p2B])
            sy.op("act", lambda: ACT.activation(out=dst[:, k * 128:(k + 1) * 128], in_=p2[:, 0:128], func=AF.Copy),
                  reads=[p2B], writes=[dstB])

    def bcast_rows_from(srcT, srcB, dst, dstB, b):
        for k in range(8):
            sy.op("dve", lambda: DVE.tensor_copy(out=bct[:], in_=srcT[:, k, b:b + 1].to_broadcast([128, 128])),
                  reads=[srcB], writes=[bctB])
            p2, p2B = rotP.next()
            sy.op("pe", lambda: PE.transpose(p2[:, 0:128], bct[:], C("identf")[:]), reads=[bctB, CB("identf")], writes=[p2B])
            sy.op("act", lambda: ACT.activation(out=dst[:, k * 128:(k + 1) * 128], in_=p2[:, 0:128], func=AF.Copy),
                  reads=[p2B], writes=[dstB])

    SC_N = 0.125
    SC_D = 32.0 ** -0.5

    normed = set()

    def emit_norm_tile(b, c, i):
        if (b, c, i) in normed or b >= nseq:
            return
        normed.add((b, c, i))
        xt, xB = stg[xi[0] % 2], stgB[xi[0] % 2]; xi[0] += 1
        sy.dma("sp", xt[:], x_d[b, c * TCH + i * 128:c * TCH + (i + 1) * 128, :], xB, writes=[xB])
        sy.op("act", lambda: ACT.activation(out=t1b[:, 0:512], in_=xt[:, 0:512], func=AF.Square,
                                            accum_out=small[:, 0:1]), reads=[xB], writes=[t1bB, smallB])
        sy.op("act", lambda: ACT.activation(out=t1b[:, 0:512], in_=xt[:, 512:1024], func=AF.Square,
                                            accum_out=small[:, 1:2]), reads=[xB], writes=[t1bB, smallB])
        sy.op("dve", lambda: DVE.tensor_tensor(out=small[:, 2:3], in0=small[:, 0:1], in1=small[:, 1:2], op=ALU.add),
              reads=[smallB], writes=[smallB])
        sy.op("act", lambda: ACT.activation(out=small[:, 3:4], in_=small[:, 2:3], func=AF.Ln, bias=epsT[:, 0:1],
                                            scale=1.0 / D), reads=[smallB, epsB], writes=[smallB])
        sy.op("act", lambda: ACT.activation(out=small[:, 4:5], in_=small[:, 3:4], func=AF.Exp, scale=-0.5),
              reads=[smallB], writes=[smallB])
        sy.op("dve", lambda: DVE.tensor_scalar(out=xn, in0=xt[:], scalar1=small[:, 4:5], scalar2=None, op0=ALU.mult),
              reads=[xB, smallB], writes=[xnB])
        pt, pB = rotP.next()
        ptb = pt[:].bitcast(BF16)
        sy.group("pe", [(lambda k=k: PE.transpose(ptb[:, k * 128:(k + 1) * 128], xn[:, k * 128:(k + 1) * 128], ident[:]))
                        for k in range(8)], reads=[xnB, identB], writes=[pB])
        for k in range(8):
            e_ = "act" if k % 2 == 0 else "dve"
            if e_ == "act":
                sy.op("act", lambda k=k: ACT.activation(out=hT[:, k, i * 128:(i + 1) * 128], in_=ptb[:, k * 128:(k + 1) * 128],
                                                        func=AF.Identity, scale=gs1[:, k, b:b + 1], bias=modT[:, k, b:b + 1]),
                      reads=[pB, gs1B, modTB], writes=[hTB])
            else:
                sy.op("dve", lambda k=k: DVE.tensor_scalar(out=hT[:, k, i * 128:(i + 1) * 128], in0=ptb[:, k * 128:(k + 1) * 128],
                                                           scalar1=gs1[:, k, b:b + 1], scalar2=modT[:, k, b:b + 1],
                                                           op0=ALU.mult, op1=ALU.add),
                      reads=[pB, gs1B, modTB], writes=[hTB])

    def attention_chunk(b, c):
        compress_B(c)
        stop_if("cmpr")

        def cmp_part(i):
            qt = c * 4 + i
            qs = slice(i * 128, (i + 1) * 128)
            onsa, onsaB = onsa2[i % 2]
            score, scoreB = score2[i % 2]
            negsel, negselB = negsel2[i % 2]
            NEGSELT, NEGSELTB = NEGSELT2[i % 2]
            attn, attnB = attn2[i % 2]
            jobs = []
            for h in range(8):
                hp, j, gq = (h % 2) * 64, h // 2, h // 4
                Ot, OB = rotO.next()
                items = [(KCMPT[:, gq, :], QNOPE[:, h, qs], id_masks(negcmp[:, qt * 128:(qt + 1) * 128]),
                          VCMP[:, gq, :], None)]

                def post(h=h, Ot=Ot, OB=OB, gq=gq):
                    sy.op("dve", lambda: DVE.tensor_scalar(out=small[:, 8:9], in0=Ot[:, 64:65], scalar1=1e-30, scalar2=None, op0=ALU.add),
                          reads=[OB], writes=[smallB])
                    sy.op("dve", lambda: DVE.reciprocal(out=small[:, 9:10], in_=small[:, 8:9]), reads=[smallB], writes=[smallB])
                    sy.op("dve", lambda: DVE.tensor_tensor(out=small[:, 10:11], in0=small[:, 9:10], in1=GATE[:, i, h * 3:h * 3 + 1], op=ALU.mult),
                          reads=[smallB, GATEB], writes=[smallB])
                    sy.op("dve", lambda: DVE.tensor_scalar(out=onsa[:, h * 64:(h + 1) * 64], in0=Ot[:, 0:64], scalar1=small[:, 10:11],
                                                           scalar2=None, op0=ALU.mult), reads=[OB, smallB], writes=[onsaB])
                    if h % 4 == 0:
                        sy.op("dve", lambda: DVE.tensor_scalar(out=score[:, gq, :], in0=Ot[:, 65:97], scalar1=small[:, 9:10],
                                                               scalar2=None, op0=ALU.mult), reads=[OB, smallB], writes=[scoreB])
                    else:
                        sy.op("dve", lambda: DVE.scalar_tensor_tensor(out=score[:, gq, :], in0=Ot[:, 65:97], scalar=small[:, 9:10],
                                                                      in1=score[:, gq, :], op0=ALU.mult, op1=ALU.add),
                              reads=[OB, smallB, scoreB], writes=[scoreB])
                jobs += make_jobs(items, Ot[:, 0:97], OB, SC_N, [VCMPB], [KCMPTB, QNOPEB, CB("negcmp"), identB], post=post)
            run_jobs(jobs)
            stop_if("acmp")
            for gq in range(2):
                sy.op("dve", lambda: DVE.scalar_tensor_tensor(out=score[:, gq, :], in0=C("forced")[:, qt * 32:(qt + 1) * 32], scalar=1e4,
                                                              in1=score[:, gq, :], op0=ALU.mult, op1=ALU.add),
                      reads=[scoreB, CB("forced")], writes=[scoreB])
                sy.op("dve", lambda: DVE.scalar_tensor_tensor(out=score[:, gq, :], in0=C("invalid")[:, qt * 32:(qt + 1) * 32], scalar=-1e30,
                                                              in1=score[:, gq, :], op0=ALU.mult, op1=ALU.add),
                      reads=[scoreB, CB("invalid")], writes=[scoreB])
                sy.op("dve", lambda: DVE.max(out=m8[:, gq * 8:(gq + 1) * 8], in_=score[:, gq, :]), reads=[scoreB], writes=[m8B])
                sy.op("dve", lambda: DVE.tensor_scalar(out=negsel[:, gq, :], in0=score[:, gq, :], scalar1=m8[:, gq * 8 + 5:gq * 8 + 6],
                                                       scalar2=NEGM, op0=ALU.is_lt, op1=ALU.mult), reads=[scoreB, m8B], writes=[negselB])

        def diff_part(i):
            qt = c * 4 + i
            qs = slice(i * 128, (i + 1) * 128)
            onsa, onsaB = onsa2[i % 2]
            score, scoreB = score2[i % 2]
            negsel, negselB = negsel2[i % 2]
            NEGSELT, NEGSELTB = NEGSELT2[i % 2]
            attn, attnB = attn2[i % 2]
            stop_if("aslc")
            jobs = []
            for h in range(8):
                j = h // 2
                Ot, OB = rotO.next()
                for m in range(2):
                    base = (h % 2) * 64 + m * 32
                    tp = (96, 0) if base == 96 else None
                    items = []
                    for kt in range(qt + 1):
                        masks = [1] if kt == qt else []
                        items.append((KD[base:base + 32, j, kt * 128:(kt + 1) * 128], QD[base:base + 32, j, qs], masks, VD[:, kt, h, :], tp))

                    def post(h=h, Ot=Ot, OB=OB):
                        sy.op("dve", lambda: DVE.reciprocal(out=small[:, 13:14], in_=Ot[:, 64:65]), reads=[OB], writes=[smallB])
                        sy.op("dve", lambda: DVE.reciprocal(out=small[:, 14:15], in_=Ot[:, 129:130]), reads=[OB], writes=[smallB])
                        sy.op("dve", lambda: DVE.tensor_tensor(out=small[:, 15:16], in0=small[:, 14:15], in1=neglam[:, 0:1], op=ALU.mult),
                              reads=[smallB, neglamB], writes=[smallB])
                        sy.op("dve", lambda: DVE.tensor_scalar(out=tmpo[:, 0:64], in0=Ot[:, 0:64], scalar1=small[:, 13:14], scalar2=None,
                                                               op0=ALU.mult), reads=[OB, smallB], writes=[tmpoB])
                        sy.op("dve", lambda: DVE.scalar_tensor_tensor(out=odif[:, h * 64:(h + 1) * 64], in0=Ot[:, 65:129], scalar=small[:, 15:16],
                                                                      in1=tmpo[:, 0:64], op0=ALU.mult, op1=ALU.add),
                              reads=[OB, smallB, tmpoB], writes=[odifB])
                    jobs += make_jobs(items, Ot[:, m * 65:(m + 1) * 65], OB, SC_D, [VDB], [KDB, QDB],
                                      post=(post if m == 1 else None), postmask=True)
            run_jobs(jobs)
            stop_if("adif")
            sy.op("act", lambda: ACT.activation(out=tmpo[:, :], in_=odif[:, :], func=AF.Square), reads=[odifB], writes=[tmpoB])
            sy.op("dve", lambda: DVE.tensor_reduce(out=small[:, 16:24], in_=tmpo[:, :].rearrange("p (h d) -> p h d", h=8), axis=AX.X, op=ALU.add),
                  reads=[tmpoB], writes=[smallB])
            sy.op("act", lambda: ACT.activation(out=small[:, 24:32], in_=small[:, 16:24], func=AF.Ln, bias=epsT[:, 0:1], scale=1.0 / 64),
                  reads=[smallB, epsB], writes=[smallB])
            sy.op("act", lambda: ACT.activation(out=small[:, 32:40], in_=small[:, 24:32], func=AF.Exp, scale=-0.5), reads=[smallB], writes=[smallB])
            for h in range(8):
                sy.op("dve", lambda h=h: DVE.scalar_tensor_tensor(out=attn[:, 512 + h * 64:512 + (h + 1) * 64], in0=odif[:, h * 64:(h + 1) * 64],
                                                                  scalar=small[:, 32 + h:33 + h], in1=goS[:], op0=ALU.mult, op1=ALU.mult),
                      reads=[odifB, smallB, goSB], writes=[attnB])

        def tr_part(i):
            qt = c * 4 + i
            qs = slice(i * 128, (i + 1) * 128)
            onsa, onsaB = onsa2[i % 2]
            score, scoreB = score2[i % 2]
            negsel, negselB = negsel2[i % 2]
            NEGSELT, NEGSELTB = NEGSELT2[i % 2]
            attn, attnB = attn2[i % 2]
            for gq in range(2):
                p2, p2B = rotP.next()
                p2b = p2[:].bitcast(BF16)
                sy.op("pe", lambda: PE.transpose(p2b[0:32, 0:128], negsel[:, gq, :], ident[:]), reads=[negselB, identB], writes=[p2B])
                sy.op("dve", lambda: DVE.tensor_copy(out=NEGSELT[0:32, gq, :], in_=p2b[0:32, 0:128]), reads=[p2B], writes=[NEGSELTB])

        def slcwin_part(i):
            qt = c * 4 + i
            qs = slice(i * 128, (i + 1) * 128)
            onsa, onsaB = onsa2[i % 2]
            score, scoreB = score2[i % 2]
            negsel, negselB = negsel2[i % 2]
            NEGSELT, NEGSELTB = NEGSELT2[i % 2]
            attn, attnB = attn2[i % 2]
            stop_if("asel")
            jobs = []
            for h in range(8):
                hp, j, gq = (h % 2) * 64, h // 2, h // 4
                Ot, OB = rotO.next()
                items = []
                for kt in range(qt + 1):
                    masks = [(esel[:, kt * 128:(kt + 1) * 128], NEGSELT[:, gq, :])]
                    if kt == qt:
                        masks += id_masks(negc[:, :])
                    items.append((KS[:, gq, kt * 128:(kt + 1) * 128], QROPE[:, h, qs], masks, VS[:, kt, gq, :], None))
                jobs += make_jobs(items, Ot[:, 0:65], OB, SC_N, [VSB], [KSB, QROPEB, NEGSELTB, CB("esel"), CB("negc"), identB])
                items = []
                for kt in range(max(0, qt - 2), qt + 1):
                    masks = []
                    if kt == qt:
                        masks = id_masks(negc[:, :])
                    elif kt == qt - 2:
                        masks = id_masks(negw[:, :])
                    items.append((KW[:, gq, kt * 128:(kt + 1) * 128], QROPE[:, h, qs], masks, VW[:, kt, gq, :], None))

                def post(h=h, Ot=Ot, OB=OB):
                    for br, c0 in ((1, 0), (2, 65)):
                        sy.op("dve", lambda: DVE.reciprocal(out=small[:, 11:12], in_=Ot[:, c0 + 64:c0 + 65]), reads=[OB], writes=[smallB])
                        sy.op("dve", lambda: DVE.tensor_tensor(out=small[:, 12:13], in0=small[:, 11:12], in1=GATE[:, i, h * 3 + br:h * 3 + br + 1],
                                                               op=ALU.mult), reads=[smallB, GATEB], writes=[smallB])
                        sy.op("dve", lambda: DVE.scalar_tensor_tensor(out=onsa[:, h * 64:(h + 1) * 64], in0=Ot[:, c0:c0 + 64], scalar=small[:, 12:13],
                                                                      in1=onsa[:, h * 64:(h + 1) * 64], op0=ALU.mult, op1=ALU.add),
                              reads=[OB, smallB, onsaB], writes=[onsaB])
                jobs += make_jobs(items, Ot[:, 65:130], OB, SC_N, [VWB], [KWB, QROPEB, CB("negc"), CB("negw"), identB], post=post)
            run_jobs(jobs)
            sy.op("act", lambda: ACT.activation(out=attn[:, 0:512], in_=onsa[:, :], func=AF.Copy), reads=[onsaB], writes=[attnB])

        def final_part(i):
            qt = c * 4 + i
            qs = slice(i * 128, (i + 1) * 128)
            onsa, onsaB = onsa2[i % 2]
            score, scoreB = score2[i % 2]
            negsel, negselB = negsel2[i % 2]
            NEGSELT, NEGSELTB = NEGSELT2[i % 2]
            attn, attnB = attn2[i % 2]
            p2, p2B = rotP.next()
            p2b = p2[:].bitcast(BF16)
            sy.group("pe", [(lambda k=k: PE.transpose(p2b[:, k * 128:(k + 1) * 128], attn[:, k * 128:(k + 1) * 128], ident[:])) for k in range(8)],
                     reads=[attnB, identB], writes=[p2B])
            sy.op("act", lambda: ACT.activation(out=attnT[:, :, :], in_=p2b[:, :].rearrange("p (k t) -> p k t", k=8), func=AF.Copy),
                  reads=[p2B], writes=[attnTB])
            xt, xB = stg[xi[0] % 2], stgB[xi[0] % 2]; xi[0] += 1
            sy.dma("sp", xt[:], x_d[b, qt * 128:(qt + 1) * 128, :], xB, writes=[xB])
            for half in range(2):
                p3, p3B = rotP.next()
                sy.group("pe", [(lambda k=k: PE.matmul(p3[:, :], lhsT=attnT[:, k, :], rhs=C("wout")[:, k, half * 512:(half + 1) * 512],
                                                       start=(k == 0), stop=(k == 7))) for k in range(8)],
                         reads=[attnTB, CB("wout")], writes=[p3B])
                sy.op("dve", lambda: DVE.tensor_tensor(out=tmpo[:, :], in0=p3[:, :], in1=G1[:, half * 512:(half + 1) * 512], op=ALU.mult),
                      reads=[p3B, G1B], writes=[tmpoB])
                sy.op("dve", lambda: DVE.tensor_tensor(out=xt[:, half * 512:(half + 1) * 512], in0=xt[:, half * 512:(half + 1) * 512],
                                                       in1=tmpo[:, :], op=ALU.add), reads=[tmpoB, xB], writes=[xB])
            sy.dma("sp", x1_dst(b, qt), xt[:], xB, reads=[xB])
            stop_if("aout")
            nb_, nc_ = (b, c + 1) if c + 1 < NCH else (b + 1, 0)
            emit_norm_tile(nb_, nc_, i)

        cmp_part(0)
        tr_part(0)
        for i in range(4):
            diff_part(i)
            if i > 0:
                final_part(i - 1)
            if i + 1 < 4:
                cmp_part(i + 1)
            slcwin_part(i)
            if i + 1 < 4:
                tr_part(i + 1)
        final_part(3)

    for b in range(nseq):
        bcast_row(G1, G1B, 16, b)
        for c in range(NCH):
            t0 = c * TCH
            for i_, tab in enumerate([cn_d, sn_d, cd_d, sd_d]):
                sy.dma("sp", ropeT[:, i_, :], tab[:, t0:t0 + TCH], ropeTB, writes=[ropeTB], add=(i_ > 0))
            for i in range(4):
                emit_norm_tile(b, c, i)
            stop_if("norm")
            pend = []
            for grp in range(5):
                if grp == 3:
                    compress_A(c)
                wt, wB = wload(win_v[:, :, grp * 512:(grp + 1) * 512], 512)
                for ci in range(4):
                    name = FM_CHUNKS[grp * 4 + ci]
                    pt, pB = rotPP.next()
                    sy.group("pe", [(lambda k=k: PE.matmul(pt[:, :], lhsT=wt[:, k, ci * 128:(ci + 1) * 128], rhs=hT[:, k, :],
                                                           start=(k == 0), stop=(k == 7))) for k in range(8)],
                             reads=[wB, hTB], writes=[pB])
                    kind, j = name[:2], int(name[2])
                    gen_ = None
                    if kind == "qn":
                        gen_ = normrope(pt, pB, "blk64", "perm64", 0, ropeT[:, 0, :], ropeT[:, 1, :],
                                 [(QNOPE[0:64, 2 * j, :], 0, 64), (QNOPE[64:128, 2 * j + 1, :], 64, 128)], [QNOPEB],
                                 [(QROPE[0:64, 2 * j, :], 0, 64), (QROPE[64:128, 2 * j + 1, :], 64, 128)], [QROPEB])
                    elif kind == "ks":
                        gen_ = normrope(pt, pB, "blk64", "perm64", 1, ropeT[:, 0, :], ropeT[:, 1, :],
                                 None, None, [(KS[:, j, t0:t0 + TCH], 0, 128)], [KSB])
                    elif kind == "kw":
                        gen_ = normrope(pt, pB, "blk64", "perm64", 2, ropeT[:, 0, :], ropeT[:, 1, :],
                                 None, None, [(KW[:, j, t0:t0 + TCH], 0, 128)], [KWB])
                    elif kind == "qd":
                        gen_ = normrope(pt, pB, "blk32", "perm32", 3, ropeT[:, 2, :], ropeT[:, 3, :],
                                 None, None, [(QD[:, j, :], 0, 128)], [QDB])
                    elif kind == "kd":
                        gen_ = normrope(pt, pB, "blk32", "perm32", 4, ropeT[:, 2, :], ropeT[:, 3, :],
                                 None, None, [(KD[:, j, t0:t0 + TCH], 0, 128)], [KDB])
                    else:
                        dst, dB = (KC2, KC2B) if kind == "kc" else (VC2, VC2B)
                        sy.op("act", lambda: ACT.activation(out=dst[0:64, j, 32:32 + TCH], in_=pt[0:64, :], func=AF.Copy),
                              reads=[pB], writes=[dB])
                        sy.op("dve", lambda: DVE.tensor_copy(out=dst[64:128, j, 31:31 + TCH], in_=pt[64:128, :]),
                              reads=[pB], writes=[dB])
                    for g_ in list(pend):
                        try:
                            next(g_)
                        except StopIteration:
                            pend.remove(g_)
                    if gen_ is not None:
                        pend.append(gen_)
            while pend:
                for g_ in list(pend):
                    try:
                        next(g_)
                    except StopIteration:
                        pend.remove(g_)
            stop_if("projfm")
            wtA, wBA = wload(win_v[:, :, NFM * 128:NFM * 128 + TM_A], TM_A)
            for i in range(4):
                kt = c * 4 + i
                pt, pB = rotPP.next()
                sy.group("pe", [(lambda k=k: PE.matmul(pt[:, 0:TM_A], lhsT=hT[:, k, i * 128:(i + 1) * 128], rhs=wtA[:, k, 0:TM_A],
                                                       start=(k == 0), stop=(k == 7))) for k in range(8)],
                         reads=[wBA, hTB], writes=[pB])
                stop_if("tmb%d_0" % i)
                sy.op("dve", lambda: DVE.tensor_copy(out=VS[:, kt, :, 0:64], in_=pt[:, 0:128].rearrange("p (g d) -> p g d", g=2)),
                      reads=[pB], writes=[VSB])
                stop_if("tmb%d_1" % i)
                sy.op("dve", lambda: DVE.tensor_copy(out=VW[:, kt, :, 0:64], in_=pt[:, 128:256].rearrange("p (g d) -> p g d", g=2)),
                      reads=[pB], writes=[VWB])
                if stage == "dbgA" and i == 1:
                    sy.op("dve", lambda: DVE.tensor_copy(out=stg[0][:, 0:280], in_=pt[:, 0:280]), reads=[pB], writes=[stgB[0]])
                    sy.dma("sp", out_d[0, 0:128, 0:280], stg[0][:, 0:280], stgB[0], reads=[stgB[0]])
                    sy.op("dve", lambda: DVE.tensor_copy(out=stg[1][:, 0:512], in_=hT[:, 0, :]), reads=[hTB], writes=[stgB[1]])
                    sy.dma("sp", out_d[0, 128:256, 0:512], stg[1][:, 0:512], stgB[1], reads=[stgB[1]])
                    raise _Stop()
                stop_if("tmb%d_2" % i)
                sy.op("act", lambda: ACT.activation(out=small[:, 8:32], in_=pt[:, 256:280], func=AF.Exp, scale=-1.0),
                      reads=[pB], writes=[smallB])
                stop_if("tmb%d_3" % i)
                sy.op("dve", lambda: DVE.tensor_scalar(out=small[:, 32:56], in0=small[:, 8:32], scalar1=1.0, scalar2=None, op0=ALU.add),
                      reads=[smallB], writes=[smallB])
                sy.op("dve", lambda: DVE.reciprocal(out=GATE[:, i, :], in_=small[:, 32:56]), reads=[smallB], writes=[GATEB])
                stop_if("tmb%d_4" % i)
            stop_if("tma")
            wtB, wBB = wload(win_v[:, :, NFM * 128 + TM_A:NCOLS], TM_B)
            for i in range(4):
                kt = c * 4 + i
                pt, pB = rotPP.next()
                sy.group("pe", [(lambda k=k: PE.matmul(pt[:, :], lhsT=hT[:, k, i * 128:(i + 1) * 128], rhs=wtB[:, k, :],
                                                       start=(k == 0), stop=(k == 7))) for k in range(8)],
                         reads=[wBB, hTB], writes=[pB])
                sy.op("act", lambda: ACT.activation(out=VD[:, kt, :, 0:64], in_=pt[:, :].rearrange("p (h d) -> p h d", h=8),
                                                    func=AF.Copy), reads=[pB], writes=[VDB])
            stop_if("proj")
            attention_chunk(b, c)
            emit_zero_fill()
    if stage == "attn":
        sy.finish()
        return nc, sy, locals()

    sy.barrier()
    arena.reset()
    NTT = nseq * NT
    NROWS = NEXP * CAP + 128
    TRASH = NEXP * CAP
    xbuf_d, ybuf_d, xbufB = g["xbuf_d"], g["ybuf_d"], g["xbufB"]
    wr_d, br_d, wg_d, wu_d, wd_d = g["wr_d"], g["br_d"], g["wg_d"], g["wu_d"], g["wd_d"]
    mats_d = g["mats_d"]
    ecap_d = g["ecap_d"]

    GS2b = A([128, D], F32); GS2bB = Buf("GS2b")
    SH2b = A([128, D], F32); SH2bB = Buf("SH2b")
    G2b = A([128, D], F32); G2bB = Buf("G2b")
    wr = A([128, 8, 36], F32); wrB = Buf("wr")
    brb = A([128, 36], F32); brbB = Buf("brb")
    ecap = A([128, 32], F32); ecapB = Buf("ecap")
    ltri = A([128, 128], BF16); ltriB = Buf("ltri")
    ones_bf = A([128, 128], BF16); onesB = Buf("ones")
    base_bc = A([128, 32], F32); baseB = Buf("base")
    SLOT = A([128, NTT, 2], I32)
    WGT = A([128, NTT, 2], F32)
    arena_mark = arena.off
    m1set = []
    m1x = [(stg[0], stgB[0]), (stg[1], stgB[1])] + [(A([128, D], F32), Buf("m1x%d" % i_)) for i_ in range(2)]
    for si_ in range(4):
        m1set.append((A([128, 512], F32), Buf("rs%d" % si_), A([128, D], F32), Buf("xn2_%d" % si_), A([128, D], F32), Buf("tmpf%d" % si_),
                      A([128, D], BF16), Buf("h2tm%d" % si_), A([128, 8, 128], F32), Buf("h2T%d" % si_), A([128, 32], BF16), Buf("Abf%d" % si_)))
    sy.dma("sp", wr, wr_d.rearrange("(k p) n -> p k n", p=128), wrB, writes=[wrB])
    sy.dma("pool", brb, br_d[0:1, :].partition_broadcast(128), brbB, writes=[brbB])
    sy.dma("sp", ecap, ecap_d[:, :], ecapB, writes=[ecapB])
    sy.dma("pool", ltri, mats_d["ltri"][:, :], ltriB, writes=[ltriB])
    sy.op("dve", lambda: DVE.memset(ones_bf, 1.0), writes=[onesB])
    sy.op("dve", lambda: DVE.memset(base_bc, 0.0), writes=[baseB])
    SLOTBs = [Buf("slot%d" % i) for i in range(NTT)]
    WGTBs = [Buf("wgt%d" % i) for i in range(NTT)]

    def m1_tile(b, ti, si):
        tt = b * NT + ti
        Sx = m1set[si]
        rs, rsB, xn2, xn2B, tmpf, tmpfB, h2tm, h2tmB, h2T, h2TB, Abf, AbfB = Sx

        def col(i_):
            return rs[:, i_:i_ + 1]
        xt, xB = m1x[si]
        sy.dma("sp", xt[:], x1_d[b, ti * 128:(ti + 1) * 128, :], xB, writes=[xB])
        yield
        sy.op("act", lambda: ACT.activation(out=tmpf[:, 0:512], in_=xt[:, 0:512], func=AF.Square, accum_out=col(0)),
              reads=[xB], writes=[tmpfB, rsB])
        yield
        sy.op("act", lambda: ACT.activation(out=tmpf[:, 512:1024], in_=xt[:, 512:1024], func=AF.Square, accum_out=col(1)),
              reads=[xB], writes=[tmpfB, rsB])
        yield
        sy.op("dve", lambda: DVE.tensor_tensor(out=col(2), in0=col(0), in1=col(1), op=ALU.add), reads=[rsB], writes=[rsB])
        yield
        sy.op("act", lambda: ACT.activation(out=col(3), in_=col(2), func=AF.Ln, bias=epsT[:, 0:1], scale=1.0 / D),
              reads=[rsB, epsB], writes=[rsB])
        yield
        sy.op("act", lambda: ACT.activation(out=col(4), in_=col(3), func=AF.Exp, scale=-0.5), reads=[rsB], writes=[rsB])
        yield
        sy.op("act", lambda: ACT.activation(out=xn2, in_=xt[:], func=AF.Identity, scale=col(4)),
              reads=[xB, rsB], writes=[xn2B])
        yield
        sy.op("pool", lambda: POOL.tensor_tensor(out=tmpf, in0=xn2, in1=GS2b, op=ALU.mult), reads=[xn2B, GS2bB], writes=[tmpfB])
        yield
        sy.op("pool", lambda: POOL.tensor_tensor(out=h2tm, in0=tmpf, in1=SH2b, op=ALU.add), reads=[tmpfB, SH2bB], writes=[h2tmB])
        yield
        for hf in range(2):
            p2, p2B = rotPP.next()
            sy.group("pe", [(lambda k=k: PE.transpose(p2[:, (k % 4) * 128:(k % 4 + 1) * 128], xn2[:, k * 128:(k + 1) * 128],
                                                      C("identf")[:])) for k in range(hf * 4, hf * 4 + 4)],
                     reads=[xn2B, CB("identf")], writes=[p2B])
            yield
            for k in range(hf * 4, hf * 4 + 4):
                sy.op("act", lambda k=k: ACT.activation(out=h2T[:, k, :], in_=p2[:, (k % 4) * 128:(k % 4 + 1) * 128], func=AF.Identity,
                                                        scale=gs2[:, k, b:b + 1], bias=modT[:, 24 + k, b:b + 1]),
                      reads=[p2B, gs2B, modTB], writes=[h2TB])
                yield
        p3, p3B = rotPP.next()
        sy.group("pe", [(lambda k=k: PE.matmul(p3[:, 0:36], lhsT=h2T[:, k, :], rhs=wr[:, k, :], start=(k == 0), stop=(k == 7)))
                        for k in range(8)], reads=[h2TB, wrB], writes=[p3B])
        yield
        LG = rs[:, 8:44]
        sy.op("dve", lambda: DVE.tensor_tensor(out=LG, in0=p3[:, 0:36], in1=brb, op=ALU.add), reads=[p3B, brbB], writes=[rsB])
        yield
        R = lambda *a, **k_: sy.op("dve", *a, reads=[rsB], writes=[rsB], **k_)
        R(lambda: DVE.tensor_reduce(out=col(5), in_=rs[:, 8:12], axis=AX.X, op=ALU.max))
        yield
        R(lambda: DVE.tensor_scalar(out=col(6), in0=col(5), scalar1=-1.0, scalar2=None, op0=ALU.mult))
        yield
        sy.op("act", lambda: ACT.activation(out=rs[:, 48:52], in_=rs[:, 8:12], func=AF.Exp, bias=col(6), accum_out=col(7)),
              reads=[rsB], writes=[rsB])
        yield
        R(lambda: DVE.reciprocal(out=col(52), in_=col(7)))
        yield
        R(lambda: DVE.tensor_scalar(out=rs[:, 56:60], in0=rs[:, 8:12], scalar1=col(5), scalar2=None, op0=ALU.is_equal))
        yield
        R(lambda: DVE.tensor_scalar(out=rs[:, 64:72], in0=rs[:, 12:20], scalar1=col(56), scalar2=None, op0=ALU.mult))
        yield
        for gi in range(1, 4):
            R(lambda gi=gi: DVE.scalar_tensor_tensor(out=rs[:, 64:72], in0=rs[:, 12 + 8 * gi:20 + 8 * gi], scalar=col(56 + gi),
                                                     in1=rs[:, 64:72], op0=ALU.mult, op1=ALU.add))
            yield
        R(lambda: DVE.max(out=rs[:, 72:80], in_=rs[:, 64:72]))
        yield
        R(lambda: DVE.tensor_tensor(out=col(80), in0=col(73), in1=col(72), op=ALU.subtract))
        yield
        sy.op("act", lambda: ACT.activation(out=col(81), in_=col(80), func=AF.Exp), reads=[rsB], writes=[rsB])
        yield
        R(lambda: DVE.tensor_scalar(out=col(82), in0=col(81), scalar1=1.0, scalar2=None, op0=ALU.add))
        yield
        R(lambda: DVE.reciprocal(out=col(83), in_=col(82)))
        yield
        R(lambda: DVE.tensor_tensor(out=col(84), in0=col(83), in1=col(52), op=ALU.mult))
        yield
        R(lambda: DVE.tensor_tensor(out=col(85), in0=col(52), in1=col(84), op=ALU.subtract))
        yield
        R(lambda: DVE.tensor_scalar(out=rs[:, 88:96], in0=rs[:, 64:72], scalar1=col(72), scalar2=None, op0=ALU.is_equal))
        yield
        R(lambda: DVE.tensor_scalar(out=rs[:, 96:104], in0=rs[:, 64:72], scalar1=col(73), scalar2=None, op0=ALU.is_equal))
        yield
        for gi in range(4):
            R(lambda gi=gi: DVE.tensor_scalar(out=rs[:, 128 + 8 * gi:136 + 8 * gi], in0=rs[:, 88:96], scalar1=col(56 + gi),
                                              scalar2=None, op0=ALU.mult))
            yield
            R(lambda gi=gi: DVE.tensor_scalar(out=rs[:, 160 + 8 * gi:168 + 8 * gi], in0=rs[:, 96:104], scalar1=col(56 + gi),
                                              scalar2=None, op0=ALU.mult))
            yield
        sy.op("dve", lambda: DVE.tensor_tensor(out=Abf, in0=rs[:, 128:160], in1=rs[:, 160:192], op=ALU.add), reads=[rsB], writes=[AbfB])
        yield
        p4, p4B = rotPP.next()
        sy.group("pe", [lambda: PE.matmul(p4[:, 0:32], lhsT=ltri, rhs=Abf, start=True, stop=True),
                        lambda: PE.matmul(p4[:, 32:64], lhsT=ones_bf, rhs=Abf, start=True, stop=True)],
                 reads=[AbfB, ltriB, onesB], writes=[p4B])
        yield
        sy.op("dve", lambda: DVE.tensor_tensor(out=rs[:, 192:224], in0=p4[:, 0:32], in1=base_bc, op=ALU.add),
              reads=[p4B, baseB, rsB], writes=[rsB])
        sy.op("dve", lambda: DVE.tensor_tensor(out=base_bc, in0=p4[:, 32:64], in1=base_bc, op=ALU.add), reads=[p4B, baseB], writes=[baseB])
        yield
        for kx, s0 in ((0, 128), (1, 160)):
            R(lambda s0=s0: DVE.tensor_tensor(out=rs[:, 224:256], in0=rs[:, s0:s0 + 32], in1=rs[:, 192:224], op=ALU.mult))
            yield
            R(lambda kx=kx: DVE.tensor_reduce(out=col(256 + kx), in_=rs[:, 224:256], axis=AX.X, op=ALU.add))
            yield
            sy.op("dve", lambda s0=s0: DVE.tensor_tensor(out=rs[:, 224:256], in0=rs[:, s0:s0 + 32], in1=ecap, op=ALU.mult),
                  reads=[rsB, ecapB], writes=[rsB])
            yield
            R(lambda kx=kx: DVE.tensor_reduce(out=col(258 + kx), in_=rs[:, 224:256], axis=AX.X, op=ALU.add))
            yield
            R(lambda kx=kx: DVE.tensor_scalar(out=col(260 + kx), in0=col(256 + kx), scalar1=float(CAP), scalar2=None, op0=ALU.is_lt))
            yield
            R(lambda kx=kx: DVE.tensor_tensor(out=col(262 + kx), in0=col(256 + kx), in1=col(258 + kx), op=ALU.add))
            yield
            R(lambda kx=kx: DVE.tensor_scalar(out=col(262 + kx), in0=col(262 + kx), scalar1=-float(TRASH), scalar2=None, op0=ALU.add))
            yield
            R(lambda kx=kx: DVE.tensor_tensor(out=col(262 + kx), in0=col(262 + kx), in1=col(260 + kx), op=ALU.mult))
            yield
            R(lambda kx=kx: DVE.tensor_scalar(out=col(262 + kx), in0=col(262 + kx), scalar1=float(TRASH), scalar2=None, op0=ALU.add))
            yield
            sy.op("dve", lambda kx=kx: DVE.tensor_copy(out=SLOT[:, tt, kx:kx + 1], in_=col(262 + kx)), reads=[rsB], writes=[SLOTBs[tt]])
            yield
            sy.op("dve", lambda kx=kx: DVE.tensor_tensor(out=WGT[:, tt, kx:kx + 1], in0=col(84 + kx), in1=col(260 + kx), op=ALU.mult),
                  reads=[rsB], writes=[WGTBs[tt]])
            yield
        for kx in range(2):
            sy.dma_fn("pool", lambda kx=kx: POOL.indirect_dma_start(
                out=xbuf_d[:, :], out_offset=bass.IndirectOffsetOnAxis(ap=SLOT[:, tt, kx:kx + 1].bitcast(U32), axis=0),
                in_=h2tm, in_offset=None), h2tmB, reads=[h2tmB, SLOTBs[tt], xbufB])
            yield

    for b in range(nseq):
        bcast_rows_from(gs2, gs2B, GS2b, GS2bB, b)
        bcast_row(SH2b, SH2bB, 24, b)
        for ti in range(0, NT, 4):
            gens = [m1_tile(b, ti + q_, q_) for q_ in range(4)]
            alive = [True] * 4
            while any(alive):
                for gi_ in range(4):
                    if alive[gi_]:
                        try:
                            next(gens[gi_])
                        except StopIteration:
                            alive[gi_] = False
    if stage == "m1":
        sy.finish()
        return nc, sy, locals()

    sy.barrier()
    arena.off = arena_mark
    WEXP = [(A([128, 8, 512], BF16), A([128, 8, 512], BF16), A([128, 4, D], BF16)) for _ in range(2)]
    WEXPB = [(Buf("wg%d" % i), Buf("wu%d" % i), Buf("wd%d" % i)) for i in range(2)]
    XT = A([128, 8, CAP], BF16); XTB = Buf("XT")
    HT = A([128, 4, CAP], BF16); HTB = Buf("HT")
    xrow = [A([128, D], BF16) for _ in range(3)]; xrowB = [Buf("xrow%d" % i) for i in range(3)]
    yst = [A([128, D], BF16) for _ in range(2)]; ystB = [Buf("yst%d" % i) for i in range(2)]
    sgb = [A([128, 384], F32) for _ in range(3)]; sgbB = [Buf("sg%d" % i) for i in range(3)]
    tmpf = A([128, D], F32); tmpfB = Buf("tmpf3")
    Yg = [A([128, D], BF16) for _ in range(4)]; YgB = [Buf("yg%d" % i) for i in range(4)]
    NST = CAP // 128
    NCC = 3
    rotGU = Rot([0, 1, 2, 3, 4, 7])
    CW = CAP // NCC
    xr = [0]

    def load_expert(e):
        i_ = e % 2
        (wg, wu, wd_), (wgB, wuB, wdB) = WEXP[i_], WEXPB[i_]
        sy.dma("pool", wg, wg_d[e].rearrange("(k p) n -> p k n", p=128), wgB, writes=[wgB])
        sy.dma("pool", wu, wu_d[e].rearrange("(k p) n -> p k n", p=128), wuB, writes=[wuB])
        sy.dma("pool", wd_, wd_d[e].rearrange("(k p) n -> p k n", p=128), wdB, writes=[wdB])

    def x_part(e):
        for s_ in range(NST):
            xw, xwB = xrow[xr[0] % 3], xrowB[xr[0] % 3]; xr[0] += 1
            r0 = e * CAP + s_ * 128
            sy.dma("sp", xw, xbuf_d[r0:r0 + 128, :], xwB, writes=[xwB])
            p2, p2B = rotP.next()
            p2b = p2[:].bitcast(BF16)
            sy.group("pe", [(lambda k=k: PE.transpose(p2b[:, k * 128:(k + 1) * 128], xw[:, k * 128:(k + 1) * 128], ident[:]))
                            for k in range(8)], reads=[xwB, identB], writes=[p2B])
            sy.op("act" if s_ % 2 == 0 else "dve",
                  (lambda: ACT.activation(out=XT[:, :, s_ * 128:(s_ + 1) * 128], in_=p2b[:, :].rearrange("p (k t) -> p k t", k=8), func=AF.Copy))
                  if s_ % 2 == 0 else
                  (lambda: DVE.tensor_copy(out=XT[:, :, s_ * 128:(s_ + 1) * 128], in_=p2b[:, :].rearrange("p (k t) -> p k t", k=8))),
                  reads=[p2B], writes=[XTB])

    def gu_part(e):
        (wg, wu, wd_), (wgB, wuB, wdB) = WEXP[e % 2], WEXPB[e % 2]
        for m in range(4):
            for cc in range(NCC):
                cs = slice(cc * CW, (cc + 1) * CW)
                pg, pgB = rotGU.next()
                sy.group("pe", [(lambda k=k: PE.matmul(pg[:, 0:CW], lhsT=wg[:, k, m * 128:(m + 1) * 128], rhs=XT[:, k, cs],
                                                       start=(k == 0), stop=(k == 7))) for k in range(8)], reads=[wgB, XTB], writes=[pgB])
                pu, puB = rotGU.next()
                sy.group("pe", [(lambda k=k: PE.matmul(pu[:, 0:CW], lhsT=wu[:, k, m * 128:(m + 1) * 128], rhs=XT[:, k, cs],
                                                       start=(k == 0), stop=(k == 7))) for k in range(8)], reads=[wuB, XTB], writes=[puB])
                sg, sgB = sgb[(m * NCC + cc) % 3], sgbB[(m * NCC + cc) % 3]
                sy.op("act", lambda: ACT.activation(out=sg[:, 0:CW], in_=pg[:, 0:CW], func=AF.Silu), reads=[pgB], writes=[sgB])
                sy.op("dve", lambda: DVE.tensor_tensor(out=HT[:, m, cs], in0=pu[:, 0:CW], in1=sg[:, 0:CW], op=ALU.mult),
                      reads=[puB, sgB], writes=[HTB])

    def dn_part(e):
        (wg, wu, wd_), (wgB, wuB, wdB) = WEXP[e % 2], WEXPB[e % 2]
        for s_ in range(NST):
            ys, ysB = yst[s_ % 2], ystB[s_ % 2]
            for half in range(2):
                po, poB = rotO.next()
                sy.group("pe", [(lambda m=m: PE.matmul(po[:, :], lhsT=HT[:, m, s_ * 128:(s_ + 1) * 128], rhs=wd_[:, m, half * 512:(half + 1) * 512],
                                                       start=(m == 0), stop=(m == 3))) for m in range(4)], reads=[HTB, wdB], writes=[poB])
                if half == 0:
                    sy.op("act", lambda: ACT.activation(out=ys[:, 0:512], in_=po[:, :], func=AF.Copy), reads=[poB], writes=[ysB])
                else:
                    sy.op("dve", lambda: DVE.tensor_copy(out=ys[:, 512:1024], in_=po[:, :]), reads=[poB], writes=[ysB])
            r0 = e * CAP + s_ * 128
            sy.dma("sp", ybuf_d[r0:r0 + 128, :], ys, ysB, reads=[ysB])

    load_expert(0)
    x_part(0)
    for e in range(NEXP):
        if e + 1 < NEXP:
            load_expert(e + 1)
        gu_part(e)
        if e + 1 < NEXP:
            x_part(e + 1)
        dn_part(e)
    if stage == "m2":
        sy.finish()
        return nc, sy, locals()

    sy.barrier()
    yi = [0]
    for b in range(nseq):
        bcast_row(G2b, G2bB, 40, b)
        for ti in range(NT):
            tt = b * NT + ti
            xt, xB = stg[xi[0] % 2], stgB[xi[0] % 2]; xi[0] += 1
            sy.dma("sp", xt[:], x1_d[b, ti * 128:(ti + 1) * 128, :], xB, writes=[xB])
            ys_ = []
            for kx in range(2):
                y_, yB_ = Yg[yi[0] % 4], YgB[yi[0] % 4]; yi[0] += 1
                sy.dma_fn("pool", lambda kx=kx, y_=y_: POOL.indirect_dma_start(
                    out=y_, out_offset=None, in_=ybuf_d[:, :],
                    in_offset=bass.IndirectOffsetOnAxis(ap=SLOT[:, tt, kx:kx + 1].bitcast(U32), axis=0)), yB_, reads=[SLOTBs[tt]], writes=[yB_])
                ys_.append((y_, yB_))
            sy.op("act", lambda: ACT.activation(out=tmpf, in_=ys_[0][0], func=AF.Identity, scale=WGT[:, tt, 0:1]),
                  reads=[ys_[0][1], WGTBs[tt]], writes=[tmpfB])
            sy.op("dve", lambda: DVE.scalar_tensor_tensor(out=tmpf, in0=ys_[1][0], scalar=WGT[:, tt, 1:2], in1=tmpf, op0=ALU.mult, op1=ALU.add),
                  reads=[ys_[1][1], WGTBs[tt], tmpfB], writes=[tmpfB])
            sy.op("dve", lambda: DVE.tensor_tensor(out=tmpf, in0=tmpf, in1=G2b, op=ALU.mult), reads=[tmpfB, G2bB], writes=[tmpfB])
            sy.op("dve", lambda: DVE.tensor_tensor(out=xt[:], in0=xt[:], in1=tmpf, op=ALU.add), reads=[tmpfB, xB], writes=[xB])
            sy.dma("sp", out_d[b, ti * 128:(ti + 1) * 128, :], xt[:], xB, reads=[xB])
    sy.finish()
    return nc, sy, locals()


def _prep_shared(inp):
    f = lambda a: np.ascontiguousarray(np.asarray(a, dtype=np.float32))
    hc = _host_consts()
    sh = {}
    sh["w_ada"] = f(inp["w_ada"][0])
    sh["b_ada_fm"] = _fm(f(inp["b_ada"][0]), 48)
    sh["b_ada_row"] = f(inp["b_ada"][0]).reshape(1, -1)
    sh["gmix_fm"] = _fm(f(inp["g_norm_mix"][0]), 8)
    sh["gffn_fm"] = _fm(f(inp["g_norm_ffn"][0]), 8)
    sh["w_inp"] = _win_cols(f(inp["w_in"][0]))
    gq = f(inp["g_nsa_q"][0]); gk = f(inp["g_nsa_k"][0])
    gdq = f(inp["g_diff_q"][0]); gdk = f(inp["g_diff_k"][0])
    sh["rowgain"] = np.ascontiguousarray(np.stack([np.tile(gq, 2), np.tile(gk[1], 2), np.tile(gk[2], 2),
                                                   np.tile(gdq, 4), np.tile(gdk, 4)], axis=1))
    sh["gk0_row"] = np.tile(gk[0], 2).reshape(1, 128)
    sh["go_row"] = f(inp["g_diff_out"][0]).reshape(1, 64)
    sh["lamv"] = np.concatenate([f(inp["lam_q1"][0]), f(inp["lam_k1"][0]), f(inp["lam_q2"][0]), f(inp["lam_k2"][0])]).reshape(1, 128)
    pe = f(inp["pe_cmp"][0])
    pefm = np.zeros((128, 32), np.float32)
    for kv in range(2):
        for j in range(16):
            pefm[0:64, kv * 16 + j] = pe[kv, 2 * j]
            pefm[64:128, kv * 16 + j] = pe[kv, 2 * j + 1]
    sh["pe_fm"] = pefm
    sh["w_cmp1"] = f(inp["w_cmp1"][0])
    w2 = f(inp["w_cmp2"][0])
    sh["w_cmp2k"] = np.ascontiguousarray(np.concatenate([w2[0], w2[0]], axis=1))
    sh["w_cmp2v"] = np.ascontiguousarray(w2[1])
    sh["w_out"] = f(inp["w_out"][0])
    for k in ("cn", "sn", "cd", "sd"):
        sh[k] = hc[k]
    for k in ("blk64", "blk32", "perm64", "perm32", "ident", "negc", "negw", "negcmp", "esel", "ov", "forced", "invalid", "ltri"):
        sh["m_" + k] = hc[k]
    sh["ecap"] = np.ascontiguousarray(np.tile((np.arange(32, dtype=np.float32) * CAP)[None, :], (128, 1)))
    sh["w_r"] = np.ascontiguousarray(np.concatenate([f(inp["w_router_group"][0]), f(inp["w_router_expert"][0])], axis=1))
    sh["b_r"] = np.concatenate([f(inp["b_router_group"][0]), f(inp["b_router_expert"][0])]).reshape(1, 36)
    sh["w_g"] = f(inp["w_exp_gate"][0])
    sh["w_u"] = f(inp["w_exp_up"][0])
    sh["w_d"] = f(inp["w_exp_down"][0])
    return sh


def _prep_core(inp, b0, nseq):
    x = np.ascontiguousarray(np.asarray(inp["x"][b0:b0 + nseq], dtype=np.float32))
    c = np.asarray(inp["c"][b0:b0 + nseq], dtype=np.float32)
    cT = np.ascontiguousarray(c.T.reshape(8, 128, nseq).transpose(1, 0, 2))
    return {"x": x, "cT": cT}


def run(inputs, nseq=4, ncores=8, stage="full", trace=False):
    nc, sy, L = build(nseq, stage=stage)
    used = set(L["used_inputs"])
    sh = {k: v for k, v in _prep_shared(inputs).items() if k in used}
    in_maps = []
    for ci in range(ncores):
        m = dict(sh)
        m.update(_prep_core(inputs, ci * nseq, nseq))
        in_maps.append(m)
    res = run_bass_kernel_spmd(nc, in_maps, core_ids=list(range(ncores)), trace=trace)
    outs = [r["out"] for r in res.results]
    return np.concatenate(outs, axis=0), res


def kernel(**inputs):
    out, _ = run(inputs, nseq=4, ncores=8, stage="full")
    return out.astype(np.float32)
```

```python
import math
import numpy as np
import ml_dtypes
import concourse.bass as bass
import concourse.mybir as mybir
from concourse.bass_utils import run_bass_kernel_spmd

F32 = mybir.dt.float32
BF16 = mybir.dt.bfloat16
I32 = mybir.dt.int32
U32 = mybir.dt.uint32
AF = mybir.ActivationFunctionType
ALU = mybir.AluOpType
AX = mybir.AxisListType

S = 2048
D = 1024
NT = S // 128
TCH = 512
NCH = S // TCH
EPS = 1e-6
NEGM = -30000.0
N_CMP = 127
NEXP = 32
CAP = 1152
LAMBDA_INIT = 0.8 - 0.6 * math.exp(-0.3 * 0)

FM_CHUNKS = ["qn0", "qn1", "qn2", "qn3", "ks0", "ks1", "kw0", "kw1",
             "kc0", "kc1", "vc0", "vc1", "qd0", "qd1", "qd2", "qd3",
             "kd0", "kd1", "kd2", "kd3"]
NFM = len(FM_CHUNKS)
TM_A = 280
TM_B = 512
NCOLS = NFM * 128 + TM_A + TM_B


class Buf:
    def __init__(self, name, excl=False):
        self.name = name
        self.excl = excl
        self.w = []
        self.r = []
        self.dsem = None
        self.dval = 0


class Sync:
    ENG = ("pe", "act", "dve", "pool", "sp")

    def __init__(self, nc):
        self.nc = nc
        self.eng = {"pe": nc.tensor, "act": nc.scalar, "dve": nc.vector,
                    "pool": nc.gpsimd, "sp": nc.sync}
        self._ctx = nc.cleanup_on_exit()
        self._ctx.__enter__()
        self.sem = {e: nc.alloc_semaphore("sem_" + e) for e in self.ENG}
        self.dpool = [nc.alloc_semaphore("dsem%d" % i) for i in range(80)]
        nc.all_engine_barrier()
        for s_ in list(self.sem.values()) + self.dpool:
            nc.gpsimd.sem_clear(s_)
        nc.all_engine_barrier()
        self.cnt = {e: 0 for e in self.ENG}
        self.known = {e: {} for e in self.ENG}
        self.dma_bufs = []
        self.self_sync = True

    def _wait(self, e, toks):
        best = {}
        for (sem, val, owner) in toks:
            if owner == e and (e == "pe" or not self.self_sync):
                continue
            k = id(sem)
            if self.known[e].get(k, 0) >= val:
                continue
            if k not in best or best[k][1] < val:
                best[k] = (sem, val)
        for k, (sem, val) in best.items():
            self.eng[e].wait_ge(sem, val)
            self.known[e][k] = val

    def _deps(self, reads, writes, e=None):
        toks = []
        for b in reads:
            toks += b.w
            if b.excl:
                toks += [t for t in b.r if t[2] != e]
        for b in writes:
            toks += b.w + b.r
        return toks

    def op(self, e, fn, reads=(), writes=(), inc=True):
        self._wait(e, self._deps(reads, writes, e))
        ins = fn()
        if inc:
            self.cnt[e] += 1
            ins.then_inc(self.sem[e], 1)
            tok = (self.sem[e], self.cnt[e], e)
            for b in reads:
                b.r.append(tok)
                if len(b.r) > 12:
                    b.r = b.r[-12:] if False else b.r
            for b in writes:
                b.w = [tok]
                b.r = []
        return ins

    def group(self, e, fns, reads=(), writes=()):
        self._wait(e, self._deps(reads, writes, e))
        ins = None
        for fn in fns:
            ins = fn()
        self.cnt[e] += 1
        ins.then_inc(self.sem[e], 1)
        tok = (self.sem[e], self.cnt[e], e)
        for b in reads:
            b.r.append(tok)
        for b in writes:
            b.w = [tok]
            b.r = []

    def dma(self, q, out, in_, sb, reads=(), writes=(), add=False):
        self._wait(q, self._deps(reads, writes))
        if sb.dsem is None:
            sb.dsem = self.dpool.pop()
            self.dma_bufs.append(sb)
        sb.dval += 16
        self.eng[q].dma_start(out=out, in_=in_).then_inc(sb.dsem, 16)
        tok = (sb.dsem, sb.dval, "dma")
        for b in reads:
            b.r.append(tok)
        for b in writes:
            if add:
                b.w.append(tok)
            else:
                b.w = [tok]
                b.r = []
        return tok

    def dma_fn(self, q, fn, sb, reads=(), writes=()):
        self._wait(q, self._deps(reads, writes, q))
        if sb.dsem is None:
            sb.dsem = self.dpool.pop()
            self.dma_bufs.append(sb)
        sb.dval += 16
        fn().then_inc(sb.dsem, 16)
        tok = (sb.dsem, sb.dval, "dma")
        for b in reads:
            b.r.append(tok)
        for b in writes:
            b.w = [tok]
            b.r = []
        return tok

    def finish(self):
        self.barrier()
        self._ctx.__exit__(None, None, None)

    def barrier(self, engines=None):
        engines = engines or self.ENG
        toks = [(self.sem[f], self.cnt[f], f) for f in self.ENG if self.cnt[f] > 0]
        toks += [(b.dsem, b.dval, "dma") for b in self.dma_bufs]
        for e in engines:
            ss = self.self_sync
            self.self_sync = True
            self._wait(e, [t for t in toks if t[2] != e])
            self.self_sync = ss


def _fm(v, nchunk):
    return np.ascontiguousarray(v.reshape(nchunk, 128).T)


def _rope_tables():
    t = np.arange(S, dtype=np.float32)

    def tab(rot_dim, hd):
        inv = (500000.0 ** (-np.arange(0, rot_dim, 2, dtype=np.float32) / rot_dim)).astype(np.float32)
        ang = t[:, None] * inv[None, :]
        cos, sin = np.cos(ang).astype(np.float32), np.sin(ang).astype(np.float32)
        half = rot_dim // 2
        C = np.ones((128, S), np.float32)
        Sg = np.zeros((128, S), np.float32)
        for p in range(128):
            d = p % hd
            if d < half:
                C[p] = cos[:, d]
                Sg[p] = -sin[:, d]
            elif d < 2 * half:
                C[p] = cos[:, d - half]
                Sg[p] = sin[:, d - half]
        return C, Sg

    cn, sn = tab(16, 64)
    cd, sd = tab(8, 32)
    return cn, sn, cd, sd


def _const_mats():
    def blk(hd):
        m = np.zeros((128, 128), np.float32)
        for p in range(128):
            b = p // hd
            m[p, b * hd:(b + 1) * hd] = 1.0 / hd
        return m

    def perm(hd, half):
        m = np.zeros((128, 128), np.float32)
        for p in range(128):
            d = p % hd
            if d < half:
                m[p + half, p] = 1.0
            elif d < 2 * half:
                m[p - half, p] = 1.0
        return m

    ident = np.eye(128, dtype=np.float32)
    k = np.arange(128)[:, None]
    q = np.arange(128)[None, :]
    negc = np.where(k > q, NEGM, 0.0).astype(np.float32)
    negw = np.where(k <= q, NEGM, 0.0).astype(np.float32)
    negcmp = np.zeros((128, NT, 128), np.float32)
    for qt in range(NT):
        t = qt * 128 + np.arange(128)[None, :]
        c = np.arange(128)[:, None]
        negcmp[:, qt, :] = np.where(16 * c + 31 <= t, 0.0, NEGM)
    esel = np.zeros((32, NT, 128), np.float32)
    for kt in range(NT):
        for key in range(128):
            esel[(kt * 128 + key) // 64, kt, key] = 1.0
    ov = np.zeros((128, 32), np.float32)
    for c in range(N_CMP):
        for j in range(32):
            o = min(16 * c + 32, 64 * j + 64) - max(16 * c, 64 * j)
            ov[c, j] = max(o, 0) / 32.0
    forced = np.zeros((128, NT, 32), np.float32)
    invalid = np.zeros((128, NT, 32), np.float32)
    for qt in range(NT):
        for p in range(128):
            t = qt * 128 + p
            cur = t // 64
            for j in range(32):
                if j > cur:
                    invalid[p, qt, j] = 1.0
                elif j == 0 or j == cur or j == cur - 1:
                    forced[p, qt, j] = 1.0
    ltri = (np.arange(128)[:, None] < np.arange(128)[None, :]).astype(np.float32)
    return dict(blk64=blk(64), blk32=blk(32), perm64=perm(64, 8), perm32=perm(32, 4), ident=ident,
                negc=negc, negw=negw, negcmp=negcmp.reshape(128, NT * 128), esel=esel.reshape(32, NT * 128),
                ov=ov, forced=forced.reshape(128, NT * 32), invalid=invalid.reshape(128, NT * 32), ltri=ltri)


def _win_cols(w_in):
    o_q, o_kv, o_g, o_qd, o_kd, o_vd = 0, 512, 1280, 1304, 1816, 2328
    cols = []

    def kv(i, g):
        base = o_kv + i * 128 + g * 64
        return list(range(base, base + 64))

    for j in range(4):
        cols += list(range(o_q + j * 128, o_q + (j + 1) * 128))
    for g in range(2):
        cols += kv(2, g) + kv(2, g)
    for g in range(2):
        cols += kv(4, g) + kv(4, g)
    for g in range(2):
        cols += kv(0, g) + kv(0, g)
    for g in range(2):
        cols += kv(1, g) + kv(1, g)
    for j in range(4):
        cols += list(range(o_qd + j * 128, o_qd + (j + 1) * 128))
    for j in range(4):
        cols += list(range(o_kd + j * 128, o_kd + (j + 1) * 128))
    cols += kv(3, 0) + kv(3, 1) + kv(5, 0) + kv(5, 1)
    cols += list(range(o_g, o_g + 24))
    cols += list(range(o_vd, o_vd + 512))
    assert len(cols) == NCOLS
    return np.ascontiguousarray(w_in[:, cols])


_CACHE = {}


def _host_consts():
    if "c" not in _CACHE:
        cn, sn, cd, sd = _rope_tables()
        cm = _const_mats()
        _CACHE["c"] = dict(cn=cn, sn=sn, cd=cd, sd=sd, **cm)
    return _CACHE["c"]


def build(nseq, stage="full", dbg=False):
    nc = bass.Bass("TRN2", target_bir_lowering=False)
    sy = Sync(nc)
    eng = sy.eng
    PE, ACT, DVE, POOL = eng["pe"], eng["act"], eng["dve"], eng["pool"]

    used_inputs = []

    def din(name, shape, dt=F32):
        used_inputs.append(name)
        return nc.dram_tensor(name, list(shape), dt, kind="ExternalInput").ap()

    x_d = din("x", [nseq, S, D])
    cT_d = din("cT", [128, 8, nseq])
    wada_d = din("w_ada", [D, 6 * D])
    bada_fm_d = din("b_ada_fm", [128, 48])
    bada_row_d = din("b_ada_row", [1, 6 * D])
    gmix_d = din("gmix_fm", [128, 8])
    gffn_d = din("gffn_fm", [128, 8])
    win_d = din("w_inp", [D, NCOLS])
    rowgain_d = din("rowgain", [128, 5])
    gk0_d = din("gk0_row", [1, 128])
    go_d = din("go_row", [1, 64])
    lamv_d = din("lamv", [1, 128])
    pefm_d = din("pe_fm", [128, 32])
    wc1_d = din("w_cmp1", [2, 2048, 256])
    wc2k_d = din("w_cmp2k", [256, 128])
    wc2v_d = din("w_cmp2v", [256, 64])
    wout_d = din("w_out", [D, D])
    cn_d = din("cn", [128, S]); sn_d = din("sn", [128, S])
    cd_d = din("cd", [128, S]); sd_d = din("sd", [128, S])
    mats_d = {k: din("m_" + k, shp) for k, shp in [
        ("blk64", [128, 128]), ("blk32", [128, 128]), ("perm64", [128, 128]), ("perm32", [128, 128]),
        ("ident", [128, 128]), ("negc", [128, 128]), ("negw", [128, 128]), ("negcmp", [128, S]),
        ("esel", [32, S]), ("ov", [128, 32]), ("forced", [128, NT * 32]), ("invalid", [128, NT * 32]),
        ("ltri", [128, 128])]}
    ecap_d = din("ecap", [128, 32])
    wr_d = din("w_r", [D, 36])
    br_d = din("b_r", [1, 36])
    wg_d = din("w_g", [NEXP, D, 512])
    wu_d = din("w_u", [NEXP, D, 512])
    wd_d = din("w_d", [NEXP, 512, D])
    out_d = nc.dram_tensor("out", [nseq, S, D], F32, kind="ExternalOutput").ap()
    modrow_d = nc.dram_tensor("modrow", [nseq, 6 * D], F32, kind="Internal").ap()
    x1_d = nc.dram_tensor("x1s", [nseq, S, D], F32, kind="Internal").ap()
    dbg_d = {}

    def sb(name, shape, dt=F32):
        return nc.alloc_sbuf_tensor(name, list(shape), dt)

    banks = [nc.alloc_psum_tensor("bank%d" % i, [128, 512], F32) for i in range(8)]
    bbuf = [Buf("bank%d" % i, excl=True) for i in range(8)]

    class Rot:
        def __init__(self, idx):
            self.idx = idx; self.i = 0

        def next(self):
            k = self.idx[self.i % len(self.idx)]
            self.i += 1
            return banks[k], bbuf[k]

    rotP = Rot([0, 1])
    rotS = Rot([2, 3, 4, 7])
    rotO = Rot([5, 6])

    cb = {}

    def const_load(name, d_ap, shape, dt, q="pool"):
        t = sb("c_" + name, shape, dt)
        b = Buf("c_" + name)
        sy.dma(q, t[:], d_ap, b, writes=[b])
        cb[name] = (t, b)
        return t, b

    for k in ["blk64", "blk32", "perm64", "perm32", "ident", "negc", "negw"]:
        const_load(k, mats_d[k][:, :], [128, 128], BF16)
    const_load("negcmp", mats_d["negcmp"][:, :], [128, S], BF16)
    esel_t = sb("c_esel", [128, S], BF16); esel_b = Buf("c_esel")
    sy.op("pool", lambda: POOL.memset(esel_t[:], 0.0), writes=[esel_b])
    sy.dma("pool", esel_t[0:32, :], mats_d["esel"][:, :], esel_b, writes=[esel_b])
    cb["esel"] = (esel_t, esel_b)
    const_load("forced", mats_d["forced"][:, :], [128, NT * 32], F32, q="sp")
    const_load("invalid", mats_d["invalid"][:, :], [128, NT * 32], F32, q="sp")
    const_load("identf", mats_d["ident"][:, :], [128, 128], F32, q="sp")
    const_load("rowgain", rowgain_d[:, :], [128, 5], F32, q="sp")
    const_load("gmix", gmix_d[:, :], [128, 8], F32, q="sp")
    const_load("gffn", gffn_d[:, :], [128, 8], F32, q="sp")
    const_load("bada_fm", bada_fm_d[:, :], [128, 48], F32, q="sp")
    const_load("cT", cT_d[:, :, :], [128, 8, nseq], BF16)
    const_load("gk0", gk0_d[0:1, :].partition_broadcast(128), [128, 128], F32)
    const_load("go", go_d[0:1, :].partition_broadcast(128), [128, 64], F32)
    const_load("lamv", lamv_d[0:1, :].partition_broadcast(128), [128, 128], F32)
    const_load("pefm", pefm_d[:, :], [128, 32], BF16)
    const_load("wout", wout_d.rearrange("(k p) n -> p k n", p=128), [128, 8, D], BF16)
    const_load("wc1k", wc1_d[0].rearrange("(j p) n -> p j n", p=128), [128, 16, 256], BF16)
    const_load("wc1v", wc1_d[1].rearrange("(j p) n -> p j n", p=128), [128, 16, 256], BF16)
    const_load("wc2k", wc2k_d.rearrange("(m p) n -> p m n", p=128), [128, 2, 128], BF16)
    const_load("wc2v", wc2v_d.rearrange("(m p) n -> p m n", p=128), [128, 2, 64], BF16)

    if stage == "consts":
        sy.finish()
        return nc, sy, locals()

    def C(name):
        return cb[name][0]

    def CB(name):
        return cb[name][1]

    epsT = sb("epsT", [128, 1]); epsB = Buf("epsT")
    sy.op("dve", lambda: DVE.memset(epsT[:], EPS), writes=[epsB])

    modT = sb("modT", [128, 48, nseq]); modTB = Buf("modT")
    gs1 = sb("gs1", [128, 8, nseq]); gs1B = Buf("gs1")
    gs2 = sb("gs2", [128, 8, nseq]); gs2B = Buf("gs2")
    neglam = sb("neglam", [128, 1]); neglamB = Buf("neglam")
    wst = [sb("wst%d" % i, [128, 8, 512], BF16) for i in range(2)]
    wstB = [Buf("wst%d" % i) for i in range(2)]
    stg = [sb("stg%d" % i, [128, 1024]) for i in range(2)]
    stgB = [Buf("stg%d" % i) for i in range(2)]
    wi = [0]

    def wload(d_ap, ncols):
        i = wi[0] % 2
        wi[0] += 1
        sy.dma("pool", wst[i][:, :, 0:ncols], d_ap, wstB[i], writes=[wstB[i]])
        return wst[i], wstB[i]

    wada_v = wada_d.rearrange("(k p) n -> p k n", p=128)
    for gi in range(12):
        wt, wB = wload(wada_v[:, :, gi * 512:(gi + 1) * 512], 512)
        if True:
            pt, pB = rotP.next()
            fns = []
            for mc in range(4):
                for k in range(8):
                    fns.append(lambda mc=mc, k=k: PE.matmul(
                        pt[:, mc * nseq:(mc + 1) * nseq], lhsT=wt[:, k, mc * 128:(mc + 1) * 128],
                        rhs=C("cT")[:, k, :], start=(k == 0), stop=(k == 7)))
            sy.group("pe", fns, reads=[wB, CB("cT")], writes=[pB])
            for mc in range(4):
                j = gi * 4 + mc
                sy.op("dve", lambda mc=mc, j=j: DVE.tensor_scalar(
                    out=modT[:, j, :], in0=pt[:, mc * nseq:(mc + 1) * nseq],
                    scalar1=C("bada_fm")[:, j:j + 1], scalar2=None, op0=ALU.add),
                    reads=[pB, CB("bada_fm")], writes=[modTB])
    for k in range(8):
        sy.op("dve", lambda k=k: DVE.tensor_scalar(out=gs1[:, k, :], in0=modT[:, 8 + k, :], scalar1=1.0,
                                                    scalar2=C("gmix")[:, k:k + 1], op0=ALU.add, op1=ALU.mult),
              reads=[modTB, CB("gmix")], writes=[gs1B])
        sy.op("dve", lambda k=k: DVE.tensor_scalar(out=gs2[:, k, :], in0=modT[:, 32 + k, :], scalar1=1.0,
                                                    scalar2=C("gffn")[:, k:k + 1], op0=ALU.add, op1=ALU.mult),
              reads=[modTB, CB("gffn")], writes=[gs2B])
    if stage == "ada":
        sy.finish()
        return nc, sy, locals()
    lt = sb("lamtmp", [128, 8]); ltB = Buf("lamtmp")
    lv = C("lamv")
    lprod = sb("lamprod", [128, 64])
    sy.op("dve", lambda: DVE.tensor_tensor(out=lprod[:, 0:32], in0=lv[:, 0:32], in1=lv[:, 32:64], op=ALU.mult),
          reads=[CB("lamv")], writes=[ltB])
    sy.op("dve", lambda: DVE.tensor_tensor(out=lprod[:, 32:64], in0=lv[:, 64:96], in1=lv[:, 96:128], op=ALU.mult),
          reads=[CB("lamv")], writes=[ltB])
    sy.op("dve", lambda: DVE.tensor_reduce(out=lt[:, 0:2], in_=lprod[:].rearrange("p (a b) -> p a b", a=2),
                                            axis=AX.X, op=ALU.add), reads=[ltB], writes=[ltB])
    lt2 = sb("lamtmp2", [128, 2]); lt2B = Buf("lamtmp2")
    sy.op("act", lambda: ACT.activation(out=lt2[:, 0:2], in_=lt[:, 0:2], func=AF.Exp), reads=[ltB], writes=[lt2B])
    sy.op("dve", lambda: DVE.tensor_tensor(out=lt[:, 4:5], in0=lt2[:, 1:2], in1=lt2[:, 0:1], op=ALU.subtract),
          reads=[lt2B], writes=[ltB])
    sy.op("dve", lambda: DVE.tensor_scalar(out=neglam[:, 0:1], in0=lt[:, 4:5], scalar1=-LAMBDA_INIT, scalar2=None,
                                            op0=ALU.add), reads=[ltB], writes=[neglamB])

    peb = sb("peb", [128, 4]); pebB = Buf("peb")
    pt, pB = rotP.next()
    fns = []
    for kv in range(2):
        w1 = C("wc1k") if kv == 0 else C("wc1v")
        for mc in range(2):
            for j in range(16):
                fns.append(lambda kv=kv, mc=mc, j=j, w1=w1: PE.matmul(
                    pt[:, (kv * 2 + mc) * 2:(kv * 2 + mc) * 2 + 1], lhsT=w1[:, j, mc * 128:(mc + 1) * 128],
                    rhs=C("pefm")[:, kv * 16 + j:kv * 16 + j + 1], start=(j == 0), stop=(j == 15)))
    sy.group("pe", fns, reads=[CB("wc1k"), CB("wc1v"), CB("pefm")], writes=[pB])
    sy.op("dve", lambda: DVE.tensor_copy(out=peb[:, 0:4], in_=pt[:, 0:8:2]), reads=[pB], writes=[pebB])

    if stage == "setup":
        sy.finish()
        return nc, sy, locals()
    return _build_rest(nc, sy, locals(), nseq, stage, dbg)


class _Stop(Exception):
    pass


class Arena:
    def __init__(self, nc, name, nbytes):
        self.t = nc.alloc_sbuf_tensor(name, [128, nbytes // 2], BF16)
        self.cap = nbytes // 2
        self.off = 0

    def reset(self):
        self.off = 0

    def get(self, shape, dt):
        n = 1
        for s_ in shape[1:]:
            n *= s_
        esz = 4 if dt in (F32, I32, U32) else 2
        n2 = n * esz // 2
        self.off = (self.off + 15) // 16 * 16
        assert self.off + n2 <= self.cap, ("arena overflow", self.off, n2, self.cap)
        ap = self.t[0:shape[0], self.off:self.off + n2]
        self.off += n2
        if esz == 4:
            ap = ap.bitcast(dt)
        if len(shape) == 3:
            ap = ap.rearrange("p (a b) -> p a b", a=shape[1])
        elif len(shape) == 4:
            ap = ap.rearrange("p (a b c) -> p a b c", a=shape[1], b=shape[2])
        return ap


def _build_rest(nc, sy, L, nseq, stage, dbg):
    try:
        return _build_rest2(nc, sy, L, nseq, stage, dbg)
    except _Stop:
        sy.finish()
        return nc, sy, L


def _build_rest2(nc, sy, L, nseq, stage, dbg):
    g = dict(L)

    def stop_if(name):
        if stage == name:
            raise _Stop()

    used_inputs = g["used_inputs"]
    PE, ACT, DVE, POOL = g["PE"], g["ACT"], g["DVE"], g["POOL"]
    C, CB = g["C"], g["CB"]
    rotP, rotS, rotO, Rot = g["rotP"], g["rotS"], g["rotO"], g["Rot"]
    rotPP = Rot([0, 1, 7, 2, 3, 4, 5, 6])
    x_d, win_d, out_d, x1_d, modrow_d = g["x_d"], g["win_d"], g["out_d"], g["x1_d"], g["modrow_d"]
    gs1, gs1B, gs2, gs2B, modT, modTB = g["gs1"], g["gs1B"], g["gs2"], g["gs2B"], g["modT"], g["modTB"]
    neglam, neglamB, peb, pebB, epsT, epsB = g["neglam"], g["neglamB"], g["peb"], g["pebB"], g["epsT"], g["epsB"]
    wload, stg, stgB = g["wload"], g["stg"], g["stgB"]
    cn_d, sn_d, cd_d, sd_d = g["cn_d"], g["sn_d"], g["cd_d"], g["sd_d"]

    NROWS_ = NEXP * CAP + 128
    xbuf_d = nc.dram_tensor("xbuf", [NROWS_, D], BF16, kind="Internal").ap()
    ybuf_d = nc.dram_tensor("ybuf", [NROWS_, D], BF16, kind="Internal").ap()
    xbufB = Buf("xbuf")
    if stage not in ("attn",):
        ztile = nc.alloc_sbuf_tensor("ztile", [128, D], BF16); ztileB = Buf("ztile")
        sy.op("dve", lambda: DVE.memset(ztile[:], 0.0), writes=[ztileB])
        for r0 in range(0, NROWS_, 128):
            sy.dma("sp", xbuf_d[r0:r0 + 128, :], ztile[:], ztileB, reads=[ztileB], writes=[xbufB], add=True)
        sy.dma("sp", ybuf_d[NEXP * CAP:NEXP * CAP + 128, :], ztile[:], ztileB, reads=[ztileB])
    g["xbuf_d"], g["ybuf_d"], g["xbufB"] = xbuf_d, ybuf_d, xbufB
    arena = Arena(nc, "arena", 133248)
    A = arena.get
    hT = A([128, 8, TCH], BF16); hTB = Buf("hT")
    QNOPE = A([128, 8, TCH], BF16); QNOPEB = Buf("qnope")
    QROPE = A([128, 8, TCH], BF16); QROPEB = Buf("qrope")
    QD = A([128, 4, TCH], BF16); QDB = Buf("qd")
    KD = A([128, 4, S], BF16); KDB = Buf("kd")
    KS = A([128, 2, S], BF16); KSB = Buf("ks")
    KW = A([128, 2, S], BF16); KWB = Buf("kw")
    KC2 = A([128, 2, TCH + 32], BF16); KC2B = Buf("kc2")
    VC2 = A([128, 2, TCH + 32], BF16); VC2B = Buf("vc2")
    VS = A([128, NT, 2, 65], BF16); VSB = Buf("vs")
    VW = A([128, NT, 2, 65], BF16); VWB = Buf("vw")
    VD = A([128, NT, 8, 65], BF16); VDB = Buf("vd")
    HIDK = A([128, 2, 2, 128], BF16); HIDKB = Buf("hidk")
    HIDV = A([128, 2, 2, 128], BF16); HIDVB = Buf("hidv")
    KCMPT = A([128, 2, 128], BF16); KCMPTB = Buf("kcmpt")
    VCMP = A([128, 2, 97], BF16); VCMPB = Buf("vcmp")
    ropeT = A([128, 4, TCH], F32); ropeTB = Buf("ropeT")
    GATE = A([128, 4, 24], F32); GATEB = Buf("gate")
    G1 = A([128, D], F32); G1B = Buf("g1")
    xn = A([128, D], BF16); xnB = Buf("xn")
    PT = [A([128, 512], BF16) for _ in range(4)]; PTB = [Buf("pt%d" % i) for i in range(4)]
    sqb = [PT[0], PT[1]]; sqbB = [PTB[0], PTB[1]]
    lnb = A([128, 512], F32); lnbB = Buf("lnb")
    rstd = A([128, 512], F32); rstdB = Buf("rstd")
    qnb = [A([128, 512], BF16) for _ in range(3)]; qnbB = [Buf("qn%d" % i) for i in range(3)]
    t1b = A([128, 512], F32); t1bB = Buf("t1b")
    t2b = A([128, 512], F32); t2bB = Buf("t2b")
    small = A([128, 256], F32); smallB = Buf("small")
    onsa, onsaB = lnb, lnbB
    odif, odifB = t2b, t2bB
    tmpo, tmpoB = t1b, t1bB
    attn = A([128, D], BF16); attnB = Buf("attn")
    attn2 = [(attn, attnB), (A([128, D], BF16), Buf("attn_b"))]
    attnT = A([128, 8, 128], BF16); attnTB = Buf("attnT")
    score = A([128, 2, 32], F32); scoreB = Buf("score")
    negsel = A([128, 2, 32], BF16); negselB = Buf("negsel")
    NEGSELT = A([128, 2, 128], BF16); NEGSELTB = Buf("negselT")
    kcn = A([128, 128], BF16); kcnB = Buf("kcn")

    sy.op("pool", lambda: POOL.memset(HIDK, 0.0), writes=[HIDKB])
    sy.op("pool", lambda: POOL.memset(HIDV, 0.0), writes=[HIDVB])
    sy.op("pool", lambda: POOL.memset(KC2, 0.0), writes=[KC2B])
    sy.op("pool", lambda: POOL.memset(VC2, 0.0), writes=[VC2B])
    sy.op("pool", lambda: POOL.memset(VS, 1.0), writes=[VSB])
    sy.op("pool", lambda: POOL.memset(VW, 1.0), writes=[VWB])
    sy.op("pool", lambda: POOL.memset(VD, 1.0), writes=[VDB])
    sy.op("pool", lambda: POOL.memset(VCMP, 1.0), writes=[VCMPB])
    sy.op("pool", lambda: POOL.memset(NEGSELT, 0.0), writes=[NEGSELTB])
    NEGSELT_b = A([128, 2, 128], BF16); NEGSELT_bB = Buf("negselT_b")
    sy.op("pool", lambda: POOL.memset(NEGSELT_b, 0.0), writes=[NEGSELT_bB])
    onsa2 = [(onsa, onsaB), (rstd, rstdB)]
    score2 = [(score, scoreB), (A([128, 2, 32], F32), Buf("score_b"))]
    negsel2 = [(negsel, negselB), (A([128, 2, 32], BF16), Buf("negsel_b"))]
    NEGSELT2 = [(NEGSELT, NEGSELTB), (NEGSELT_b, NEGSELT_bB)]
    sy.op("pool", lambda: POOL.memset(QNOPE, 0.0), writes=[QNOPEB])
    sy.op("pool", lambda: POOL.memset(QROPE, 0.0), writes=[QROPEB])
    goS = nc.alloc_sbuf_tensor("goS", [128, 64], F32); goSB = Buf("goS")
    sy.op("dve", lambda: DVE.tensor_scalar(out=goS[:], in0=C("go")[:], scalar1=1.0 - LAMBDA_INIT, scalar2=None, op0=ALU.mult),
          reads=[CB("go")], writes=[goSB])
    causal01 = nc.alloc_sbuf_tensor("causal01", [128, 128], BF16); causal01B = Buf("causal01")
    sy.op("dve", lambda: DVE.tensor_scalar(out=causal01[:], in0=C("negc")[:], scalar1=-0.5, scalar2=None, op0=ALU.is_gt),
          reads=[CB("negc")], writes=[causal01B])
    ovt = sb_tmp = nc.alloc_sbuf_tensor("ovt", [128, 32], BF16)
    ovB = Buf("ovt")
    sy.dma("pool", ovt[:], g["mats_d"]["ov"][:, :], ovB, writes=[ovB])
    for gq in range(2):
        sy.op("dve", lambda gq=gq: DVE.tensor_copy(out=VCMP[:, gq, 65:97], in_=ovt[:]), reads=[ovB], writes=[VCMPB])

    stop_if("init")
    ident = C("ident"); identB = CB("ident")
    rowgain = C("rowgain")
    cnt = {"sq": 0, "qn": 0, "pt": 0}

    def normrope(pt, pB, blkname, permname, gcol, ctab, stab, nope_dsts, nope_bufs, rope_dsts, rope_bufs, ncols=TCH):
        i = cnt["sq"] % 2; cnt["sq"] += 1
        sq, sqB = sqb[i], sqbB[i]
        sy.op("act", lambda: ACT.activation(out=sq[:, 0:ncols], in_=pt[:, 0:ncols], func=AF.Square),
              reads=[pB], writes=[sqB])
        p2, p2B = rotPP.next()
        sy.op("pe", lambda: PE.matmul(p2[:, 0:ncols], lhsT=C(blkname)[:], rhs=sq[:, 0:ncols], start=True, stop=True),
              reads=[sqB, CB(blkname)], writes=[p2B])
        sy.op("act", lambda: ACT.activation(out=lnb[:, 0:ncols], in_=p2[:, 0:ncols], func=AF.Ln, bias=epsT[:, 0:1]),
              reads=[p2B, epsB], writes=[lnbB])
        sy.op("act", lambda: ACT.activation(out=rstd[:, 0:ncols], in_=lnb[:, 0:ncols], func=AF.Exp, scale=-0.5),
              reads=[lnbB], writes=[rstdB])
        j = cnt["qn"] % 3; cnt["qn"] += 1
        qn, qnB = qnb[j][:, 0:ncols], qnbB[j]
        sy.op("dve", lambda: DVE.scalar_tensor_tensor(out=qn, in0=pt[:, 0:ncols], scalar=rowgain[:, gcol:gcol + 1],
                                                      in1=rstd[:, 0:ncols], op0=ALU.mult, op1=ALU.mult),
              reads=[pB, rstdB, CB("rowgain")], writes=[qnB])
        for (dst, lo, hi) in (nope_dsts or []):
            sy.op("act", lambda: ACT.activation(out=dst, in_=qn[lo:hi, :], func=AF.Copy), reads=[qnB], writes=nope_bufs)
        yield
        p3, p3B = rotPP.next()
        sy.op("pe", lambda: PE.matmul(p3[:, 0:ncols], lhsT=C(permname)[:], rhs=qn, start=True, stop=True),
              reads=[qnB, CB(permname)], writes=[p3B])
        sy.op("dve", lambda: DVE.tensor_tensor(out=t1b[:, 0:ncols], in0=qn, in1=ctab, op=ALU.mult),
              reads=[qnB, ropeTB], writes=[t1bB])
        sy.op("dve", lambda: DVE.tensor_tensor(out=t2b[:, 0:ncols], in0=p3[:, 0:ncols], in1=stab, op=ALU.mult),
              reads=[p3B, ropeTB], writes=[t2bB])
        for (dst, lo, hi) in rope_dsts:
            sy.op("dve", lambda: DVE.tensor_tensor(out=dst, in0=t1b[lo:hi, 0:ncols], in1=t2b[lo:hi, 0:ncols], op=ALU.add),
                  reads=[t1bB, t2bB], writes=rope_bufs)

    win_v = win_d.rearrange("(k p) n -> p k n", p=128)
    xi = [0]

    negc, negw, negcmp, esel = C("negc"), C("negw"), C("negcmp"), C("esel")
    ptc = [0]
    m8 = nc.alloc_sbuf_tensor("m8", [128, 16], F32); m8B = Buf("m8")

    def run_jobs(jobs):
        st = [None] * len(jobs)

        def issue_qk(n):
            bank, bB = rotS.next()
            st[n] = (bank, bB)
            jb = jobs[n]
            sy.group("pe", [(lambda f=f, bank=bank: f(bank)) for f in jb["qk"]], reads=jb["qk_reads"], writes=[bB])

        DEPTH = 3
        for n in range(min(DEPTH, len(jobs))):
            issue_qk(n)
        for n, jb in enumerate(jobs):
            if n + DEPTH < len(jobs):
                issue_qk(n + DEPTH)
            bank, bB = st[n]
            pi = ptc[0] % 4; ptc[0] += 1
            P, PB = PT[pi], PTB[pi]
            ncol = jb["ncol"]
            sy.op("act", lambda: ACT.activation(out=P[:, 0:ncol], in_=bank[:, 0:ncol], func=AF.Exp, scale=jb["scale"]),
                  reads=[bB], writes=[PB])
            if jb.get("mask01") is not None:
                c0 = jb["mask01"]
                sy.op("pool", lambda: POOL.tensor_tensor(out=P[:, c0:c0 + 128], in0=P[:, c0:c0 + 128], in1=causal01[:], op=ALU.mult),
                      reads=[PB, causal01B], writes=[PB])
            sy.group("pe", [(lambda f=f, P=P: f(P)) for f in jb["pv"]], reads=[PB] + jb["pv_reads"], writes=[jb["O"]])
            if jb.get("post") is not None:
                jb["post"]()

    def make_jobs(items, Oap, OB, scale, vreads, kreads, post=None, postmask=False):
        jobs = []
        nb = (len(items) + 3) // 4
        for bi in range(nb):
            blk = items[bi * 4:(bi + 1) * 4]
            qk, pv = [], []
            mask01 = None
            for n, (lk, rq, masks, va, tp) in enumerate(blk):
                gidx = bi * 4 + n
                if postmask and masks:
                    mask01 = n * 128
                    masks = []
                def fqk(bank, lk=lk, rq=rq, masks=masks, n=n, tp=tp):
                    out = bank[:, n * 128:(n + 1) * 128]
                    kw = {} if tp is None else {"tile_position": tp}
                    r = PE.matmul(out, lhsT=lk, rhs=rq, start=True, stop=(len(masks) == 0), **kw)
                    for mi, (ml, mr) in enumerate(masks):
                        r = PE.matmul(out, lhsT=ml, rhs=mr, start=False, stop=(mi == len(masks) - 1))
                    return r
                qk.append(fqk)
                def fpv(P, va=va, n=n, gidx=gidx):
                    return PE.matmul(Oap, lhsT=P[:, n * 128:(n + 1) * 128], rhs=va, start=(gidx == 0), stop=(gidx == len(items) - 1))
                pv.append(fpv)
            jobs.append(dict(qk=qk, qk_reads=kreads, ncol=len(blk) * 128, scale=scale, mask01=mask01, pv=pv,
                             pv_reads=vreads, O=OB, post=(post if bi == nb - 1 else None)))
        return jobs

    def id_masks(neg_ap):
        return [(ident[:], neg_ap)]

    def compress_A(c):
        i0 = 1 if c == 0 else 0
        nblk = 32 - i0
        blk0 = 32 * c - 1 + i0
        for kv in range(2):
            src_t, srcB = (KC2, KC2B) if kv == 0 else (VC2, VC2B)
            w1, w1B = (C("wc1k"), CB("wc1k")) if kv == 0 else (C("wc1v"), CB("wc1v"))
            HID, HIDB = (HIDK, HIDKB) if kv == 0 else (HIDV, HIDVB)
            for gq in range(2):
                pt, pB = rotP.next()
                fns = []
                for m in range(2):
                    for j in range(16):
                        st0 = 16 + 2 * j + 16 * i0
                        fns.append(lambda m=m, j=j, st0=st0: PE.matmul(
                            pt[:, m * 32 + i0:m * 32 + 32], lhsT=w1[:, j, m * 128:(m + 1) * 128],
                            rhs=src_t[:, gq, st0:st0 + 16 * (nblk - 1) + 1:16], start=(j == 0), stop=(j == 15)))
                sy.group("pe", fns, reads=[srcB, w1B], writes=[pB])
                hx = small[:, 64:128]
                for m in range(2):
                    sy.op("dve", lambda m=m: DVE.tensor_scalar(out=hx[:, m * 32:(m + 1) * 32], in0=pt[:, m * 32:(m + 1) * 32],
                                                               scalar1=peb[:, kv * 2 + m:kv * 2 + m + 1], scalar2=None, op0=ALU.add),
                          reads=[pB, pebB], writes=[smallB])
                h2 = small[:, 128:192]
                sy.op("dve", lambda: DVE.tensor_tensor(out=h2, in0=hx, in1=hx, op=ALU.mult), reads=[smallB], writes=[smallB])
                sy.op("dve", lambda: DVE.tensor_scalar(out=h2, in0=h2, scalar1=0.044715, scalar2=1.0, op0=ALU.mult, op1=ALU.add),
                      reads=[smallB], writes=[smallB])
                sy.op("dve", lambda: DVE.tensor_tensor(out=h2, in0=h2, in1=hx, op=ALU.mult), reads=[smallB], writes=[smallB])
                h3 = small[:, 192:256]
                sy.op("act", lambda: ACT.activation(out=h3, in_=h2, func=AF.Exp, scale=-1.5957691216057308),
                      reads=[smallB], writes=[smallB])
                sy.op("dve", lambda: DVE.tensor_scalar(out=h3, in0=h3, scalar1=1.0, scalar2=None, op0=ALU.add),
                      reads=[smallB], writes=[smallB])
                sy.op("dve", lambda: DVE.reciprocal(out=h2, in_=h3), reads=[smallB], writes=[smallB])
                for m in range(2):
                    sy.op("dve", lambda m=m: DVE.tensor_tensor(out=HID[:, gq, m, blk0:blk0 + nblk], in0=hx[:, m * 32 + i0:m * 32 + 32],
                                                               in1=h2[:, m * 32 + i0:m * 32 + 32], op=ALU.mult),
                          reads=[smallB], writes=[HIDB])
        for t_, tB in ((KC2, KC2B), (VC2, VC2B)):
            sy.op("dve", lambda: DVE.tensor_copy(out=t_[:, :, 0:32], in_=t_[:, :, TCH:TCH + 32]), reads=[tB], writes=[tB])

    def compress_B(c):
        for gq in range(2):
            pt, pB = rotP.next()
            sy.group("pe", [(lambda m=m: PE.matmul(pt[:, 0:128], lhsT=HIDK[:, gq, m, :], rhs=C("wc2k")[:, m, :],
                                                   start=(m == 0), stop=(m == 1))) for m in range(2)],
                     reads=[HIDKB, CB("wc2k")], writes=[pB])
            sy.op("act", lambda: ACT.activation(out=t1b[:, 0:64], in_=pt[:, 0:64], func=AF.Square, accum_out=small[:, 5:6]),
                  reads=[pB], writes=[t1bB, smallB])
            sy.op("act", lambda: ACT.activation(out=small[:, 6:7], in_=small[:, 5:6], func=AF.Ln, bias=epsT[:, 0:1], scale=1.0 / 64),
                  reads=[smallB, epsB], writes=[smallB])
            sy.op("act", lambda: ACT.activation(out=small[:, 7:8], in_=small[:, 6:7], func=AF.Exp, scale=-0.5),
                  reads=[smallB], writes=[smallB])
            sy.op("dve", lambda: DVE.scalar_tensor_tensor(out=kcn, in0=pt[:, 0:128], scalar=small[:, 7:8], in1=C("gk0")[:],
                                                          op0=ALU.mult, op1=ALU.mult), reads=[pB, smallB, CB("gk0")], writes=[kcnB])
            p2, p2B = rotP.next()
            p2b = p2[:].bitcast(BF16)
            sy.op("pe", lambda: PE.transpose(p2b[:, 0:128], kcn, ident[:]), reads=[kcnB, identB], writes=[p2B])
            sy.op("dve", lambda: DVE.tensor_copy(out=KCMPT[:, gq, :], in_=p2b[:, 0:128]), reads=[p2B], writes=[KCMPTB])
            p3, p3B = rotP.next()
            sy.group("pe", [(lambda m=m: PE.matmul(p3[:, 0:64], lhsT=HIDV[:, gq, m, :], rhs=C("wc2v")[:, m, :],
                                                   start=(m == 0), stop=(m == 1))) for m in range(2)],
                     reads=[HIDVB, CB("wc2v")], writes=[p3B])
            sy.op("act", lambda: ACT.activation(out=VCMP[:, gq, 0:64], in_=p3[:, 0:64], func=AF.Copy), reads=[p3B], writes=[VCMPB])

    def x1_dst(b, qt):
        t_ = out_d if stage == "attn" else x1_d
        return t_[b, qt * 128:(qt + 1) * 128, :]

    bct = nc.alloc_sbuf_tensor("bct", [128, 128], F32); bctB = Buf("bct")

    def bcast_row(dst, dstB, j0, b):
        for k in range(8):
            sy.op("dve", lambda: DVE.tensor_copy(out=bct[:], in_=modT[:, j0 + k, b:b + 1].to_broadcast([128, 128])),
                  reads=[modTB], writes=[bctB])
            p2, p2B = rotP.next()
            sy.op("pe", lambda: PE.transpose(p2[:, 0:128], bct[:], C("identf")[:]), reads=[bctB, CB("identf")], writes=[p2B])
            sy.op("act", lambda: ACT.activation(out=dst[:, k * 128:(k + 1) * 128], in_=p2[:, 0:128], func=AF.Copy),
                  reads=[p2B], writes=[dstB])

    def bcast_rows_from(srcT, srcB, dst, dstB, b):
        for k in range(8):
            sy.op("dve", lambda: DVE.tensor_copy(out=bct[:], in_=srcT[:, k, b:b + 1].to_broadcast([128, 128])),
                  reads=[srcB], writes=[bctB])
            p2, p2B = rotP.next()
            sy.op("pe", lambda: PE.transpose(p2[:, 0:128], bct[:], C("identf")[:]), reads=[bctB, CB("identf")], writes=[p2B])
            sy.op("act", lambda: ACT.activation(out=dst[:, k * 128:(k + 1) * 128], in_=p2[:, 0:128], func=AF.Copy),
                  reads=[p2B], writes=[dstB])

    SC_N = 0.125
    SC_D = 32.0 ** -0.5

    normed = set()

    def emit_norm_tile(b, c, i):
        if (b, c, i) in normed or b >= nseq:
            return
        normed.add((b, c, i))
        xt, xB = stg[xi[0] % 2], stgB[xi[0] % 2]; xi[0] += 1
        sy.dma("sp", xt[:], x_d[b, c * TCH + i * 128:c * TCH + (i + 1) * 128, :], xB, writes=[xB])
        sy.op("act", lambda: ACT.activation(out=t1b[:, 0:512], in_=xt[:, 0:512], func=AF.Square,
                                            accum_out=small[:, 0:1]), reads=[xB], writes=[t1bB, smallB])
        sy.op("act", lambda: ACT.activation(out=t1b[:, 0:512], in_=xt[:, 512:1024], func=AF.Square,
                                            accum_out=small[:, 1:2]), reads=[xB], writes=[t1bB, smallB])
        sy.op("dve", lambda: DVE.tensor_tensor(out=small[:, 2:3], in0=small[:, 0:1], in1=small[:, 1:2], op=ALU.add),
              reads=[smallB], writes=[smallB])
        sy.op("act", lambda: ACT.activation(out=small[:, 3:4], in_=small[:, 2:3], func=AF.Ln, bias=epsT[:, 0:1],
                                            scale=1.0 / D), reads=[smallB, epsB], writes=[smallB])
        sy.op("act", lambda: ACT.activation(out=small[:, 4:5], in_=small[:, 3:4], func=AF.Exp, scale=-0.5),
              reads=[smallB], writes=[smallB])
        sy.op("dve", lambda: DVE.tensor_scalar(out=xn, in0=xt[:], scalar1=small[:, 4:5], scalar2=None, op0=ALU.mult),
              reads=[xB, smallB], writes=[xnB])
        pt, pB = rotP.next()
        ptb = pt[:].bitcast(BF16)
        sy.group("pe", [(lambda k=k: PE.transpose(ptb[:, k * 128:(k + 1) * 128], xn[:, k * 128:(k + 1) * 128], ident[:]))
                        for k in range(8)], reads=[xnB, identB], writes=[pB])
        for k in range(8):
            e_ = "act" if k % 2 == 0 else "dve"
            if e_ == "act":
                sy.op("act", lambda k=k: ACT.activation(out=hT[:, k, i * 128:(i + 1) * 128], in_=ptb[:, k * 128:(k + 1) * 128],
                                                        func=AF.Identity, scale=gs1[:, k, b:b + 1], bias=modT[:, k, b:b + 1]),
                      reads=[pB, gs1B, modTB], writes=[hTB])
            else:
                sy.op("dve", lambda k=k: DVE.tensor_scalar(out=hT[:, k, i * 128:(i + 1) * 128], in0=ptb[:, k * 128:(k + 1) * 128],
                                                           scalar1=gs1[:, k, b:b + 1], scalar2=modT[:, k, b:b + 1],
                                                           op0=ALU.mult, op1=ALU.add),
                      reads=[pB, gs1B, modTB], writes=[hTB])

    def attention_chunk(b, c):
        compress_B(c)
        stop_if("cmpr")

        def cmp_part(i):
            qt = c * 4 + i
            qs = slice(i * 128, (i + 1) * 128)
            onsa, onsaB = onsa2[i % 2]
            score, scoreB = score2[i % 2]
            negsel, negselB = negsel2[i % 2]
            NEGSELT, NEGSELTB = NEGSELT2[i % 2]
            attn, attnB = attn2[i % 2]
            jobs = []
            for h in range(8):
                hp, j, gq = (h % 2) * 64, h // 2, h // 4
                Ot, OB = rotO.next()
                items = [(KCMPT[:, gq, :], QNOPE[:, h, qs], id_masks(negcmp[:, qt * 128:(qt + 1) * 128]),
                          VCMP[:, gq, :], None)]

                def post(h=h, Ot=Ot, OB=OB, gq=gq):
                    sy.op("dve", lambda: DVE.tensor_scalar(out=small[:, 8:9], in0=Ot[:, 64:65], scalar1=1e-30, scalar2=None, op0=ALU.add),
                          reads=[OB], writes=[smallB])
                    sy.op("dve", lambda: DVE.reciprocal(out=small[:, 9:10], in_=small[:, 8:9]), reads=[smallB], writes=[smallB])
                    sy.op("dve", lambda: DVE.tensor_tensor(out=small[:, 10:11], in0=small[:, 9:10], in1=GATE[:, i, h * 3:h * 3 + 1], op=ALU.mult),
                          reads=[smallB, GATEB], writes=[smallB])
                    sy.op("dve", lambda: DVE.tensor_scalar(out=onsa[:, h * 64:(h + 1) * 64], in0=Ot[:, 0:64], scalar1=small[:, 10:11],
                                                           scalar2=None, op0=ALU.mult), reads=[OB, smallB], writes=[onsaB])
                    if h % 4 == 0:
                        sy.op("dve", lambda: DVE.tensor_scalar(out=score[:, gq, :], in0=Ot[:, 65:97], scalar1=small[:, 9:10],
                                                               scalar2=None, op0=ALU.mult), reads=[OB, smallB], writes=[scoreB])
                    else:
                        sy.op("dve", lambda: DVE.scalar_tensor_tensor(out=score[:, gq, :], in0=Ot[:, 65:97], scalar=small[:, 9:10],
                                                                      in1=score[:, gq, :], op0=ALU.mult, op1=ALU.add),
                              reads=[OB, smallB, scoreB], writes=[scoreB])
                jobs += make_jobs(items, Ot[:, 0:97], OB, SC_N, [VCMPB], [KCMPTB, QNOPEB, CB("negcmp"), identB], post=post)
            run_jobs(jobs)
            stop_if("acmp")
            for gq in range(2):
                sy.op("dve", lambda: DVE.scalar_tensor_tensor(out=score[:, gq, :], in0=C("forced")[:, qt * 32:(qt + 1) * 32], scalar=1e4,
                                                              in1=score[:, gq, :], op0=ALU.mult, op1=ALU.add),
                      reads=[scoreB, CB("forced")], writes=[scoreB])
                sy.op("dve", lambda: DVE.scalar_tensor_tensor(out=score[:, gq, :], in0=C("invalid")[:, qt * 32:(qt + 1) * 32], scalar=-1e30,
                                                              in1=score[:, gq, :], op0=ALU.mult, op1=ALU.add),
                      reads=[scoreB, CB("invalid")], writes=[scoreB])
                sy.op("dve", lambda: DVE.max(out=m8[:, gq * 8:(gq + 1) * 8], in_=score[:, gq, :]), reads=[scoreB], writes=[m8B])
                sy.op("dve", lambda: DVE.tensor_scalar(out=negsel[:, gq, :], in0=score[:, gq, :], scalar1=m8[:, gq * 8 + 5:gq * 8 + 6],
                                                       scalar2=NEGM, op0=ALU.is_lt, op1=ALU.mult), reads=[scoreB, m8B], writes=[negselB])

        def diff_part(i):
            qt = c * 4 + i
            qs = slice(i * 128, (i + 1) * 128)
            onsa, onsaB = onsa2[i % 2]
            score, scoreB = score2[i % 2]
            negsel, negselB = negsel2[i % 2]
            NEGSELT, NEGSELTB = NEGSELT2[i % 2]
            attn, attnB = attn2[i % 2]
            stop_if("aslc")
            jobs = []
            for h in range(8):
                j = h // 2
                Ot, OB = rotO.next()
                for m in range(2):
                    base = (h % 2) * 64 + m * 32
                    tp = (96, 0) if base == 96 else None
                    items = []
                    for kt in range(qt + 1):
                        masks = [1] if kt == qt else []
                        items.append((KD[base:base + 32, j, kt * 128:(kt + 1) * 128], QD[base:base + 32, j, qs], masks, VD[:, kt, h, :], tp))

                    def post(h=h, Ot=Ot, OB=OB):
                        sy.op("dve", lambda: DVE.reciprocal(out=small[:, 13:14], in_=Ot[:, 64:65]), reads=[OB], writes=[smallB])
                        sy.op("dve", lambda: DVE.reciprocal(out=small[:, 14:15], in_=Ot[:, 129:130]), reads=[OB], writes=[smallB])
                        sy.op("dve", lambda: DVE.tensor_tensor(out=small[:, 15:16], in0=small[:, 14:15], in1=neglam[:, 0:1], op=ALU.mult),
                              reads=[smallB, neglamB], writes=[smallB])
                        sy.op("dve", lambda: DVE.tensor_scalar(out=tmpo[:, 0:64], in0=Ot[:, 0:64], scalar1=small[:, 13:14], scalar2=None,
                                                               op0=ALU.mult), reads=[OB, smallB], writes=[tmpoB])
                        sy.op("dve", lambda: DVE.scalar_tensor_tensor(out=odif[:, h * 64:(h + 1) * 64], in0=Ot[:, 65:129], scalar=small[:, 15:16],
                                                                      in1=tmpo[:, 0:64], op0=ALU.mult, op1=ALU.add),
                              reads=[OB, smallB, tmpoB], writes=[odifB])
                    jobs += make_jobs(items, Ot[:, m * 65:(m + 1) * 65], OB, SC_D, [VDB], [KDB, QDB],
                                      post=(post if m == 1 else None), postmask=True)
            run_jobs(jobs)
            stop_if("adif")
            sy.op("act", lambda: ACT.activation(out=tmpo[:, :], in_=odif[:, :], func=AF.Square), reads=[odifB], writes=[tmpoB])
            sy.op("dve", lambda: DVE.tensor_reduce(out=small[:, 16:24], in_=tmpo[:, :].rearrange("p (h d) -> p h d", h=8), axis=AX.X, op=ALU.add),
                  reads=[tmpoB], writes=[smallB])
            sy.op("act", lambda: ACT.activation(out=small[:, 24:32], in_=small[:, 16:24], func=AF.Ln, bias=epsT[:, 0:1], scale=1.0 / 64),
                  reads=[smallB, epsB], writes=[smallB])
            sy.op("act", lambda: ACT.activation(out=small[:, 32:40], in_=small[:, 24:32], func=AF.Exp, scale=-0.5), reads=[smallB], writes=[smallB])
            for h in range(8):
                sy.op("dve", lambda h=h: DVE.scalar_tensor_tensor(out=attn[:, 512 + h * 64:512 + (h + 1) * 64], in0=odif[:, h * 64:(h + 1) * 64],
                                                                  scalar=small[:, 32 + h:33 + h], in1=goS[:], op0=ALU.mult, op1=ALU.mult),
                      reads=[odifB, smallB, goSB], writes=[attnB])

        def tr_part(i):
            qt = c * 4 + i
            qs = slice(i * 128, (i + 1) * 128)
            onsa, onsaB = onsa2[i % 2]
            score, scoreB = score2[i % 2]
            negsel, negselB = negsel2[i % 2]
            NEGSELT, NEGSELTB = NEGSELT2[i % 2]
            attn, attnB = attn2[i % 2]
            for gq in range(2):
                p2, p2B = rotP.next()
                p2b = p2[:].bitcast(BF16)
                sy.op("pe", lambda: PE.transpose(p2b[0:32, 0:128], negsel[:, gq, :], ident[:]), reads=[negselB, identB], writes=[p2B])
                sy.op("dve", lambda: DVE.tensor_copy(out=NEGSELT[0:32, gq, :], in_=p2b[0:32, 0:128]), reads=[p2B], writes=[NEGSELTB])

        def slcwin_part(i):
            qt = c * 4 + i
            qs = slice(i * 128, (i + 1) * 128)
            onsa, onsaB = onsa2[i % 2]
            score, scoreB = score2[i % 2]
            negsel, negselB = negsel2[i % 2]
            NEGSELT, NEGSELTB = NEGSELT2[i % 2]
            attn, attnB = attn2[i % 2]
            stop_if("asel")
            jobs = []
            for h in range(8):
                hp, j, gq = (h % 2) * 64, h // 2, h // 4
                Ot, OB = rotO.next()
                items = []
                for kt in range(qt + 1):
                    masks = [(esel[:, kt * 128:(kt + 1) * 128], NEGSELT[:, gq, :])]
                    if kt == qt:
                        masks += id_masks(negc[:, :])
                    items.append((KS[:, gq, kt * 128:(kt + 1) * 128], QROPE[:, h, qs], masks, VS[:, kt, gq, :], None))
                jobs += make_jobs(items, Ot[:, 0:65], OB, SC_N, [VSB], [KSB, QROPEB, NEGSELTB, CB("esel"), CB("negc"), identB])
                items = []
                for kt in range(max(0, qt - 2), qt + 1):
                    masks = []
                    if kt == qt:
                        masks = id_masks(negc[:, :])
                    elif kt == qt - 2:
                        masks = id_masks(negw[:, :])
                    items.append((KW[:, gq, kt * 128:(kt + 1) * 128], QROPE[:, h, qs], masks, VW[:, kt, gq, :], None))

                def post(h=h, Ot=Ot, OB=OB):
                    for br, c0 in ((1, 0), (2, 65)):
                        sy.op("dve", lambda: DVE.reciprocal(out=small[:, 11:12], in_=Ot[:, c0 + 64:c0 + 65]), reads=[OB], writes=[smallB])
                        sy.op("dve", lambda: DVE.tensor_tensor(out=small[:, 12:13], in0=small[:, 11:12], in1=GATE[:, i, h * 3 + br:h * 3 + br + 1],
                                                               op=ALU.mult), reads=[smallB, GATEB], writes=[smallB])
                        sy.op("dve", lambda: DVE.scalar_tensor_tensor(out=onsa[:, h * 64:(h + 1) * 64], in0=Ot[:, c0:c0 + 64], scalar=small[:, 12:13],
                                                                      in1=onsa[:, h * 64:(h + 1) * 64], op0=ALU.mult, op1=ALU.add),
                              reads=[OB, smallB, onsaB], writes=[onsaB])
                jobs += make_jobs(items, Ot[:, 65:130], OB, SC_N, [VWB], [KWB, QROPEB, CB("negc"), CB("negw"), identB], post=post)
            run_jobs(jobs)
            sy.op("act", lambda: ACT.activation(out=attn[:, 0:512], in_=onsa[:, :], func=AF.Copy), reads=[onsaB], writes=[attnB])

        def final_part(i):
            qt = c * 4 + i
            qs = slice(i * 128, (i + 1) * 128)
            onsa, onsaB = onsa2[i % 2]
            score, scoreB = score2[i % 2]
            negsel, negselB = negsel2[i % 2]
            NEGSELT, NEGSELTB = NEGSELT2[i % 2]
            attn, attnB = attn2[i % 2]
            p2, p2B = rotP.next()
            p2b = p2[:].bitcast(BF16)
            sy.group("pe", [(lambda k=k: PE.transpose(p2b[:, k * 128:(k + 1) * 128], attn[:, k * 128:(k + 1) * 128], ident[:])) for k in range(8)],
                     reads=[attnB, identB], writes=[p2B])
            sy.op("act", lambda: ACT.activation(out=attnT[:, :, :], in_=p2b[:, :].rearrange("p (k t) -> p k t", k=8), func=AF.Copy),
                  reads=[p2B], writes=[attnTB])
            xt, xB = stg[xi[0] % 2], stgB[xi[0] % 2]; xi[0] += 1
            sy.dma("sp", xt[:], x_d[b, qt * 128:(qt + 1) * 128, :], xB, writes=[xB])
            for half in range(2):
                p3, p3B = rotP.next()
                sy.group("pe", [(lambda k=k: PE.matmul(p3[:, :], lhsT=attnT[:, k, :], rhs=C("wout")[:, k, half * 512:(half + 1) * 512],
                                                       start=(k == 0), stop=(k == 7))) for k in range(8)],
                         reads=[attnTB, CB("wout")], writes=[p3B])
                sy.op("dve", lambda: DVE.tensor_tensor(out=tmpo[:, :], in0=p3[:, :], in1=G1[:, half * 512:(half + 1) * 512], op=ALU.mult),
                      reads=[p3B, G1B], writes=[tmpoB])
                sy.op("dve", lambda: DVE.tensor_tensor(out=xt[:, half * 512:(half + 1) * 512], in0=xt[:, half * 512:(half + 1) * 512],
                                                       in1=tmpo[:, :], op=ALU.add), reads=[tmpoB, xB], writes=[xB])
            sy.dma("sp", x1_dst(b, qt), xt[:], xB, reads=[xB])
            stop_if("aout")
            nb_, nc_ = (b, c + 1) if c + 1 < NCH else (b + 1, 0)
            emit_norm_tile(nb_, nc_, i)

        cmp_part(0)
        tr_part(0)
        for i in range(4):
            diff_part(i)
            if i > 0:
                final_part(i - 1)
            if i + 1 < 4:
                cmp_part(i + 1)
            slcwin_part(i)
            if i + 1 < 4:
                tr_part(i + 1)
        final_part(3)

    for b in range(nseq):
        bcast_row(G1, G1B, 16, b)
        for c in range(NCH):
            t0 = c * TCH
            for i_, tab in enumerate([cn_d, sn_d, cd_d, sd_d]):
                sy.dma("sp", ropeT[:, i_, :], tab[:, t0:t0 + TCH], ropeTB, writes=[ropeTB], add=(i_ > 0))
            for i in range(4):
                emit_norm_tile(b, c, i)
            stop_if("norm")
            pend = []
            for grp in range(5):
                if grp == 3:
                    compress_A(c)
                wt, wB = wload(win_v[:, :, grp * 512:(grp + 1) * 512], 512)
                for ci in range(4):
                    name = FM_CHUNKS[grp * 4 + ci]
                    pt, pB = rotPP.next()
                    sy.group("pe", [(lambda k=k: PE.matmul(pt[:, :], lhsT=wt[:, k, ci * 128:(ci + 1) * 128], rhs=hT[:, k, :],
                                                           start=(k == 0), stop=(k == 7))) for k in range(8)],
                             reads=[wB, hTB], writes=[pB])
                    kind, j = name[:2], int(name[2])
                    gen_ = None
                    if kind == "qn":
                        gen_ = normrope(pt, pB, "blk64", "perm64", 0, ropeT[:, 0, :], ropeT[:, 1, :],
                                 [(QNOPE[0:64, 2 * j, :], 0, 64), (QNOPE[64:128, 2 * j + 1, :], 64, 128)], [QNOPEB],
                                 [(QROPE[0:64, 2 * j, :], 0, 64), (QROPE[64:128, 2 * j + 1, :], 64, 128)], [QROPEB])
                    elif kind == "ks":
                        gen_ = normrope(pt, pB, "blk64", "perm64", 1, ropeT[:, 0, :], ropeT[:, 1, :],
                                 None, None, [(KS[:, j, t0:t0 + TCH], 0, 128)], [KSB])
                    elif kind == "kw":
                        gen_ = normrope(pt, pB, "blk64", "perm64", 2, ropeT[:, 0, :], ropeT[:, 1, :],
                                 None, None, [(KW[:, j, t0:t0 + TCH], 0, 128)], [KWB])
                    elif kind == "qd":
                        gen_ = normrope(pt, pB, "blk32", "perm32", 3, ropeT[:, 2, :], ropeT[:, 3, :],
                                 None, None, [(QD[:, j, :], 0, 128)], [QDB])
                    elif kind == "kd":
                        gen_ = normrope(pt, pB, "blk32", "perm32", 4, ropeT[:, 2, :], ropeT[:, 3, :],
                                 None, None, [(KD[:, j, t0:t0 + TCH], 0, 128)], [KDB])
                    else:
                        dst, dB = (KC2, KC2B) if kind == "kc" else (VC2, VC2B)
                        sy.op("act", lambda: ACT.activation(out=dst[0:64, j, 32:32 + TCH], in_=pt[0:64, :], func=AF.Copy),
                              reads=[pB], writes=[dB])
                        sy.op("dve", lambda: DVE.tensor_copy(out=dst[64:128, j, 31:31 + TCH], in_=pt[64:128, :]),
                              reads=[pB], writes=[dB])
                    for g_ in list(pend):
                        try:
                            next(g_)
                        except StopIteration:
                            pend.remove(g_)
                    if gen_ is not None:
                        pend.append(gen_)
            while pend:
                for g_ in list(pend):
                    try:
                        next(g_)
                    except StopIteration:
                        pend.remove(g_)
            stop_if("projfm")
            wtA, wBA = wload(win_v[:, :, NFM * 128:NFM * 128 + TM_A], TM_A)
            for i in range(4):
                kt = c * 4 + i
                pt, pB = rotPP.next()
                sy.group("pe", [(lambda k=k: PE.matmul(pt[:, 0:TM_A], lhsT=hT[:, k, i * 128:(i + 1) * 128], rhs=wtA[:, k, 0:TM_A],
                                                       start=(k == 0), stop=(k == 7))) for k in range(8)],
                         reads=[wBA, hTB], writes=[pB])
                stop_if("tmb%d_0" % i)
                sy.op("dve", lambda: DVE.tensor_copy(out=VS[:, kt, :, 0:64], in_=pt[:, 0:128].rearrange("p (g d) -> p g d", g=2)),
                      reads=[pB], writes=[VSB])
                stop_if("tmb%d_1" % i)
                sy.op("dve", lambda: DVE.tensor_copy(out=VW[:, kt, :, 0:64], in_=pt[:, 128:256].rearrange("p (g d) -> p g d", g=2)),
                      reads=[pB], writes=[VWB])
                if stage == "dbgA" and i == 1:
                    sy.op("dve", lambda: DVE.tensor_copy(out=stg[0][:, 0:280], in_=pt[:, 0:280]), reads=[pB], writes=[stgB[0]])
                    sy.dma("sp", out_d[0, 0:128, 0:280], stg[0][:, 0:280], stgB[0], reads=[stgB[0]])
                    sy.op("dve", lambda: DVE.tensor_copy(out=stg[1][:, 0:512], in_=hT[:, 0, :]), reads=[hTB], writes=[stgB[1]])
                    sy.dma("sp", out_d[0, 128:256, 0:512], stg[1][:, 0:512], stgB[1], reads=[stgB[1]])
                    raise _Stop()
                stop_if("tmb%d_2" % i)
                sy.op("act", lambda: ACT.activation(out=small[:, 8:32], in_=pt[:, 256:280], func=AF.Exp, scale=-1.0),
                      reads=[pB], writes=[smallB])
                stop_if("tmb%d_3" % i)
                sy.op("dve", lambda: DVE.tensor_scalar(out=small[:, 32:56], in0=small[:, 8:32], scalar1=1.0, scalar2=None, op0=ALU.add),
                      reads=[smallB], writes=[smallB])
                sy.op("dve", lambda: DVE.reciprocal(out=GATE[:, i, :], in_=small[:, 32:56]), reads=[smallB], writes=[GATEB])
                stop_if("tmb%d_4" % i)
            stop_if("tma")
            wtB, wBB = wload(win_v[:, :, NFM * 128 + TM_A:NCOLS], TM_B)
            for i in range(4):
                kt = c * 4 + i
                pt, pB = rotPP.next()
                sy.group("pe", [(lambda k=k: PE.matmul(pt[:, :], lhsT=hT[:, k, i * 128:(i + 1) * 128], rhs=wtB[:, k, :],
                                                       start=(k == 0), stop=(k == 7))) for k in range(8)],
                         reads=[wBB, hTB], writes=[pB])
                sy.op("act", lambda: ACT.activation(out=VD[:, kt, :, 0:64], in_=pt[:, :].rearrange("p (h d) -> p h d", h=8),
                                                    func=AF.Copy), reads=[pB], writes=[VDB])
            stop_if("proj")
            attention_chunk(b, c)
    if stage == "attn":
        sy.finish()
        return nc, sy, locals()

    sy.barrier()
    arena.reset()
    NTT = nseq * NT
    NROWS = NEXP * CAP + 128
    TRASH = NEXP * CAP
    xbuf_d, ybuf_d, xbufB = g["xbuf_d"], g["ybuf_d"], g["xbufB"]
    wr_d, br_d, wg_d, wu_d, wd_d = g["wr_d"], g["br_d"], g["wg_d"], g["wu_d"], g["wd_d"]
    mats_d = g["mats_d"]
    ecap_d = g["ecap_d"]

    GS2b = A([128, D], F32); GS2bB = Buf("GS2b")
    SH2b = A([128, D], F32); SH2bB = Buf("SH2b")
    G2b = A([128, D], F32); G2bB = Buf("G2b")
    wr = A([128, 8, 36], F32); wrB = Buf("wr")
    brb = A([128, 36], F32); brbB = Buf("brb")
    ecap = A([128, 32], F32); ecapB = Buf("ecap")
    ltri = A([128, 128], BF16); ltriB = Buf("ltri")
    ones_bf = A([128, 128], BF16); onesB = Buf("ones")
    base_bc = A([128, 32], F32); baseB = Buf("base")
    SLOT = A([128, NTT, 2], I32)
    WGT = A([128, NTT, 2], F32)
    arena_mark = arena.off
    m1set = []
    m1x = [(stg[0], stgB[0]), (stg[1], stgB[1])] + [(A([128, D], F32), Buf("m1x%d" % i_)) for i_ in range(2)]
    for si_ in range(4):
        m1set.append((A([128, 512], F32), Buf("rs%d" % si_), A([128, D], F32), Buf("xn2_%d" % si_), A([128, D], F32), Buf("tmpf%d" % si_),
                      A([128, D], BF16), Buf("h2tm%d" % si_), A([128, 8, 128], F32), Buf("h2T%d" % si_), A([128, 32], BF16), Buf("Abf%d" % si_)))
    sy.dma("sp", wr, wr_d.rearrange("(k p) n -> p k n", p=128), wrB, writes=[wrB])
    sy.dma("pool", brb, br_d[0:1, :].partition_broadcast(128), brbB, writes=[brbB])
    sy.dma("sp", ecap, ecap_d[:, :], ecapB, writes=[ecapB])
    sy.dma("pool", ltri, mats_d["ltri"][:, :], ltriB, writes=[ltriB])
    sy.op("dve", lambda: DVE.memset(ones_bf, 1.0), writes=[onesB])
    sy.op("dve", lambda: DVE.memset(base_bc, 0.0), writes=[baseB])
    SLOTBs = [Buf("slot%d" % i) for i in range(NTT)]
    WGTBs = [Buf("wgt%d" % i) for i in range(NTT)]

    def m1_tile(b, ti, si):
        tt = b * NT + ti
        Sx = m1set[si]
        rs, rsB, xn2, xn2B, tmpf, tmpfB, h2tm, h2tmB, h2T, h2TB, Abf, AbfB = Sx

        def col(i_):
            return rs[:, i_:i_ + 1]
        xt, xB = m1x[si]
        sy.dma("sp", xt[:], x1_d[b, ti * 128:(ti + 1) * 128, :], xB, writes=[xB])
        yield
        sy.op("act", lambda: ACT.activation(out=tmpf[:, 0:512], in_=xt[:, 0:512], func=AF.Square, accum_out=col(0)),
              reads=[xB], writes=[tmpfB, rsB])
        yield
        sy.op("act", lambda: ACT.activation(out=tmpf[:, 512:1024], in_=xt[:, 512:1024], func=AF.Square, accum_out=col(1)),
              reads=[xB], writes=[tmpfB, rsB])
        yield
        sy.op("dve", lambda: DVE.tensor_tensor(out=col(2), in0=col(0), in1=col(1), op=ALU.add), reads=[rsB], writes=[rsB])
        yield
        sy.op("act", lambda: ACT.activation(out=col(3), in_=col(2), func=AF.Ln, bias=epsT[:, 0:1], scale=1.0 / D),
              reads=[rsB, epsB], writes=[rsB])
        yield
        sy.op("act", lambda: ACT.activation(out=col(4), in_=col(3), func=AF.Exp, scale=-0.5), reads=[rsB], writes=[rsB])
        yield
        sy.op("act", lambda: ACT.activation(out=xn2, in_=xt[:], func=AF.Identity, scale=col(4)),
              reads=[xB, rsB], writes=[xn2B])
        yield
        sy.op("pool", lambda: POOL.tensor_tensor(out=tmpf, in0=xn2, in1=GS2b, op=ALU.mult), reads=[xn2B, GS2bB], writes=[tmpfB])
        yield
        sy.op("pool", lambda: POOL.tensor_tensor(out=h2tm, in0=tmpf, in1=SH2b, op=ALU.add), reads=[tmpfB, SH2bB], writes=[h2tmB])
        yield
        for hf in range(2):
            p2, p2B = rotPP.next()
            sy.group("pe", [(lambda k=k: PE.transpose(p2[:, (k % 4) * 128:(k % 4 + 1) * 128], xn2[:, k * 128:(k + 1) * 128],
                                                      C("identf")[:])) for k in range(hf * 4, hf * 4 + 4)],
                     reads=[xn2B, CB("identf")], writes=[p2B])
            yield
            for k in range(hf * 4, hf * 4 + 4):
                sy.op("act", lambda k=k: ACT.activation(out=h2T[:, k, :], in_=p2[:, (k % 4) * 128:(k % 4 + 1) * 128], func=AF.Identity,
                                                        scale=gs2[:, k, b:b + 1], bias=modT[:, 24 + k, b:b + 1]),
                      reads=[p2B, gs2B, modTB], writes=[h2TB])
                yield
        p3, p3B = rotPP.next()
        sy.group("pe", [(lambda k=k: PE.matmul(p3[:, 0:36], lhsT=h2T[:, k, :], rhs=wr[:, k, :], start=(k == 0), stop=(k == 7)))
                        for k in range(8)], reads=[h2TB, wrB], writes=[p3B])
        yield
        LG = rs[:, 8:44]
        sy.op("dve", lambda: DVE.tensor_tensor(out=LG, in0=p3[:, 0:36], in1=brb, op=ALU.add), reads=[p3B, brbB], writes=[rsB])
        yield
        R = lambda *a, **k_: sy.op("dve", *a, reads=[rsB], writes=[rsB], **k_)
        R(lambda: DVE.tensor_reduce(out=col(5), in_=rs[:, 8:12], axis=AX.X, op=ALU.max))
        yield
        R(lambda: DVE.tensor_scalar(out=col(6), in0=col(5), scalar1=-1.0, scalar2=None, op0=ALU.mult))
        yield
        sy.op("act", lambda: ACT.activation(out=rs[:, 48:52], in_=rs[:, 8:12], func=AF.Exp, bias=col(6), accum_out=col(7)),
              reads=[rsB], writes=[rsB])
        yield
        R(lambda: DVE.reciprocal(out=col(52), in_=col(7)))
        yield
        R(lambda: DVE.tensor_scalar(out=rs[:, 56:60], in0=rs[:, 8:12], scalar1=col(5), scalar2=None, op0=ALU.is_equal))
        yield
        R(lambda: DVE.tensor_scalar(out=rs[:, 64:72], in0=rs[:, 12:20], scalar1=col(56), scalar2=None, op0=ALU.mult))
        yield
        for gi in range(1, 4):
            R(lambda gi=gi: DVE.scalar_tensor_tensor(out=rs[:, 64:72], in0=rs[:, 12 + 8 * gi:20 + 8 * gi], scalar=col(56 + gi),
                                                     in1=rs[:, 64:72], op0=ALU.mult, op1=ALU.add))
            yield
        R(lambda: DVE.max(out=rs[:, 72:80], in_=rs[:, 64:72]))
        yield
        R(lambda: DVE.tensor_tensor(out=col(80), in0=col(73), in1=col(72), op=ALU.subtract))
        yield
        sy.op("act", lambda: ACT.activation(out=col(81), in_=col(80), func=AF.Exp), reads=[rsB], writes=[rsB])
        yield
        R(lambda: DVE.tensor_scalar(out=col(82), in0=col(81), scalar1=1.0, scalar2=None, op0=ALU.add))
        yield
        R(lambda: DVE.reciprocal(out=col(83), in_=col(82)))
        yield
        R(lambda: DVE.tensor_tensor(out=col(84), in0=col(83), in1=col(52), op=ALU.mult))
        yield
        R(lambda: DVE.tensor_tensor(out=col(85), in0=col(52), in1=col(84), op=ALU.subtract))
        yield
        R(lambda: DVE.tensor_scalar(out=rs[:, 88:96], in0=rs[:, 64:72], scalar1=col(72), scalar2=None, op0=ALU.is_equal))
        yield
        R(lambda: DVE.tensor_scalar(out=rs[:, 96:104], in0=rs[:, 64:72], scalar1=col(73), scalar2=None, op0=ALU.is_equal))
        yield
        for gi in range(4):
            R(lambda gi=gi: DVE.tensor_scalar(out=rs[:, 128 + 8 * gi:136 + 8 * gi], in0=rs[:, 88:96], scalar1=col(56 + gi),
                                              scalar2=None, op0=ALU.mult))
            yield
            R(lambda gi=gi: DVE.tensor_scalar(out=rs[:, 160 + 8 * gi:168 + 8 * gi], in0=rs[:, 96:104], scalar1=col(56 + gi),
                                              scalar2=None, op0=ALU.mult))
            yield
        sy.op("dve", lambda: DVE.tensor_tensor(out=Abf, in0=rs[:, 128:160], in1=rs[:, 160:192], op=ALU.add), reads=[rsB], writes=[AbfB])
        yield
        p4, p4B = rotPP.next()
        sy.group("pe", [lambda: PE.matmul(p4[:, 0:32], lhsT=ltri, rhs=Abf, start=True, stop=True),
                        lambda: PE.matmul(p4[:, 32:64], lhsT=ones_bf, rhs=Abf, start=True, stop=True)],
                 reads=[AbfB, ltriB, onesB], writes=[p4B])
        yield
        sy.op("dve", lambda: DVE.tensor_tensor(out=rs[:, 192:224], in0=p4[:, 0:32], in1=base_bc, op=ALU.add),
              reads=[p4B, baseB, rsB], writes=[rsB])
        sy.op("dve", lambda: DVE.tensor_tensor(out=base_bc, in0=p4[:, 32:64], in1=base_bc, op=ALU.add), reads=[p4B, baseB], writes=[baseB])
        yield
        for kx, s0 in ((0, 128), (1, 160)):
            R(lambda s0=s0: DVE.tensor_tensor(out=rs[:, 224:256], in0=rs[:, s0:s0 + 32], in1=rs[:, 192:224], op=ALU.mult))
            yield
            R(lambda kx=kx: DVE.tensor_reduce(out=col(256 + kx), in_=rs[:, 224:256], axis=AX.X, op=ALU.add))
            yield
            sy.op("dve", lambda s0=s0: DVE.tensor_tensor(out=rs[:, 224:256], in0=rs[:, s0:s0 + 32], in1=ecap, op=ALU.mult),
                  reads=[rsB, ecapB], writes=[rsB])
            yield
            R(lambda kx=kx: DVE.tensor_reduce(out=col(258 + kx), in_=rs[:, 224:256], axis=AX.X, op=ALU.add))
            yield
            R(lambda kx=kx: DVE.tensor_scalar(out=col(260 + kx), in0=col(256 + kx), scalar1=float(CAP), scalar2=None, op0=ALU.is_lt))
            yield
            R(lambda kx=kx: DVE.tensor_tensor(out=col(262 + kx), in0=col(256 + kx), in1=col(258 + kx), op=ALU.add))
            yield
            R(lambda kx=kx: DVE.tensor_scalar(out=col(262 + kx), in0=col(262 + kx), scalar1=-float(TRASH), scalar2=None, op0=ALU.add))
            yield
            R(lambda kx=kx: DVE.tensor_tensor(out=col(262 + kx), in0=col(262 + kx), in1=col(260 + kx), op=ALU.mult))
            yield
            R(lambda kx=kx: DVE.tensor_scalar(out=col(262 + kx), in0=col(262 + kx), scalar1=float(TRASH), scalar2=None, op0=ALU.add))
            yield
            sy.op("dve", lambda kx=kx: DVE.tensor_copy(out=SLOT[:, tt, kx:kx + 1], in_=col(262 + kx)), reads=[rsB], writes=[SLOTBs[tt]])
            yield
            sy.op("dve", lambda kx=kx: DVE.tensor_tensor(out=WGT[:, tt, kx:kx + 1], in0=col(84 + kx), in1=col(260 + kx), op=ALU.mult),
                  reads=[rsB], writes=[WGTBs[tt]])
            yield
        for kx in range(2):
            sy.dma_fn("pool", lambda kx=kx: POOL.indirect_dma_start(
                out=xbuf_d[:, :], out_offset=bass.IndirectOffsetOnAxis(ap=SLOT[:, tt, kx:kx + 1].bitcast(U32), axis=0),
                in_=h2tm, in_offset=None), h2tmB, reads=[h2tmB, SLOTBs[tt], xbufB])
            yield

    for b in range(nseq):
        bcast_rows_from(gs2, gs2B, GS2b, GS2bB, b)
        bcast_row(SH2b, SH2bB, 24, b)
        for ti in range(0, NT, 4):
            gens = [m1_tile(b, ti + q_, q_) for q_ in range(4)]
            alive = [True] * 4
            while any(alive):
                for gi_ in range(4):
                    if alive[gi_]:
                        try:
                            next(gens[gi_])
                        except StopIteration:
                            alive[gi_] = False
    if stage == "m1":
        sy.finish()
        return nc, sy, locals()

    sy.barrier()
    arena.off = arena_mark
    WEXP = [(A([128, 8, 512], BF16), A([128, 8, 512], BF16), A([128, 4, D], BF16)) for _ in range(2)]
    WEXPB = [(Buf("wg%d" % i), Buf("wu%d" % i), Buf("wd%d" % i)) for i in range(2)]
    XT = A([128, 8, CAP], BF16); XTB = Buf("XT")
    HT = A([128, 4, CAP], BF16); HTB = Buf("HT")
    xrow = [A([128, D], BF16) for _ in range(3)]; xrowB = [Buf("xrow%d" % i) for i in range(3)]
    yst = [A([128, D], BF16) for _ in range(2)]; ystB = [Buf("yst%d" % i) for i in range(2)]
    sgb = [A([128, 384], F32) for _ in range(3)]; sgbB = [Buf("sg%d" % i) for i in range(3)]
    tmpf = A([128, D], F32); tmpfB = Buf("tmpf3")
    Yg = [A([128, D], BF16) for _ in range(4)]; YgB = [Buf("yg%d" % i) for i in range(4)]
    NST = CAP // 128
    NCC = 3
    rotGU = Rot([0, 1, 2, 3, 4, 7])
    CW = CAP // NCC
    xr = [0]

    def load_expert(e):
        i_ = e % 2
        (wg, wu, wd_), (wgB, wuB, wdB) = WEXP[i_], WEXPB[i_]
        sy.dma("pool", wg, wg_d[e].rearrange("(k p) n -> p k n", p=128), wgB, writes=[wgB])
        sy.dma("pool", wu, wu_d[e].rearrange("(k p) n -> p k n", p=128), wuB, writes=[wuB])
        sy.dma("pool", wd_, wd_d[e].rearrange("(k p) n -> p k n", p=128), wdB, writes=[wdB])

    def x_part(e):
        for s_ in range(NST):
            xw, xwB = xrow[xr[0] % 3], xrowB[xr[0] % 3]; xr[0] += 1
            r0 = e * CAP + s_ * 128
            sy.dma("sp", xw, xbuf_d[r0:r0 + 128, :], xwB, writes=[xwB])
            p2, p2B = rotP.next()
            p2b = p2[:].bitcast(BF16)
            sy.group("pe", [(lambda k=k: PE.transpose(p2b[:, k * 128:(k + 1) * 128], xw[:, k * 128:(k + 1) * 128], ident[:]))
                            for k in range(8)], reads=[xwB, identB], writes=[p2B])
            sy.op("act" if s_ % 2 == 0 else "dve",
                  (lambda: ACT.activation(out=XT[:, :, s_ * 128:(s_ + 1) * 128], in_=p2b[:, :].rearrange("p (k t) -> p k t", k=8), func=AF.Copy))
                  if s_ % 2 == 0 else
                  (lambda: DVE.tensor_copy(out=XT[:, :, s_ * 128:(s_ + 1) * 128], in_=p2b[:, :].rearrange("p (k t) -> p k t", k=8))),
                  reads=[p2B], writes=[XTB])

    def gu_part(e):
        (wg, wu, wd_), (wgB, wuB, wdB) = WEXP[e % 2], WEXPB[e % 2]
        for m in range(4):
            for cc in range(NCC):
                cs = slice(cc * CW, (cc + 1) * CW)
                pg, pgB = rotGU.next()
                sy.group("pe", [(lambda k=k: PE.matmul(pg[:, 0:CW], lhsT=wg[:, k, m * 128:(m + 1) * 128], rhs=XT[:, k, cs],
                                                       start=(k == 0), stop=(k == 7))) for k in range(8)], reads=[wgB, XTB], writes=[pgB])
                pu, puB = rotGU.next()
                sy.group("pe", [(lambda k=k: PE.matmul(pu[:, 0:CW], lhsT=wu[:, k, m * 128:(m + 1) * 128], rhs=XT[:, k, cs],
                                                       start=(k == 0), stop=(k == 7))) for k in range(8)], reads=[wuB, XTB], writes=[puB])
                sg, sgB = sgb[(m * NCC + cc) % 3], sgbB[(m * NCC + cc) % 3]
                sy.op("act", lambda: ACT.activation(out=sg[:, 0:CW], in_=pg[:, 0:CW], func=AF.Silu), reads=[pgB], writes=[sgB])
                sy.op("dve", lambda: DVE.tensor_tensor(out=HT[:, m, cs], in0=pu[:, 0:CW], in1=sg[:, 0:CW], op=ALU.mult),
                      reads=[puB, sgB], writes=[HTB])

    def dn_part(e):
        (wg, wu, wd_), (wgB, wuB, wdB) = WEXP[e % 2], WEXPB[e % 2]
        for s_ in range(NST):
            ys, ysB = yst[s_ % 2], ystB[s_ % 2]
            for half in range(2):
                po, poB = rotO.next()
                sy.group("pe", [(lambda m=m: PE.matmul(po[:, :], lhsT=HT[:, m, s_ * 128:(s_ + 1) * 128], rhs=wd_[:, m, half * 512:(half + 1) * 512],
                                                       start=(m == 0), stop=(m == 3))) for m in range(4)], reads=[HTB, wdB], writes=[poB])
                if half == 0:
                    sy.op("act", lambda: ACT.activation(out=ys[:, 0:512], in_=po[:, :], func=AF.Copy), reads=[poB], writes=[ysB])
                else:
                    sy.op("dve", lambda: DVE.tensor_copy(out=ys[:, 512:1024], in_=po[:, :]), reads=[poB], writes=[ysB])
            r0 = e * CAP + s_ * 128
            sy.dma("sp", ybuf_d[r0:r0 + 128, :], ys, ysB, reads=[ysB])

    load_expert(0)
    x_part(0)
    for e in range(NEXP):
        if e + 1 < NEXP:
            load_expert(e + 1)
        gu_part(e)
        if e + 1 < NEXP:
            x_part(e + 1)
        dn_part(e)
    if stage == "m2":
        sy.finish()
        return nc, sy, locals()

    sy.barrier()
    yi = [0]
    for b in range(nseq):
        bcast_row(G2b, G2bB, 40, b)
        for ti in range(NT):
            tt = b * NT + ti
            xt, xB = stg[xi[0] % 2], stgB[xi[0] % 2]; xi[0] += 1
            sy.dma("sp", xt[:], x1_d[b, ti * 128:(ti + 1) * 128, :], xB, writes=[xB])
            ys_ = []
            for kx in range(2):
                y_, yB_ = Yg[yi[0] % 4], YgB[yi[0] % 4]; yi[0] += 1
                sy.dma_fn("pool", lambda kx=kx, y_=y_: POOL.indirect_dma_start(
                    out=y_, out_offset=None, in_=ybuf_d[:, :],
                    in_offset=bass.IndirectOffsetOnAxis(ap=SLOT[:, tt, kx:kx + 1].bitcast(U32), axis=0)), yB_, reads=[SLOTBs[tt]], writes=[yB_])
                ys_.append((y_, yB_))
            sy.op("act", lambda: ACT.activation(out=tmpf, in_=ys_[0][0], func=AF.Identity, scale=WGT[:, tt, 0:1]),
                  reads=[ys_[0][1], WGTBs[tt]], writes=[tmpfB])
            sy.op("dve", lambda: DVE.scalar_tensor_tensor(out=tmpf, in0=ys_[1][0], scalar=WGT[:, tt, 1:2], in1=tmpf, op0=ALU.mult, op1=ALU.add),
                  reads=[ys_[1][1], WGTBs[tt], tmpfB], writes=[tmpfB])
            sy.op("dve", lambda: DVE.tensor_tensor(out=tmpf, in0=tmpf, in1=G2b, op=ALU.mult), reads=[tmpfB, G2bB], writes=[tmpfB])
            sy.op("dve", lambda: DVE.tensor_tensor(out=xt[:], in0=xt[:], in1=tmpf, op=ALU.add), reads=[tmpfB, xB], writes=[xB])
            sy.dma("sp", out_d[b, ti * 128:(ti + 1) * 128, :], xt[:], xB, reads=[xB])
    sy.finish()
    return nc, sy, locals()


def _prep_shared(inp):
    f = lambda a: np.ascontiguousarray(np.asarray(a, dtype=np.float32))
    hc = _host_consts()
    sh = {}
    sh["w_ada"] = f(inp["w_ada"][0])
    sh["b_ada_fm"] = _fm(f(inp["b_ada"][0]), 48)
    sh["b_ada_row"] = f(inp["b_ada"][0]).reshape(1, -1)
    sh["gmix_fm"] = _fm(f(inp["g_norm_mix"][0]), 8)
    sh["gffn_fm"] = _fm(f(inp["g_norm_ffn"][0]), 8)
    sh["w_inp"] = _win_cols(f(inp["w_in"][0]))
    gq = f(inp["g_nsa_q"][0]); gk = f(inp["g_nsa_k"][0])
    gdq = f(inp["g_diff_q"][0]); gdk = f(inp["g_diff_k"][0])
    sh["rowgain"] = np.ascontiguousarray(np.stack([np.tile(gq, 2), np.tile(gk[1], 2), np.tile(gk[2], 2),
                                                   np.tile(gdq, 4), np.tile(gdk, 4)], axis=1))
    sh["gk0_row"] = np.tile(gk[0], 2).reshape(1, 128)
    sh["go_row"] = f(inp["g_diff_out"][0]).reshape(1, 64)
    sh["lamv"] = np.concatenate([f(inp["lam_q1"][0]), f(inp["lam_k1"][0]), f(inp["lam_q2"][0]), f(inp["lam_k2"][0])]).reshape(1, 128)
    pe = f(inp["pe_cmp"][0])
    pefm = np.zeros((128, 32), np.float32)
    for kv in range(2):
        for j in range(16):
            pefm[0:64, kv * 16 + j] = pe[kv, 2 * j]
            pefm[64:128, kv * 16 + j] = pe[kv, 2 * j + 1]
    sh["pe_fm"] = pefm
    sh["w_cmp1"] = f(inp["w_cmp1"][0])
    w2 = f(inp["w_cmp2"][0])
    sh["w_cmp2k"] = np.ascontiguousarray(np.concatenate([w2[0], w2[0]], axis=1))
    sh["w_cmp2v"] = np.ascontiguousarray(w2[1])
    sh["w_out"] = f(inp["w_out"][0])
    for k in ("cn", "sn", "cd", "sd"):
        sh[k] = hc[k]
    for k in ("blk64", "blk32", "perm64", "perm32", "ident", "negc", "negw", "negcmp", "esel", "ov", "forced", "invalid", "ltri"):
        sh["m_" + k] = hc[k]
    sh["ecap"] = np.ascontiguousarray(np.tile((np.arange(32, dtype=np.float32) * CAP)[None, :], (128, 1)))
    sh["w_r"] = np.ascontiguousarray(np.concatenate([f(inp["w_router_group"][0]), f(inp["w_router_expert"][0])], axis=1))
    sh["b_r"] = np.concatenate([f(inp["b_router_group"][0]), f(inp["b_router_expert"][0])]).reshape(1, 36)
    sh["w_g"] = f(inp["w_exp_gate"][0])
    sh["w_u"] = f(inp["w_exp_up"][0])
    sh["w_d"] = f(inp["w_exp_down"][0])
    return sh


def _prep_core(inp, b0, nseq):
    x = np.ascontiguousarray(np.asarray(inp["x"][b0:b0 + nseq], dtype=np.float32))
    c = np.asarray(inp["c"][b0:b0 + nseq], dtype=np.float32)
    cT = np.ascontiguousarray(c.T.reshape(8, 128, nseq).transpose(1, 0, 2))
    return {"x": x, "cT": cT}


def run(inputs, nseq=4, ncores=8, stage="full", trace=False):
    nc, sy, L = build(nseq, stage=stage)
    used = set(L["used_inputs"])
    sh = {k: v for k, v in _prep_shared(inputs).items() if k in used}
    in_maps = []
    for ci in range(ncores):
        m = dict(sh)
        m.update(_prep_core(inputs, ci * nseq, nseq))
        in_maps.append(m)
    res = run_bass_kernel_spmd(nc, in_maps, core_ids=list(range(ncores)), trace=trace)
    outs = [r["out"] for r in res.results]
    return np.concatenate(outs, axis=0), res


def kernel(**inputs):
    out, _ = run(inputs, nseq=4, ncores=8, stage="full")
    return out.astype(np.float32)
```

```python
import math
import numpy as np
import ml_dtypes
import concourse.bass as bass
import concourse.mybir as mybir
from concourse.bass_utils import run_bass_kernel_spmd

F32 = mybir.dt.float32
BF16 = mybir.dt.bfloat16
I32 = mybir.dt.int32
U32 = mybir.dt.uint32
AF = mybir.ActivationFunctionType
ALU = mybir.AluOpType
AX = mybir.AxisListType

S = 2048
D = 1024
NT = S // 128
TCH = 512
NCH = S // TCH
EPS = 1e-6
NEGM = -30000.0
N_CMP = 127
NEXP = 32
CAP = 1152
LAMBDA_INIT = 0.8 - 0.6 * math.exp(-0.3 * 0)

FM_CHUNKS = ["qn0", "qn1", "qn2", "qn3", "ks0", "ks1", "kw0", "kw1",
             "kc0", "kc1", "vc0", "vc1", "qd0", "qd1", "qd2", "qd3",
             "kd0", "kd1", "kd2", "kd3"]
NFM = len(FM_CHUNKS)
TM_A = 280
TM_B = 512
NCOLS = NFM * 128 + TM_A + TM_B


class Buf:
    def __init__(self, name, excl=False):
        self.name = name
        self.excl = excl
        self.w = []
        self.r = []
        self.dsem = None
        self.dval = 0


class Sync:
    ENG = ("pe", "act", "dve", "pool", "sp")

    def __init__(self, nc):
        self.nc = nc
        self.eng = {"pe": nc.tensor, "act": nc.scalar, "dve": nc.vector,
                    "pool": nc.gpsimd, "sp": nc.sync}
        self._ctx = nc.cleanup_on_exit()
        self._ctx.__enter__()
        self.sem = {e: nc.alloc_semaphore("sem_" + e) for e in self.ENG}
        self.dpool = [nc.alloc_semaphore("dsem%d" % i) for i in range(80)]
        nc.all_engine_barrier()
        for s_ in list(self.sem.values()) + self.dpool:
            nc.gpsimd.sem_clear(s_)
        nc.all_engine_barrier()
        self.cnt = {e: 0 for e in self.ENG}
        self.known = {e: {} for e in self.ENG}
        self.dma_bufs = []
        self.self_sync = True

    def _wait(self, e, toks):
        best = {}
        for (sem, val, owner) in toks:
            if owner == e and (e == "pe" or not self.self_sync):
                continue
            k = id(sem)
            if self.known[e].get(k, 0) >= val:
                continue
            if k not in best or best[k][1] < val:
                best[k] = (sem, val)
        for k, (sem, val) in best.items():
            self.eng[e].wait_ge(sem, val)
            self.known[e][k] = val

    def _deps(self, reads, writes, e=None):
        toks = []
        for b in reads:
            toks += b.w
            if b.excl:
                toks += [t for t in b.r if t[2] != e]
        for b in writes:
            toks += b.w + b.r
        return toks

    def op(self, e, fn, reads=(), writes=(), inc=True):
        self._wait(e, self._deps(reads, writes, e))
        ins = fn()
        if inc:
            self.cnt[e] += 1
            ins.then_inc(self.sem[e], 1)
            tok = (self.sem[e], self.cnt[e], e)
            for b in reads:
                b.r.append(tok)
                if len(b.r) > 12:
                    b.r = b.r[-12:] if False else b.r
            for b in writes:
                b.w = [tok]
                b.r = []
        return ins

    def group(self, e, fns, reads=(), writes=()):
        self._wait(e, self._deps(reads, writes, e))
        ins = None
        for fn in fns:
            ins = fn()
        self.cnt[e] += 1
        ins.then_inc(self.sem[e], 1)
        tok = (self.sem[e], self.cnt[e], e)
        for b in reads:
            b.r.append(tok)
        for b in writes:
            b.w = [tok]
            b.r = []

    def dma(self, q, out, in_, sb, reads=(), writes=(), add=False):
        self._wait(q, self._deps(reads, writes))
        if sb.dsem is None:
            sb.dsem = self.dpool.pop()
            self.dma_bufs.append(sb)
        sb.dval += 16
        self.eng[q].dma_start(out=out, in_=in_).then_inc(sb.dsem, 16)
        tok = (sb.dsem, sb.dval, "dma")
        for b in reads:
            b.r.append(tok)
        for b in writes:
            if add:
                b.w.append(tok)
            else:
                b.w = [tok]
                b.r = []
        return tok

    def dma_fn(self, q, fn, sb, reads=(), writes=()):
        self._wait(q, self._deps(reads, writes, q))
        if sb.dsem is None:
            sb.dsem = self.dpool.pop()
            self.dma_bufs.append(sb)
        sb.dval += 16
        fn().then_inc(sb.dsem, 16)
        tok = (sb.dsem, sb.dval, "dma")
        for b in reads:
            b.r.append(tok)
        for b in writes:
            b.w = [tok]
            b.r = []
        return tok

    def finish(self):
        self.barrier()
        self._ctx.__exit__(None, None, None)

    def barrier(self, engines=None):
        engines = engines or self.ENG
        toks = [(self.sem[f], self.cnt[f], f) for f in self.ENG if self.cnt[f] > 0]
        toks += [(b.dsem, b.dval, "dma") for b in self.dma_bufs]
        for e in engines:
            ss = self.self_sync
            self.self_sync = True
            self._wait(e, [t for t in toks if t[2] != e])
            self.self_sync = ss


def _fm(v, nchunk):
    return np.ascontiguousarray(v.reshape(nchunk, 128).T)


def _rope_tables():
    t = np.arange(S, dtype=np.float32)

    def tab(rot_dim, hd):
        inv = (500000.0 ** (-np.arange(0, rot_dim, 2, dtype=np.float32) / rot_dim)).astype(np.float32)
        ang = t[:, None] * inv[None, :]
        cos, sin = np.cos(ang).astype(np.float32), np.sin(ang).astype(np.float32)
        half = rot_dim // 2
        C = np.ones((128, S), np.float32)
        Sg = np.zeros((128, S), np.float32)
        for p in range(128):
            d = p % hd
            if d < half:
                C[p] = cos[:, d]
                Sg[p] = -sin[:, d]
            elif d < 2 * half:
                C[p] = cos[:, d - half]
                Sg[p] = sin[:, d - half]
        return C, Sg

    cn, sn = tab(16, 64)
    cd, sd = tab(8, 32)
    return cn, sn, cd, sd


def _const_mats():
    def blk(hd):
        m = np.zeros((128, 128), np.float32)
        for p in range(128):
            b = p // hd
            m[p, b * hd:(b + 1) * hd] = 1.0 / hd
        return m

    def perm(hd, half):
        m = np.zeros((128, 128), np.float32)
        for p in range(128):
            d = p % hd
            if d < half:
                m[p + half, p] = 1.0
            elif d < 2 * half:
                m[p - half, p] = 1.0
        return m

    ident = np.eye(128, dtype=np.float32)
    k = np.arange(128)[:, None]
    q = np.arange(128)[None, :]
    negc = np.where(k > q, NEGM, 0.0).astype(np.float32)
    negw = np.where(k <= q, NEGM, 0.0).astype(np.float32)
    negcmp = np.zeros((128, NT, 128), np.float32)
    for qt in range(NT):
        t = qt * 128 + np.arange(128)[None, :]
        c = np.arange(128)[:, None]
        negcmp[:, qt, :] = np.where(16 * c + 31 <= t, 0.0, NEGM)
    esel = np.zeros((32, NT, 128), np.float32)
    for kt in range(NT):
        for key in range(128):
            esel[(kt * 128 + key) // 64, kt, key] = 1.0
    ov = np.zeros((128, 32), np.float32)
    for c in range(N_CMP):
        for j in range(32):
            o = min(16 * c + 32, 64 * j + 64) - max(16 * c, 64 * j)
            ov[c, j] = max(o, 0) / 32.0
    forced = np.zeros((128, NT, 32), np.float32)
    invalid = np.zeros((128, NT, 32), np.float32)
    for qt in range(NT):
        for p in range(128):
            t = qt * 128 + p
            cur = t // 64
            for j in range(32):
                if j > cur:
                    invalid[p, qt, j] = 1.0
                elif j == 0 or j == cur or j == cur - 1:
                    forced[p, qt, j] = 1.0
    ltri = (np.arange(128)[:, None] < np.arange(128)[None, :]).astype(np.float32)
    return dict(blk64=blk(64), blk32=blk(32), perm64=perm(64, 8), perm32=perm(32, 4), ident=ident,
                negc=negc, negw=negw, negcmp=negcmp.reshape(128, NT * 128), esel=esel.reshape(32, NT * 128),
                ov=ov, forced=forced.reshape(128, NT * 32), invalid=invalid.reshape(128, NT * 32), ltri=ltri)


def _win_cols(w_in):
    o_q, o_kv, o_g, o_qd, o_kd, o_vd = 0, 512, 1280, 1304, 1816, 2328
    cols = []

    def kv(i, g):
        base = o_kv + i * 128 + g * 64
        return list(range(base, base + 64))

    for j in range(4):
        cols += list(range(o_q + j * 128, o_q + (j + 1) * 128))
    for g in range(2):
        cols += kv(2, g) + kv(2, g)
    for g in range(2):
        cols += kv(4, g) + kv(4, g)
    for g in range(2):
        cols += kv(0, g) + kv(0, g)
    for g in range(2):
        cols += kv(1, g) + kv(1, g)
    for j in range(4):
        cols += list(range(o_qd + j * 128, o_qd + (j + 1) * 128))
    for j in range(4):
        cols += list(range(o_kd + j * 128, o_kd + (j + 1) * 128))
    cols += kv(3, 0) + kv(3, 1) + kv(5, 0) + kv(5, 1)
    cols += list(range(o_g, o_g + 24))
    cols += list(range(o_vd, o_vd + 512))
    assert len(cols) == NCOLS
    return np.ascontiguousarray(w_in[:, cols])


_CACHE = {}


def _host_consts():
    if "c" not in _CACHE:
        cn, sn, cd, sd = _rope_tables()
        cm = _const_mats()
        _CACHE["c"] = dict(cn=cn, sn=sn, cd=cd, sd=sd, **cm)
    return _CACHE["c"]


def build(nseq, stage="full", dbg=False):
    nc = bass.Bass("TRN2", target_bir_lowering=False)
    sy = Sync(nc)
    eng = sy.eng
    PE, ACT, DVE, POOL = eng["pe"], eng["act"], eng["dve"], eng["pool"]

    used_inputs = []

    def din(name, shape, dt=F32):
        used_inputs.append(name)
        return nc.dram_tensor(name, list(shape), dt, kind="ExternalInput").ap()

    x_d = din("x", [nseq, S, D])
    cT_d = din("cT", [128, 8, nseq])
    wada_d = din("w_ada", [D, 6 * D])
    bada_fm_d = din("b_ada_fm", [128, 48])
    bada_row_d = din("b_ada_row", [1, 6 * D])
    gmix_d = din("gmix_fm", [128, 8])
    gffn_d = din("gffn_fm", [128, 8])
    win_d = din("w_inp", [D, NCOLS])
    rowgain_d = din("rowgain", [128, 5])
    gk0_d = din("gk0_row", [1, 128])
    go_d = din("go_row", [1, 64])
    lamv_d = din("lamv", [1, 128])
    pefm_d = din("pe_fm", [128, 32])
    wc1_d = din("w_cmp1", [2, 2048, 256])
    wc2k_d = din("w_cmp2k", [256, 128])
    wc2v_d = din("w_cmp2v", [256, 64])
    wout_d = din("w_out", [D, D])
    cn_d = din("cn", [128, S]); sn_d = din("sn", [128, S])
    cd_d = din("cd", [128, S]); sd_d = din("sd", [128, S])
    mats_d = {k: din("m_" + k, shp) for k, shp in [
        ("blk64", [128, 128]), ("blk32", [128, 128]), ("perm64", [128, 128]), ("perm32", [128, 128]),
        ("ident", [128, 128]), ("negc", [128, 128]), ("negw", [128, 128]), ("negcmp", [128, S]),
        ("esel", [32, S]), ("ov", [128, 32]), ("forced", [128, NT * 32]), ("invalid", [128, NT * 32]),
        ("ltri", [128, 128])]}
    ecap_d = din("ecap", [128, 32])
    wr_d = din("w_r", [D, 36])
    br_d = din("b_r", [1, 36])
    wg_d = din("w_g", [NEXP, D, 512])
    wu_d = din("w_u", [NEXP, D, 512])
    wd_d = din("w_d", [NEXP, 512, D])
    out_d = nc.dram_tensor("out", [nseq, S, D], F32, kind="ExternalOutput").ap()
    modrow_d = nc.dram_tensor("modrow", [nseq, 6 * D], F32, kind="Internal").ap()
    x1_d = nc.dram_tensor("x1s", [nseq, S, D], F32, kind="Internal").ap()
    dbg_d = {}

    def sb(name, shape, dt=F32):
        return nc.alloc_sbuf_tensor(name, list(shape), dt)

    banks = [nc.alloc_psum_tensor("bank%d" % i, [128, 512], F32) for i in range(8)]
    bbuf = [Buf("bank%d" % i, excl=True) for i in range(8)]

    class Rot:
        def __init__(self, idx):
            self.idx = idx; self.i = 0

        def next(self):
            k = self.idx[self.i % len(self.idx)]
            self.i += 1
            return banks[k], bbuf[k]

    rotP = Rot([0, 1])
    rotS = Rot([2, 3, 4, 7])
    rotO = Rot([5, 6])

    cb = {}

    def const_load(name, d_ap, shape, dt, q="pool"):
        t = sb("c_" + name, shape, dt)
        b = Buf("c_" + name)
        sy.dma(q, t[:], d_ap, b, writes=[b])
        cb[name] = (t, b)
        return t, b

    for k in ["blk64", "blk32", "perm64", "perm32", "ident", "negc", "negw"]:
        const_load(k, mats_d[k][:, :], [128, 128], BF16)
    const_load("negcmp", mats_d["negcmp"][:, :], [128, S], BF16)
    esel_t = sb("c_esel", [128, S], BF16); esel_b = Buf("c_esel")
    sy.op("pool", lambda: POOL.memset(esel_t[:], 0.0), writes=[esel_b])
    sy.dma("pool", esel_t[0:32, :], mats_d["esel"][:, :], esel_b, writes=[esel_b])
    cb["esel"] = (esel_t, esel_b)
    const_load("forced", mats_d["forced"][:, :], [128, NT * 32], F32, q="sp")
    const_load("invalid", mats_d["invalid"][:, :], [128, NT * 32], F32, q="sp")
    const_load("identf", mats_d["ident"][:, :], [128, 128], F32, q="sp")
    const_load("rowgain", rowgain_d[:, :], [128, 5], F32, q="sp")
    const_load("gmix", gmix_d[:, :], [128, 8], F32, q="sp")
    const_load("gffn", gffn_d[:, :], [128, 8], F32, q="sp")
    const_load("bada_fm", bada_fm_d[:, :], [128, 48], F32, q="sp")
    const_load("cT", cT_d[:, :, :], [128, 8, nseq], BF16)
    const_load("gk0", gk0_d[0:1, :].partition_broadcast(128), [128, 128], F32)
    const_load("go", go_d[0:1, :].partition_broadcast(128), [128, 64], F32)
    const_load("lamv", lamv_d[0:1, :].partition_broadcast(128), [128, 128], F32)
    const_load("pefm", pefm_d[:, :], [128, 32], BF16)
    const_load("wout", wout_d.rearrange("(k p) n -> p k n", p=128), [128, 8, D], BF16)
    const_load("wc1k", wc1_d[0].rearrange("(j p) n -> p j n", p=128), [128, 16, 256], BF16)
    const_load("wc1v", wc1_d[1].rearrange("(j p) n -> p j n", p=128), [128, 16, 256], BF16)
    const_load("wc2k", wc2k_d.rearrange("(m p) n -> p m n", p=128), [128, 2, 128], BF16)
    const_load("wc2v", wc2v_d.rearrange("(m p) n -> p m n", p=128), [128, 2, 64], BF16)

    if stage == "consts":
        sy.finish()
        return nc, sy, locals()

    def C(name):
        return cb[name][0]

    def CB(name):
        return cb[name][1]

    epsT = sb("epsT", [128, 1]); epsB = Buf("epsT")
    sy.op("dve", lambda: DVE.memset(epsT[:], EPS), writes=[epsB])

    modT = sb("modT", [128, 48, nseq]); modTB = Buf("modT")
    gs1 = sb("gs1", [128, 8, nseq]); gs1B = Buf("gs1")
    gs2 = sb("gs2", [128, 8, nseq]); gs2B = Buf("gs2")
    neglam = sb("neglam", [128, 1]); neglamB = Buf("neglam")
    wst = [sb("wst%d" % i, [128, 8, 512], BF16) for i in range(2)]
    wstB = [Buf("wst%d" % i) for i in range(2)]
    stg = [sb("stg%d" % i, [128, 1024]) for i in range(2)]
    stgB = [Buf("stg%d" % i) for i in range(2)]
    wi = [0]

    def wload(d_ap, ncols):
        i = wi[0] % 2
        wi[0] += 1
        sy.dma("pool", wst[i][:, :, 0:ncols], d_ap, wstB[i], writes=[wstB[i]])
        return wst[i], wstB[i]

    wada_v = wada_d.rearrange("(k p) n -> p k n", p=128)
    for gi in range(12):
        wt, wB = wload(wada_v[:, :, gi * 512:(gi + 1) * 512], 512)
        if True:
            pt, pB = rotP.next()
            fns = []
            for mc in range(4):
                for k in range(8):
                    fns.append(lambda mc=mc, k=k: PE.matmul(
                        pt[:, mc * nseq:(mc + 1) * nseq], lhsT=wt[:, k, mc * 128:(mc + 1) * 128],
                        rhs=C("cT")[:, k, :], start=(k == 0), stop=(k == 7)))
            sy.group("pe", fns, reads=[wB, CB("cT")], writes=[pB])
            for mc in range(4):
                j = gi * 4 + mc
                sy.op("dve", lambda mc=mc, j=j: DVE.tensor_scalar(
                    out=modT[:, j, :], in0=pt[:, mc * nseq:(mc + 1) * nseq],
                    scalar1=C("bada_fm")[:, j:j + 1], scalar2=None, op0=ALU.add),
                    reads=[pB, CB("bada_fm")], writes=[modTB])
    for k in range(8):
        sy.op("dve", lambda k=k: DVE.tensor_scalar(out=gs1[:, k, :], in0=modT[:, 8 + k, :], scalar1=1.0,
                                                    scalar2=C("gmix")[:, k:k + 1], op0=ALU.add, op1=ALU.mult),
              reads=[modTB, CB("gmix")], writes=[gs1B])
        sy.op("dve", lambda k=k: DVE.tensor_scalar(out=gs2[:, k, :], in0=modT[:, 32 + k, :], scalar1=1.0,
                                                    scalar2=C("gffn")[:, k:k + 1], op0=ALU.add, op1=ALU.mult),
              reads=[modTB, CB("gffn")], writes=[gs2B])
    if stage == "ada":
        sy.finish()
        return nc, sy, locals()
    lt = sb("lamtmp", [128, 8]); ltB = Buf("lamtmp")
    lv = C("lamv")
    lprod = sb("lamprod", [128, 64])
    sy.op("dve", lambda: DVE.tensor_tensor(out=lprod[:, 0:32], in0=lv[:, 0:32], in1=lv[:, 32:64], op=ALU.mult),
          reads=[CB("lamv")], writes=[ltB])
    sy.op("dve", lambda: DVE.tensor_tensor(out=lprod[:, 32:64], in0=lv[:, 64:96], in1=lv[:, 96:128], op=ALU.mult),
          reads=[CB("lamv")], writes=[ltB])
    sy.op("dve", lambda: DVE.tensor_reduce(out=lt[:, 0:2], in_=lprod[:].rearrange("p (a b) -> p a b", a=2),
                                            axis=AX.X, op=ALU.add), reads=[ltB], writes=[ltB])
    lt2 = sb("lamtmp2", [128, 2]); lt2B = Buf("lamtmp2")
    sy.op("act", lambda: ACT.activation(out=lt2[:, 0:2], in_=lt[:, 0:2], func=AF.Exp), reads=[ltB], writes=[lt2B])
    sy.op("dve", lambda: DVE.tensor_tensor(out=lt[:, 4:5], in0=lt2[:, 1:2], in1=lt2[:, 0:1], op=ALU.subtract),
          reads=[lt2B], writes=[ltB])
    sy.op("dve", lambda: DVE.tensor_scalar(out=neglam[:, 0:1], in0=lt[:, 4:5], scalar1=-LAMBDA_INIT, scalar2=None,
                                            op0=ALU.add), reads=[ltB], writes=[neglamB])

    peb = sb("peb", [128, 4]); pebB = Buf("peb")
    pt, pB = rotP.next()
    fns = []
    for kv in range(2):
        w1 = C("wc1k") if kv == 0 else C("wc1v")
        for mc in range(2):
            for j in range(16):
                fns.append(lambda kv=kv, mc=mc, j=j, w1=w1: PE.matmul(
                    pt[:, (kv * 2 + mc) * 2:(kv * 2 + mc) * 2 + 1], lhsT=w1[:, j, mc * 128:(mc + 1) * 128],
                    rhs=C("pefm")[:, kv * 16 + j:kv * 16 + j + 1], start=(j == 0), stop=(j == 15)))
    sy.group("pe", fns, reads=[CB("wc1k"), CB("wc1v"), CB("pefm")], writes=[pB])
    sy.op("dve", lambda: DVE.tensor_copy(out=peb[:, 0:4], in_=pt[:, 0:8:2]), reads=[pB], writes=[pebB])

    if stage == "setup":
        sy.finish()
        return nc, sy, locals()
    return _build_rest(nc, sy, locals(), nseq, stage, dbg)


class _Stop(Exception):
    pass


class Arena:
    def __init__(self, nc, name, nbytes):
        self.t = nc.alloc_sbuf_tensor(name, [128, nbytes // 2], BF16)
        self.cap = nbytes // 2
        self.off = 0

    def reset(self):
        self.off = 0

    def get(self, shape, dt):
        n = 1
        for s_ in shape[1:]:
            n *= s_
        esz = 4 if dt in (F32, I32, U32) else 2
        n2 = n * esz // 2
        self.off = (self.off + 15) // 16 * 16
        assert self.off + n2 <= self.cap, ("arena overflow", self.off, n2, self.cap)
        ap = self.t[0:shape[0], self.off:self.off + n2]
        self.off += n2
        if esz == 4:
            ap = ap.bitcast(dt)
        if len(shape) == 3:
            ap = ap.rearrange("p (a b) -> p a b", a=shape[1])
        elif len(shape) == 4:
            ap = ap.rearrange("p (a b c) -> p a b c", a=shape[1], b=shape[2])
        return ap


def _build_rest(nc, sy, L, nseq, stage, dbg):
    try:
        return _build_rest2(nc, sy, L, nseq, stage, dbg)
    except _Stop:
        sy.finish()
        return nc, sy, L


def _build_rest2(nc, sy, L, nseq, stage, dbg):
    g = dict(L)

    def stop_if(name):
        if stage == name:
            raise _Stop()

    used_inputs = g["used_inputs"]
    PE, ACT, DVE, POOL = g["PE"], g["ACT"], g["DVE"], g["POOL"]
    C, CB = g["C"], g["CB"]
    rotP, rotS, rotO, Rot = g["rotP"], g["rotS"], g["rotO"], g["Rot"]
    rotPP = Rot([0, 1, 7, 2, 3, 4, 5, 6])
    x_d, win_d, out_d, x1_d, modrow_d = g["x_d"], g["win_d"], g["out_d"], g["x1_d"], g["modrow_d"]
    gs1, gs1B, gs2, gs2B, modT, modTB = g["gs1"], g["gs1B"], g["gs2"], g["gs2B"], g["modT"], g["modTB"]
    neglam, neglamB, peb, pebB, epsT, epsB = g["neglam"], g["neglamB"], g["peb"], g["pebB"], g["epsT"], g["epsB"]
    wload, stg, stgB = g["wload"], g["stg"], g["stgB"]
    cn_d, sn_d, cd_d, sd_d = g["cn_d"], g["sn_d"], g["cd_d"], g["sd_d"]

    NROWS_ = NEXP * CAP + 128
    xbuf_d = nc.dram_tensor("xbuf", [NROWS_, D], BF16, kind="Internal").ap()
    ybuf_d = nc.dram_tensor("ybuf", [NROWS_, D], BF16, kind="Internal").ap()
    xbufB = Buf("xbuf")
    if stage not in ("attn",):
        ztile = nc.alloc_sbuf_tensor("ztile", [128, D], BF16); ztileB = Buf("ztile")
        sy.op("dve", lambda: DVE.memset(ztile[:], 0.0), writes=[ztileB])
        for r0 in range(0, NROWS_, 128):
            sy.dma("sp", xbuf_d[r0:r0 + 128, :], ztile[:], ztileB, reads=[ztileB], writes=[xbufB], add=True)
        sy.dma("sp", ybuf_d[NEXP * CAP:NEXP * CAP + 128, :], ztile[:], ztileB, reads=[ztileB])
    g["xbuf_d"], g["ybuf_d"], g["xbufB"] = xbuf_d, ybuf_d, xbufB
    arena = Arena(nc, "arena", 133248)
    A = arena.get
    hT = A([128, 8, TCH], BF16); hTB = Buf("hT")
    QNOPE = A([128, 8, TCH], BF16); QNOPEB = Buf("qnope")
    QROPE = A([128, 8, TCH], BF16); QROPEB = Buf("qrope")
    QD = A([128, 4, TCH], BF16); QDB = Buf("qd")
    KD = A([128, 4, S], BF16); KDB = Buf("kd")
    KS = A([128, 2, S], BF16); KSB = Buf("ks")
    KW = A([128, 2, S], BF16); KWB = Buf("kw")
    KC2 = A([128, 2, TCH + 32], BF16); KC2B = Buf("kc2")
    VC2 = A([128, 2, TCH + 32], BF16); VC2B = Buf("vc2")
    VS = A([128, NT, 2, 65], BF16); VSB = Buf("vs")
    VW = A([128, NT, 2, 65], BF16); VWB = Buf("vw")
    VD = A([128, NT, 8, 65], BF16); VDB = Buf("vd")
    HIDK = A([128, 2, 2, 128], BF16); HIDKB = Buf("hidk")
    HIDV = A([128, 2, 2, 128], BF16); HIDVB = Buf("hidv")
    KCMPT = A([128, 2, 128], BF16); KCMPTB = Buf("kcmpt")
    VCMP = A([128, 2, 97], BF16); VCMPB = Buf("vcmp")
    ropeT = A([128, 4, TCH], F32); ropeTB = Buf("ropeT")
    GATE = A([128, 4, 24], F32); GATEB = Buf("gate")
    G1 = A([128, D], F32); G1B = Buf("g1")
    xn = A([128, D], BF16); xnB = Buf("xn")
    PT = [A([128, 512], BF16) for _ in range(4)]; PTB = [Buf("pt%d" % i) for i in range(4)]
    sqb = [PT[0], PT[1]]; sqbB = [PTB[0], PTB[1]]
    lnb = A([128, 512], F32); lnbB = Buf("lnb")
    rstd = A([128, 512], F32); rstdB = Buf("rstd")
    qnb = [A([128, 512], BF16) for _ in range(3)]; qnbB = [Buf("qn%d" % i) for i in range(3)]
    t1b = A([128, 512], F32); t1bB = Buf("t1b")
    t2b = A([128, 512], F32); t2bB = Buf("t2b")
    small = A([128, 256], F32); smallB = Buf("small")
    onsa, onsaB = lnb, lnbB
    odif, odifB = t2b, t2bB
    tmpo, tmpoB = t1b, t1bB
    attn = A([128, D], BF16); attnB = Buf("attn")
    attn2 = [(attn, attnB), (A([128, D], BF16), Buf("attn_b"))]
    attnT = A([128, 8, 128], BF16); attnTB = Buf("attnT")
    score = A([128, 2, 32], F32); scoreB = Buf("score")
    negsel = A([128, 2, 32], BF16); negselB = Buf("negsel")
    NEGSELT = A([128, 2, 128], BF16); NEGSELTB = Buf("negselT")
    kcn = A([128, 128], BF16); kcnB = Buf("kcn")

    sy.op("pool", lambda: POOL.memset(HIDK, 0.0), writes=[HIDKB])
    sy.op("pool", lambda: POOL.memset(HIDV, 0.0), writes=[HIDVB])
    sy.op("pool", lambda: POOL.memset(KC2, 0.0), writes=[KC2B])
    sy.op("pool", lambda: POOL.memset(VC2, 0.0), writes=[VC2B])
    sy.op("pool", lambda: POOL.memset(VS, 1.0), writes=[VSB])
    sy.op("pool", lambda: POOL.memset(VW, 1.0), writes=[VWB])
    sy.op("pool", lambda: POOL.memset(VD, 1.0), writes=[VDB])
    sy.op("pool", lambda: POOL.memset(VCMP, 1.0), writes=[VCMPB])
    sy.op("pool", lambda: POOL.memset(NEGSELT, 0.0), writes=[NEGSELTB])
    NEGSELT_b = A([128, 2, 128], BF16); NEGSELT_bB = Buf("negselT_b")
    sy.op("pool", lambda: POOL.memset(NEGSELT_b, 0.0), writes=[NEGSELT_bB])
    onsa2 = [(onsa, onsaB), (rstd, rstdB)]
    score2 = [(score, scoreB), (A([128, 2, 32], F32), Buf("score_b"))]
    negsel2 = [(negsel, negselB), (A([128, 2, 32], BF16), Buf("negsel_b"))]
    NEGSELT2 = [(NEGSELT, NEGSELTB), (NEGSELT_b, NEGSELT_bB)]
    sy.op("pool", lambda: POOL.memset(QNOPE, 0.0), writes=[QNOPEB])
    sy.op("pool", lambda: POOL.memset(QROPE, 0.0), writes=[QROPEB])
    goS = nc.alloc_sbuf_tensor("goS", [128, 64], F32); goSB = Buf("goS")
    sy.op("dve", lambda: DVE.tensor_scalar(out=goS[:], in0=C("go")[:], scalar1=1.0 - LAMBDA_INIT, scalar2=None, op0=ALU.mult),
          reads=[CB("go")], writes=[goSB])
    causal01 = nc.alloc_sbuf_tensor("causal01", [128, 128], BF16); causal01B = Buf("causal01")
    sy.op("dve", lambda: DVE.tensor_scalar(out=causal01[:], in0=C("negc")[:], scalar1=-0.5, scalar2=None, op0=ALU.is_gt),
          reads=[CB("negc")], writes=[causal01B])
    ovt = sb_tmp = nc.alloc_sbuf_tensor("ovt", [128, 32], BF16)
    ovB = Buf("ovt")
    sy.dma("pool", ovt[:], g["mats_d"]["ov"][:, :], ovB, writes=[ovB])
    for gq in range(2):
        sy.op("dve", lambda gq=gq: DVE.tensor_copy(out=VCMP[:, gq, 65:97], in_=ovt[:]), reads=[ovB], writes=[VCMPB])

    stop_if("init")
    ident = C("ident"); identB = CB("ident")
    rowgain = C("rowgain")
    cnt = {"sq": 0, "qn": 0, "pt": 0}

    def normrope(pt, pB, blkname, permname, gcol, ctab, stab, nope_dsts, nope_bufs, rope_dsts, rope_bufs, ncols=TCH):
        i = cnt["sq"] % 2; cnt["sq"] += 1
        sq, sqB = sqb[i], sqbB[i]
        sy.op("act", lambda: ACT.activation(out=sq[:, 0:ncols], in_=pt[:, 0:ncols], func=AF.Square),
              reads=[pB], writes=[sqB])
        p2, p2B = rotPP.next()
        sy.op("pe", lambda: PE.matmul(p2[:, 0:ncols], lhsT=C(blkname)[:], rhs=sq[:, 0:ncols], start=True, stop=True),
              reads=[sqB, CB(blkname)], writes=[p2B])
        sy.op("act", lambda: ACT.activation(out=lnb[:, 0:ncols], in_=p2[:, 0:ncols], func=AF.Ln, bias=epsT[:, 0:1]),
              reads=[p2B, epsB], writes=[lnbB])
        sy.op("act", lambda: ACT.activation(out=rstd[:, 0:ncols], in_=lnb[:, 0:ncols], func=AF.Exp, scale=-0.5),
              reads=[lnbB], writes=[rstdB])
        j = cnt["qn"] % 3; cnt["qn"] += 1
        qn, qnB = qnb[j][:, 0:ncols], qnbB[j]
        sy.op("dve", lambda: DVE.scalar_tensor_tensor(out=qn, in0=pt[:, 0:ncols], scalar=rowgain[:, gcol:gcol + 1],
                                                      in1=rstd[:, 0:ncols], op0=ALU.mult, op1=ALU.mult),
              reads=[pB, rstdB, CB("rowgain")], writes=[qnB])
        for (dst, lo, hi) in (nope_dsts or []):
            sy.op("act", lambda: ACT.activation(out=dst, in_=qn[lo:hi, :], func=AF.Copy), reads=[qnB], writes=nope_bufs)
        yield
        p3, p3B = rotPP.next()
        sy.op("pe", lambda: PE.matmul(p3[:, 0:ncols], lhsT=C(permname)[:], rhs=qn, start=True, stop=True),
              reads=[qnB, CB(permname)], writes=[p3B])
        sy.op("dve", lambda: DVE.tensor_tensor(out=t1b[:, 0:ncols], in0=qn, in1=ctab, op=ALU.mult),
              reads=[qnB, ropeTB], writes=[t1bB])
        sy.op("dve", lambda: DVE.tensor_tensor(out=t2b[:, 0:ncols], in0=p3[:, 0:ncols], in1=stab, op=ALU.mult),
              reads=[p3B, ropeTB], writes=[t2bB])
        for (dst, lo, hi) in rope_dsts:
            sy.op("dve", lambda: DVE.tensor_tensor(out=dst, in0=t1b[lo:hi, 0:ncols], in1=t2b[lo:hi, 0:ncols], op=ALU.add),
                  reads=[t1bB, t2bB], writes=rope_bufs)

    win_v = win_d.rearrange("(k p) n -> p k n", p=128)
    xi = [0]

    negc, negw, negcmp, esel = C("negc"), C("negw"), C("negcmp"), C("esel")
    ptc = [0]
    m8 = nc.alloc_sbuf_tensor("m8", [128, 16], F32); m8B = Buf("m8")

    def run_jobs(jobs):
        st = [None] * len(jobs)

        def issue_qk(n):
            bank, bB = rotS.next()
            st[n] = (bank, bB)
            jb = jobs[n]
            sy.group("pe", [(lambda f=f, bank=bank: f(bank)) for f in jb["qk"]], reads=jb["qk_reads"], writes=[bB])

        DEPTH = 3
        for n in range(min(DEPTH, len(jobs))):
            issue_qk(n)
        for n, jb in enumerate(jobs):
            if n + DEPTH < len(jobs):
                issue_qk(n + DEPTH)
            bank, bB = st[n]
            pi = ptc[0] % 4; ptc[0] += 1
            P, PB = PT[pi], PTB[pi]
            ncol = jb["ncol"]
            sy.op("act", lambda: ACT.activation(out=P[:, 0:ncol], in_=bank[:, 0:ncol], func=AF.Exp, scale=jb["scale"]),
                  reads=[bB], writes=[PB])
            if jb.get("mask01") is not None:
                c0 = jb["mask01"]
                sy.op("pool", lambda: POOL.tensor_tensor(out=P[:, c0:c0 + 128], in0=P[:, c0:c0 + 128], in1=causal01[:], op=ALU.mult),
                      reads=[PB, causal01B], writes=[PB])
            sy.group("pe", [(lambda f=f, P=P: f(P)) for f in jb["pv"]], reads=[PB] + jb["pv_reads"], writes=[jb["O"]])
            if jb.get("post") is not None:
                jb["post"]()

    def make_jobs(items, Oap, OB, scale, vreads, kreads, post=None, postmask=False):
        jobs = []
        nb = (len(items) + 3) // 4
        for bi in range(nb):
            blk = items[bi * 4:(bi + 1) * 4]
            qk, pv = [], []
            mask01 = None
            for n, (lk, rq, masks, va, tp) in enumerate(blk):
                gidx = bi * 4 + n
                if postmask and masks:
                    mask01 = n * 128
                    masks = []
                def fqk(bank, lk=lk, rq=rq, masks=masks, n=n, tp=tp):
                    out = bank[:, n * 128:(n + 1) * 128]
                    kw = {} if tp is None else {"tile_position": tp}
                    r = PE.matmul(out, lhsT=lk, rhs=rq, start=True, stop=(len(masks) == 0), **kw)
                    for mi, (ml, mr) in enumerate(masks):
                        r = PE.matmul(out, lhsT=ml, rhs=mr, start=False, stop=(mi == len(masks) - 1))
                    return r
                qk.append(fqk)
                def fpv(P, va=va, n=n, gidx=gidx):
                    return PE.matmul(Oap, lhsT=P[:, n * 128:(n + 1) * 128], rhs=va, start=(gidx == 0), stop=(gidx == len(items) - 1))
                pv.append(fpv)
            jobs.append(dict(qk=qk, qk_reads=kreads, ncol=len(blk) * 128, scale=scale, mask01=mask01, pv=pv,
                             pv_reads=vreads, O=OB, post=(post if bi == nb - 1 else None)))
        return jobs

    def id_masks(neg_ap):
        return [(ident[:], neg_ap)]

    def compress_A(c):
        i0 = 1 if c == 0 else 0
        nblk = 32 - i0
        blk0 = 32 * c - 1 + i0
        for kv in range(2):
            src_t, srcB = (KC2, KC2B) if kv == 0 else (VC2, VC2B)
            w1, w1B = (C("wc1k"), CB("wc1k")) if kv == 0 else (C("wc1v"), CB("wc1v"))
            HID, HIDB = (HIDK, HIDKB) if kv == 0 else (HIDV, HIDVB)
            for gq in range(2):
                pt, pB = rotP.next()
                fns = []
                for m in range(2):
                    for j in range(16):
                        st0 = 16 + 2 * j + 16 * i0
                        fns.append(lambda m=m, j=j, st0=st0: PE.matmul(
                            pt[:, m * 32 + i0:m * 32 + 32], lhsT=w1[:, j, m * 128:(m + 1) * 128],
                            rhs=src_t[:, gq, st0:st0 + 16 * (nblk - 1) + 1:16], start=(j == 0), stop=(j == 15)))
                sy.group("pe", fns, reads=[srcB, w1B], writes=[pB])
                hx = small[:, 64:128]
                for m in range(2):
                    sy.op("dve", lambda m=m: DVE.tensor_scalar(out=hx[:, m * 32:(m + 1) * 32], in0=pt[:, m * 32:(m + 1) * 32],
                                                               scalar1=peb[:, kv * 2 + m:kv * 2 + m + 1], scalar2=None, op0=ALU.add),
                          reads=[pB, pebB], writes=[smallB])
                h2 = small[:, 128:192]
                sy.op("dve", lambda: DVE.tensor_tensor(out=h2, in0=hx, in1=hx, op=ALU.mult), reads=[smallB], writes=[smallB])
                sy.op("dve", lambda: DVE.tensor_scalar(out=h2, in0=h2, scalar1=0.044715, scalar2=1.0, op0=ALU.mult, op1=ALU.add),
                      reads=[smallB], writes=[smallB])
                sy.op("dve", lambda: DVE.tensor_tensor(out=h2, in0=h2, in1=hx, op=ALU.mult), reads=[smallB], writes=[smallB])
                h3 = small[:, 192:256]
                sy.op("act", lambda: ACT.activation(out=h3, in_=h2, func=AF.Exp, scale=-1.5957691216057308),
                      reads=[smallB], writes=[smallB])
                sy.op("dve", lambda: DVE.tensor_scalar(out=h3, in0=h3, scalar1=1.0, scalar2=None, op0=ALU.add),
                      reads=[smallB], writes=[smallB])
                sy.op("dve", lambda: DVE.reciprocal(out=h2, in_=h3), reads=[smallB], writes=[smallB])
                for m in range(2):
                    sy.op("dve", lambda m=m: DVE.tensor_tensor(out=HID[:, gq, m, blk0:blk0 + nblk], in0=hx[:, m * 32 + i0:m * 32 + 32],
                                                               in1=h2[:, m * 32 + i0:m * 32 + 32], op=ALU.mult),
                          reads=[smallB], writes=[HIDB])
        for t_, tB in ((KC2, KC2B), (VC2, VC2B)):
            sy.op("dve", lambda: DVE.tensor_copy(out=t_[:, :, 0:32], in_=t_[:, :, TCH:TCH + 32]), reads=[tB], writes=[tB])

    def compress_B(c):
        for gq in range(2):
            pt, pB = rotP.next()
            sy.group("pe", [(lambda m=m: PE.matmul(pt[:, 0:128], lhsT=HIDK[:, gq, m, :], rhs=C("wc2k")[:, m, :],
                                                   start=(m == 0), stop=(m == 1))) for m in range(2)],
                     reads=[HIDKB, CB("wc2k")], writes=[pB])
            sy.op("act", lambda: ACT.activation(out=t1b[:, 0:64], in_=pt[:, 0:64], func=AF.Square, accum_out=small[:, 5:6]),
                  reads=[pB], writes=[t1bB, smallB])
            sy.op("act", lambda: ACT.activation(out=small[:, 6:7], in_=small[:, 5:6], func=AF.Ln, bias=epsT[:, 0:1], scale=1.0 / 64),
                  reads=[smallB, epsB], writes=[smallB])
            sy.op("act", lambda: ACT.activation(out=small[:, 7:8], in_=small[:, 6:7], func=AF.Exp, scale=-0.5),
                  reads=[smallB], writes=[smallB])
            sy.op("dve", lambda: DVE.scalar_tensor_tensor(out=kcn, in0=pt[:, 0:128], scalar=small[:, 7:8], in1=C("gk0")[:],
                                                          op0=ALU.mult, op1=ALU.mult), reads=[pB, smallB, CB("gk0")], writes=[kcnB])
            p2, p2B = rotP.next()
            p2b = p2[:].bitcast(BF16)
            sy.op("pe", lambda: PE.transpose(p2b[:, 0:128], kcn, ident[:]), reads=[kcnB, identB], writes=[p2B])
            sy.op("dve", lambda: DVE.tensor_copy(out=KCMPT[:, gq, :], in_=p2b[:, 0:128]), reads=[p2B], writes=[KCMPTB])
            p3, p3B = rotP.next()
            sy.group("pe", [(lambda m=m: PE.matmul(p3[:, 0:64], lhsT=HIDV[:, gq, m, :], rhs=C("wc2v")[:, m, :],
                                                   start=(m == 0), stop=(m == 1))) for m in range(2)],
                     reads=[HIDVB, CB("wc2v")], writes=[p3B])
            sy.op("act", lambda: ACT.activation(out=VCMP[:, gq, 0:64], in_=p3[:, 0:64], func=AF.Copy), reads=[p3B], writes=[VCMPB])

    def x1_dst(b, qt):
        t_ = out_d if stage == "attn" else x1_d
        return t_[b, qt * 128:(qt + 1) * 128, :]

    bct = nc.alloc_sbuf_tensor("bct", [128, 128], F32); bctB = Buf("bct")

    def bcast_row(dst, dstB, j0, b):
        for k in range(8):
            sy.op("dve", lambda: DVE.tensor_copy(out=bct[:], in_=modT[:, j0 + k, b:b + 1].to_broadcast([128, 128])),
                  reads=[modTB], writes=[bctB])
            p2, p2B = rotP.next()
            sy.op("pe", lambda: PE.transpose(p2[:, 0:128], bct[:], C("identf")[:]), reads=[bctB, CB("identf")], writes=[p2B])
            sy.op("act", lambda: ACT.activation(out=dst[:, k * 128:(k + 1) * 128], in_=p2[:, 0:128], func=AF.Copy),
                  reads=[p2B], writes=[dstB])

    def bcast_rows_from(srcT, srcB, dst, dstB, b):
        for k in range(8):
            sy.op("dve", lambda: DVE.tensor_copy(out=bct[:], in_=srcT[:, k, b:b + 1].to_broadcast([128, 128])),
                  reads=[srcB], writes=[bctB])
            p2, p2B = rotP.next()
            sy.op("pe", lambda: PE.transpose(p2[:, 0:128], bct[:], C("identf")[:]), reads=[bctB, CB("identf")], writes=[p2B])
            sy.op("act", lambda: ACT.activation(out=dst[:, k * 128:(k + 1) * 128], in_=p2[:, 0:128], func=AF.Copy),
                  reads=[p2B], writes=[dstB])

    SC_N = 0.125
    SC_D = 32.0 ** -0.5

    normed = set()

    def emit_norm_tile(b, c, i):
        if (b, c, i) in normed or b >= nseq:
            return
        normed.add((b, c, i))
        xt, xB = stg[xi[0] % 2], stgB[xi[0] % 2]; xi[0] += 1
        sy.dma("sp", xt[:], x_d[b, c * TCH + i * 128:c * TCH + (i + 1) * 128, :], xB, writes=[xB])
        sy.op("act", lambda: ACT.activation(out=t1b[:, 0:512], in_=xt[:, 0:512], func=AF.Square,
                                            accum_out=small[:, 0:1]), reads=[xB], writes=[t1bB, smallB])
        sy.op("act", lambda: ACT.activation(out=t1b[:, 0:512], in_=xt[:, 512:1024], func=AF.Square,
                                            accum_out=small[:, 1:2]), reads=[xB], writes=[t1bB, smallB])
        sy.op("dve", lambda: DVE.tensor_tensor(out=small[:, 2:3], in0=small[:, 0:1], in1=small[:, 1:2], op=ALU.add),
              reads=[smallB], writes=[smallB])
        sy.op("act", lambda: ACT.activation(out=small[:, 3:4], in_=small[:, 2:3], func=AF.Ln, bias=epsT[:, 0:1],
                                            scale=1.0 / D), reads=[smallB, epsB], writes=[smallB])
        sy.op("act", lambda: ACT.activation(out=small[:, 4:5], in_=small[:, 3:4], func=AF.Exp, scale=-0.5),
              reads=[smallB], writes=[smallB])
        sy.op("dve", lambda: DVE.tensor_scalar(out=xn, in0=xt[:], scalar1=small[:, 4:5], scalar2=None, op0=ALU.mult),
              reads=[xB, smallB], writes=[xnB])
        pt, pB = rotP.next()
        ptb = pt[:].bitcast(BF16)
        sy.group("pe", [(lambda k=k: PE.transpose(ptb[:, k * 128:(k + 1) * 128], xn[:, k * 128:(k + 1) * 128], ident[:]))
                        for k in range(8)], reads=[xnB, identB], writes=[pB])
        for k in range(8):
            e_ = "act" if k % 2 == 0 else "dve"
            if e_ == "act":
                sy.op("act", lambda k=k: ACT.activation(out=hT[:, k, i * 128:(i + 1) * 128], in_=ptb[:, k * 128:(k + 1) * 128],
                                                        func=AF.Identity, scale=gs1[:, k, b:b + 1], bias=modT[:, k, b:b + 1]),
                      reads=[pB, gs1B, modTB], writes=[hTB])
            else:
                sy.op("dve", lambda k=k: DVE.tensor_scalar(out=hT[:, k, i * 128:(i + 1) * 128], in0=ptb[:, k * 128:(k + 1) * 128],
                                                           scalar1=gs1[:, k, b:b + 1], scalar2=modT[:, k, b:b + 1],
                                                           op0=ALU.mult, op1=ALU.add),
                      reads=[pB, gs1B, modTB], writes=[hTB])

    def attention_chunk(b, c):
        compress_B(c)
        stop_if("cmpr")

        def cmp_part(i):
            qt = c * 4 + i
            qs = slice(i * 128, (i + 1) * 128)
            onsa, onsaB = onsa2[i % 2]
            score, scoreB = score2[i % 2]
            negsel, negselB = negsel2[i % 2]
            NEGSELT, NEGSELTB = NEGSELT2[i % 2]
            attn, attnB = attn2[i % 2]
            jobs = []
            for h in range(8):
                hp, j, gq = (h % 2) * 64, h // 2, h // 4
                Ot, OB = rotO.next()
                items = [(KCMPT[:, gq, :], QNOPE[:, h, qs], id_masks(negcmp[:, qt * 128:(qt + 1) * 128]),
                          VCMP[:, gq, :], None)]

                def post(h=h, Ot=Ot, OB=OB, gq=gq):
                    sy.op("dve", lambda: DVE.tensor_scalar(out=small[:, 8:9], in0=Ot[:, 64:65], scalar1=1e-30, scalar2=None, op0=ALU.add),
                          reads=[OB], writes=[smallB])
                    sy.op("dve", lambda: DVE.reciprocal(out=small[:, 9:10], in_=small[:, 8:9]), reads=[smallB], writes=[smallB])
                    sy.op("dve", lambda: DVE.tensor_tensor(out=small[:, 10:11], in0=small[:, 9:10], in1=GATE[:, i, h * 3:h * 3 + 1], op=ALU.mult),
                          reads=[smallB, GATEB], writes=[smallB])
                    sy.op("dve", lambda: DVE.tensor_scalar(out=onsa[:, h * 64:(h + 1) * 64], in0=Ot[:, 0:64], scalar1=small[:, 10:11],
                                                           scalar2=None, op0=ALU.mult), reads=[OB, smallB], writes=[onsaB])
                    if h % 4 == 0:
                        sy.op("dve", lambda: DVE.tensor_scalar(out=score[:, gq, :], in0=Ot[:, 65:97], scalar1=small[:, 9:10],
                                                               scalar2=None, op0=ALU.mult), reads=[OB, smallB], writes=[scoreB])
                    else:
                        sy.op("dve", lambda: DVE.scalar_tensor_tensor(out=score[:, gq, :], in0=Ot[:, 65:97], scalar=small[:, 9:10],
                                                                      in1=score[:, gq, :], op0=ALU.mult, op1=ALU.add),
                              reads=[OB, smallB, scoreB], writes=[scoreB])
                jobs += make_jobs(items, Ot[:, 0:97], OB, SC_N, [VCMPB], [KCMPTB, QNOPEB, CB("negcmp"), identB], post=post)
            run_jobs(jobs)
            stop_if("acmp")
            for gq in range(2):
                sy.op("dve", lambda: DVE.scalar_tensor_tensor(out=score[:, gq, :], in0=C("forced")[:, qt * 32:(qt + 1) * 32], scalar=1e4,
                                                              in1=score[:, gq, :], op0=ALU.mult, op1=ALU.add),
                      reads=[scoreB, CB("forced")], writes=[scoreB])
                sy.op("dve", lambda: DVE.scalar_tensor_tensor(out=score[:, gq, :], in0=C("invalid")[:, qt * 32:(qt + 1) * 32], scalar=-1e30,
                                                              in1=score[:, gq, :], op0=ALU.mult, op1=ALU.add),
                      reads=[scoreB, CB("invalid")], writes=[scoreB])
                sy.op("dve", lambda: DVE.max(out=m8[:, gq * 8:(gq + 1) * 8], in_=score[:, gq, :]), reads=[scoreB], writes=[m8B])
                sy.op("dve", lambda: DVE.tensor_scalar(out=negsel[:, gq, :], in0=score[:, gq, :], scalar1=m8[:, gq * 8 + 5:gq * 8 + 6],
                                                       scalar2=NEGM, op0=ALU.is_lt, op1=ALU.mult), reads=[scoreB, m8B], writes=[negselB])

        def diff_part(i):
            qt = c * 4 + i
            qs = slice(i * 128, (i + 1) * 128)
            onsa, onsaB = onsa2[i % 2]
            score, scoreB = score2[i % 2]
            negsel, negselB = negsel2[i % 2]
            NEGSELT, NEGSELTB = NEGSELT2[i % 2]
            attn, attnB = attn2[i % 2]
            stop_if("aslc")
            jobs = []
            for h in range(8):
                j = h // 2
                Ot, OB = rotO.next()
                for m in range(2):
                    base = (h % 2) * 64 + m * 32
                    tp = (96, 0) if base == 96 else None
                    items = []
                    for kt in range(qt + 1):
                        masks = [1] if kt == qt else []
                        items.append((KD[base:base + 32, j, kt * 128:(kt + 1) * 128], QD[base:base + 32, j, qs], masks, VD[:, kt, h, :], tp))

                    def post(h=h, Ot=Ot, OB=OB):
                        sy.op("dve", lambda: DVE.reciprocal(out=small[:, 13:14], in_=Ot[:, 64:65]), reads=[OB], writes=[smallB])
                        sy.op("dve", lambda: DVE.reciprocal(out=small[:, 14:15], in_=Ot[:, 129:130]), reads=[OB], writes=[smallB])
                        sy.op("dve", lambda: DVE.tensor_tensor(out=small[:, 15:16], in0=small[:, 14:15], in1=neglam[:, 0:1], op=ALU.mult),
                              reads=[smallB, neglamB], writes=[smallB])
                        sy.op("dve", lambda: DVE.tensor_scalar(out=tmpo[:, 0:64], in0=Ot[:, 0:64], scalar1=small[:, 13:14], scalar2=None,
                                                               op0=ALU.mult), reads=[OB, smallB], writes=[tmpoB])
                        sy.op("dve", lambda: DVE.scalar_tensor_tensor(out=odif[:, h * 64:(h + 1) * 64], in0=Ot[:, 65:129], scalar=small[:, 15:16],
                                                                      in1=tmpo[:, 0:64], op0=ALU.mult, op1=ALU.add),
                              reads=[OB, smallB, tmpoB], writes=[odifB])
                    jobs += make_jobs(items, Ot[:, m * 65:(m + 1) * 65], OB, SC_D, [VDB], [KDB, QDB],
                                      post=(post if m == 1 else None), postmask=True)
            run_jobs(jobs)
            stop_if("adif")
            sy.op("act", lambda: ACT.activation(out=tmpo[:, :], in_=odif[:, :], func=AF.Square), reads=[odifB], writes=[tmpoB])
            sy.op("dve", lambda: DVE.tensor_reduce(out=small[:, 16:24], in_=tmpo[:, :].rearrange("p (h d) -> p h d", h=8), axis=AX.X, op=ALU.add),
                  reads=[tmpoB], writes=[smallB])
            sy.op("act", lambda: ACT.activation(out=small[:, 24:32], in_=small[:, 16:24], func=AF.Ln, bias=epsT[:, 0:1], scale=1.0 / 64),
                  reads=[smallB, epsB], writes=[smallB])
            sy.op("act", lambda: ACT.activation(out=small[:, 32:40], in_=small[:, 24:32], func=AF.Exp, scale=-0.5), reads=[smallB], writes=[smallB])
            for h in range(8):
                sy.op("dve", lambda h=h: DVE.scalar_tensor_tensor(out=attn[:, 512 + h * 64:512 + (h + 1) * 64], in0=odif[:, h * 64:(h + 1) * 64],
                                                                  scalar=small[:, 32 + h:33 + h], in1=goS[:], op0=ALU.mult, op1=ALU.mult),
                      reads=[odifB, smallB, goSB], writes=[attnB])

        def tr_part(i):
            qt = c * 4 + i
            qs = slice(i * 128, (i + 1) * 128)
            onsa, onsaB = onsa2[i % 2]
            score, scoreB = score2[i % 2]
            negsel, negselB = negsel2[i % 2]
            NEGSELT, NEGSELTB = NEGSELT2[i % 2]
            attn, attnB = attn2[i % 2]
            for gq in range(2):
                p2, p2B = rotP.next()
                p2b = p2[:].bitcast(BF16)
                sy.op("pe", lambda: PE.transpose(p2b[0:32, 0:128], negsel[:, gq, :], ident[:]), reads=[negselB, identB], writes=[p2B])
                sy.op("dve", lambda: DVE.tensor_copy(out=NEGSELT[0:32, gq, :], in_=p2b[0:32, 0:128]), reads=[p2B], writes=[NEGSELTB])

        def slcwin_part(i):
            qt = c * 4 + i
            qs = slice(i * 128, (i + 1) * 128)
            onsa, onsaB = onsa2[i % 2]
            score, scoreB = score2[i % 2]
            negsel, negselB = negsel2[i % 2]
            NEGSELT, NEGSELTB = NEGSELT2[i % 2]
            attn, attnB = attn2[i % 2]
            stop_if("asel")
            jobs = []
            for h in range(8):
                hp, j, gq = (h % 2) * 64, h // 2, h // 4
                Ot, OB = rotO.next()
                items = []
                for kt in range(qt + 1):
                    masks = [(esel[:, kt * 128:(kt + 1) * 128], NEGSELT[:, gq, :])]
                    if kt == qt:
                        masks += id_masks(negc[:, :])
                    items.append((KS[:, gq, kt * 128:(kt + 1) * 128], QROPE[:, h, qs], masks, VS[:, kt, gq, :], None))
                jobs += make_jobs(items, Ot[:, 0:65], OB, SC_N, [VSB], [KSB, QROPEB, NEGSELTB, CB("esel"), CB("negc"), identB])
                items = []
                for kt in range(max(0, qt - 2), qt + 1):
                    masks = []
                    if kt == qt:
                        masks = id_masks(negc[:, :])
                    elif kt == qt - 2:
                        masks = id_masks(negw[:, :])
                    items.append((KW[:, gq, kt * 128:(kt + 1) * 128], QROPE[:, h, qs], masks, VW[:, kt, gq, :], None))

                def post(h=h, Ot=Ot, OB=OB):
                    for br, c0 in ((1, 0), (2, 65)):
                        sy.op("dve", lambda: DVE.reciprocal(out=small[:, 11:12], in_=Ot[:, c0 + 64:c0 + 65]), reads=[OB], writes=[smallB])
                        sy.op("dve", lambda: DVE.tensor_tensor(out=small[:, 12:13], in0=small[:, 11:12], in1=GATE[:, i, h * 3 + br:h * 3 + br + 1],
                                                               op=ALU.mult), reads=[smallB, GATEB], writes=[smallB])
                        sy.op("dve", lambda: DVE.scalar_tensor_tensor(out=onsa[:, h * 64:(h + 1) * 64], in0=Ot[:, c0:c0 + 64], scalar=small[:, 12:13],
                                                                      in1=onsa[:, h * 64:(h + 1) * 64], op0=ALU.mult, op1=ALU.add),
                              reads=[OB, smallB, onsaB], writes=[onsaB])
                jobs += make_jobs(items, Ot[:, 65:130], OB, SC_N, [VWB], [KWB, QROPEB, CB("negc"), CB("negw"), identB], post=post)
            run_jobs(jobs)
            sy.op("act", lambda: ACT.activation(out=attn[:, 0:512], in_=onsa[:, :], func=AF.Copy), reads=[onsaB], writes=[attnB])

        def final_part(i):
            qt = c * 4 + i
            qs = slice(i * 128, (i + 1) * 128)
            onsa, onsaB = onsa2[i % 2]
            score, scoreB = score2[i % 2]
            negsel, negselB = negsel2[i % 2]
            NEGSELT, NEGSELTB = NEGSELT2[i % 2]
            attn, attnB = attn2[i % 2]
            p2, p2B = rotP.next()
            p2b = p2[:].bitcast(BF16)
            sy.group("pe", [(lambda k=k: PE.transpose(p2b[:, k * 128:(k + 1) * 128], attn[:, k * 128:(k + 1) * 128], ident[:])) for k in range(8)],
                     reads=[attnB, identB], writes=[p2B])
            sy.op("act", lambda: ACT.activation(out=attnT[:, :, :], in_=p2b[:, :].rearrange("p (k t) -> p k t", k=8), func=AF.Copy),
                  reads=[p2B], writes=[attnTB])
            xt, xB = stg[xi[0] % 2], stgB[xi[0] % 2]; xi[0] += 1
            sy.dma("sp", xt[:], x_d[b, qt * 128:(qt + 1) * 128, :], xB, writes=[xB])
            for half in range(2):
                p3, p3B = rotP.next()
                sy.group("pe", [(lambda k=k: PE.matmul(p3[:, :], lhsT=attnT[:, k, :], rhs=C("wout")[:, k, half * 512:(half + 1) * 512],
                                                       start=(k == 0), stop=(k == 7))) for k in range(8)],
                         reads=[attnTB, CB("wout")], writes=[p3B])
                sy.op("dve", lambda: DVE.tensor_tensor(out=tmpo[:, :], in0=p3[:, :], in1=G1[:, half * 512:(half + 1) * 512], op=ALU.mult),
                      reads=[p3B, G1B], writes=[tmpoB])
                sy.op("dve", lambda: DVE.tensor_tensor(out=xt[:, half * 512:(half + 1) * 512], in0=xt[:, half * 512:(half + 1) * 512],
                                                       in1=tmpo[:, :], op=ALU.add), reads=[tmpoB, xB], writes=[xB])
            sy.dma("sp", x1_dst(b, qt), xt[:], xB, reads=[xB])
            stop_if("aout")
            nb_, nc_ = (b, c + 1) if c + 1 < NCH else (b + 1, 0)
            emit_norm_tile(nb_, nc_, i)

        cmp_part(0)
        tr_part(0)
        for i in range(4):
            diff_part(i)
            if i > 0:
                final_part(i - 1)
            if i + 1 < 4:
                cmp_part(i + 1)
            slcwin_part(i)
            if i + 1 < 4:
                tr_part(i + 1)
        final_part(3)

    for b in range(nseq):
        bcast_row(G1, G1B, 16, b)
        for c in range(NCH):
            t0 = c * TCH
            for i_, tab in enumerate([cn_d, sn_d, cd_d, sd_d]):
                sy.dma("sp", ropeT[:, i_, :], tab[:, t0:t0 + TCH], ropeTB, writes=[ropeTB], add=(i_ > 0))
            for i in range(4):
                emit_norm_tile(b, c, i)
            stop_if("norm")
            pend = []
            for grp in range(5):
                if grp == 3:
                    compress_A(c)
                wt, wB = wload(win_v[:, :, grp * 512:(grp + 1) * 512], 512)
                for ci in range(4):
                    name = FM_CHUNKS[grp * 4 + ci]
                    pt, pB = rotPP.next()
                    sy.group("pe", [(lambda k=k: PE.matmul(pt[:, :], lhsT=wt[:, k, ci * 128:(ci + 1) * 128], rhs=hT[:, k, :],
                                                           start=(k == 0), stop=(k == 7))) for k in range(8)],
                             reads=[wB, hTB], writes=[pB])
                    kind, j = name[:2], int(name[2])
                    gen_ = None
                    if kind == "qn":
                        gen_ = normrope(pt, pB, "blk64", "perm64", 0, ropeT[:, 0, :], ropeT[:, 1, :],
                                 [(QNOPE[0:64, 2 * j, :], 0, 64), (QNOPE[64:128, 2 * j + 1, :], 64, 128)], [QNOPEB],
                                 [(QROPE[0:64, 2 * j, :], 0, 64), (QROPE[64:128, 2 * j + 1, :], 64, 128)], [QROPEB])
                    elif kind == "ks":
                        gen_ = normrope(pt, pB, "blk64", "perm64", 1, ropeT[:, 0, :], ropeT[:, 1, :],
                                 None, None, [(KS[:, j, t0:t0 + TCH], 0, 128)], [KSB])
                    elif kind == "kw":
                        gen_ = normrope(pt, pB, "blk64", "perm64", 2, ropeT[:, 0, :], ropeT[:, 1, :],
                                 None, None, [(KW[:, j, t0:t0 + TCH], 0, 128)], [KWB])
                    elif kind == "qd":
                        gen_ = normrope(pt, pB, "blk32", "perm32", 3, ropeT[:, 2, :], ropeT[:, 3, :],
                                 None, None, [(QD[:, j, :], 0, 128)], [QDB])
                    elif kind == "kd":
                        gen_ = normrope(pt, pB, "blk32", "perm32", 4, ropeT[:, 2, :], ropeT[:, 3, :],
                                 None, None, [(KD[:, j, t0:t0 + TCH], 0, 128)], [KDB])
                    else:
                        dst, dB = (KC2, KC2B) if kind == "kc" else (VC2, VC2B)
                        sy.op("act", lambda: ACT.activation(out=dst[0:64, j, 32:32 + TCH], in_=pt[0:64, :], func=AF.Copy),
                              reads=[pB], writes=[dB])
                        sy.op("dve", lambda: DVE.tensor_copy(out=dst[64:128, j, 31:31 + TCH], in_=pt[64:128, :]),
                              reads=[pB], writes=[dB])
                    for g_ in list(pend):
                        try:
                            next(g_)
                        except StopIteration:
                            pend.remove(g_)
                    if gen_ is not None:
                        pend.append(gen_)
            while pend:
                for g_ in list(pend):
                    try:
                        next(g_)
                    except StopIteration:
                        pend.remove(g_)
            stop_if("projfm")
            wtA, wBA = wload(win_v[:, :, NFM * 128:NFM * 128 + TM_A], TM_A)
            for i in range(4):
                kt = c * 4 + i
                pt, pB = rotPP.next()
                sy.group("pe", [(lambda k=k: PE.matmul(pt[:, 0:TM_A], lhsT=hT[:, k, i * 128:(i + 1) * 128], rhs=wtA[:, k, 0:TM_A],
                                                       start=(k == 0), stop=(k == 7))) for k in range(8)],
                         reads=[wBA, hTB], writes=[pB])
                stop_if("tmb%d_0" % i)
                sy.op("dve", lambda: DVE.tensor_copy(out=VS[:, kt, :, 0:64], in_=pt[:, 0:128].rearrange("p (g d) -> p g d", g=2)),
                      reads=[pB], writes=[VSB])
                stop_if("tmb%d_1" % i)
                sy.op("dve", lambda: DVE.tensor_copy(out=VW[:, kt, :, 0:64], in_=pt[:, 128:256].rearrange("p (g d) -> p g d", g=2)),
                      reads=[pB], writes=[VWB])
                if stage == "dbgA" and i == 1:
                    sy.op("dve", lambda: DVE.tensor_copy(out=stg[0][:, 0:280], in_=pt[:, 0:280]), reads=[pB], writes=[stgB[0]])
                    sy.dma("sp", out_d[0, 0:128, 0:280], stg[0][:, 0:280], stgB[0], reads=[stgB[0]])
                    sy.op("dve", lambda: DVE.tensor_copy(out=stg[1][:, 0:512], in_=hT[:, 0, :]), reads=[hTB], writes=[stgB[1]])
                    sy.dma("sp", out_d[0, 128:256, 0:512], stg[1][:, 0:512], stgB[1], reads=[stgB[1]])
                    raise _Stop()
                stop_if("tmb%d_2" % i)
                sy.op("act", lambda: ACT.activation(out=small[:, 8:32], in_=pt[:, 256:280], func=AF.Exp, scale=-1.0),
                      reads=[pB], writes=[smallB])
                stop_if("tmb%d_3" % i)
                sy.op("dve", lambda: DVE.tensor_scalar(out=small[:, 32:56], in0=small[:, 8:32], scalar1=1.0, scalar2=None, op0=ALU.add),
                      reads=[smallB], writes=[smallB])
                sy.op("dve", lambda: DVE.reciprocal(out=GATE[:, i, :], in_=small[:, 32:56]), reads=[smallB], writes=[GATEB])
                stop_if("tmb%d_4" % i)
            stop_if("tma")
            wtB, wBB = wload(win_v[:, :, NFM * 128 + TM_A:NCOLS], TM_B)
            for i in range(4):
                kt = c * 4 + i
                pt, pB = rotPP.next()
                sy.group("pe", [(lambda k=k: PE.matmul(pt[:, :], lhsT=hT[:, k, i * 128:(i + 1) * 128], rhs=wtB[:, k, :],
                                                       start=(k == 0), stop=(k == 7))) for k in range(8)],
                         reads=[wBB, hTB], writes=[pB])
                sy.op("act", lambda: ACT.activation(out=VD[:, kt, :, 0:64], in_=pt[:, :].rearrange("p (h d) -> p h d", h=8),
                                                    func=AF.Copy), reads=[pB], writes=[VDB])
            stop_if("proj")
            attention_chunk(b, c)
    if stage == "attn":
        sy.finish()
        return nc, sy, locals()

    sy.barrier()
    arena.reset()
    NTT = nseq * NT
    NROWS = NEXP * CAP + 128
    TRASH = NEXP * CAP
    xbuf_d, ybuf_d, xbufB = g["xbuf_d"], g["ybuf_d"], g["xbufB"]
    wr_d, br_d, wg_d, wu_d, wd_d = g["wr_d"], g["br_d"], g["wg_d"], g["wu_d"], g["wd_d"]
    mats_d = g["mats_d"]
    ecap_d = g["ecap_d"]

    GS2b = A([128, D], F32); GS2bB = Buf("GS2b")
    SH2b = A([128, D], F32); SH2bB = Buf("SH2b")
    G2b = A([128, D], F32); G2bB = Buf("G2b")
    wr = A([128, 8, 36], F32); wrB = Buf("wr")
    brb = A([128, 36], F32); brbB = Buf("brb")
    ecap = A([128, 32], F32); ecapB = Buf("ecap")
    ltri = A([128, 128], BF16); ltriB = Buf("ltri")
    ones_bf = A([128, 128], BF16); onesB = Buf("ones")
    base_bc = A([128, 32], F32); baseB = Buf("base")
    SLOT = A([128, NTT, 2], I32)
    WGT = A([128, NTT, 2], F32)
    arena_mark = arena.off
    m1set = []
    m1x = [(stg[0], stgB[0]), (stg[1], stgB[1])] + [(A([128, D], F32), Buf("m1x%d" % i_)) for i_ in range(2)]
    for si_ in range(4):
        m1set.append((A([128, 512], F32), Buf("rs%d" % si_), A([128, D], F32), Buf("xn2_%d" % si_), A([128, D], F32), Buf("tmpf%d" % si_),
                      A([128, D], BF16), Buf("h2tm%d" % si_), A([128, 8, 128], F32), Buf("h2T%d" % si_), A([128, 32], BF16), Buf("Abf%d" % si_)))
    sy.dma("sp", wr, wr_d.rearrange("(k p) n -> p k n", p=128), wrB, writes=[wrB])
    sy.dma("pool", brb, br_d[0:1, :].partition_broadcast(128), brbB, writes=[brbB])
    sy.dma("sp", ecap, ecap_d[:, :], ecapB, writes=[ecapB])
    sy.dma("pool", ltri, mats_d["ltri"][:, :], ltriB, writes=[ltriB])
    sy.op("dve", lambda: DVE.memset(ones_bf, 1.0), writes=[onesB])
    sy.op("dve", lambda: DVE.memset(base_bc, 0.0), writes=[baseB])
    SLOTBs = [Buf("slot%d" % i) for i in range(NTT)]
    WGTBs = [Buf("wgt%d" % i) for i in range(NTT)]

    def m1_tile(b, ti, si):
        tt = b * NT + ti
        Sx = m1set[si]
        rs, rsB, xn2, xn2B, tmpf, tmpfB, h2tm, h2tmB, h2T, h2TB, Abf, AbfB = Sx

        def col(i_):
            return rs[:, i_:i_ + 1]
        xt, xB = m1x[si]
        sy.dma("sp", xt[:], x1_d[b, ti * 128:(ti + 1) * 128, :], xB, writes=[xB])
        yield
        sy.op("act", lambda: ACT.activation(out=tmpf[:, 0:512], in_=xt[:, 0:512], func=AF.Square, accum_out=col(0)),
              reads=[xB], writes=[tmpfB, rsB])
        yield
        sy.op("act", lambda: ACT.activation(out=tmpf[:, 512:1024], in_=xt[:, 512:1024], func=AF.Square, accum_out=col(1)),
              reads=[xB], writes=[tmpfB, rsB])
        yield
        sy.op("dve", lambda: DVE.tensor_tensor(out=col(2), in0=col(0), in1=col(1), op=ALU.add), reads=[rsB], writes=[rsB])
        yield
        sy.op("act", lambda: ACT.activation(out=col(3), in_=col(2), func=AF.Ln, bias=epsT[:, 0:1], scale=1.0 / D),
              reads=[rsB, epsB], writes=[rsB])
        yield
        sy.op("act", lambda: ACT.activation(out=col(4), in_=col(3), func=AF.Exp, scale=-0.5), reads=[rsB], writes=[rsB])
        yield
        sy.op("act", lambda: ACT.activation(out=xn2, in_=xt[:], func=AF.Identity, scale=col(4)),
              reads=[xB, rsB], writes=[xn2B])
        yield
        sy.op("pool", lambda: POOL.tensor_tensor(out=tmpf, in0=xn2, in1=GS2b, op=ALU.mult), reads=[xn2B, GS2bB], writes=[tmpfB])
        yield
        sy.op("pool", lambda: POOL.tensor_tensor(out=h2tm, in0=tmpf, in1=SH2b, op=ALU.add), reads=[tmpfB, SH2bB], writes=[h2tmB])
        yield
        for hf in range(2):
            p2, p2B = rotPP.next()
            sy.group("pe", [(lambda k=k: PE.transpose(p2[:, (k % 4) * 128:(k % 4 + 1) * 128], xn2[:, k * 128:(k + 1) * 128],
                                                      C("identf")[:])) for k in range(hf * 4, hf * 4 + 4)],
                     reads=[xn2B, CB("identf")], writes=[p2B])
            yield
            for k in range(hf * 4, hf * 4 + 4):
                sy.op("act", lambda k=k: ACT.activation(out=h2T[:, k, :], in_=p2[:, (k % 4) * 128:(k % 4 + 1) * 128], func=AF.Identity,
                                                        scale=gs2[:, k, b:b + 1], bias=modT[:, 24 + k, b:b + 1]),
                      reads=[p2B, gs2B, modTB], writes=[h2TB])
                yield
        p3, p3B = rotPP.next()
        sy.group("pe", [(lambda k=k: PE.matmul(p3[:, 0:36], lhsT=h2T[:, k, :], rhs=wr[:, k, :], start=(k == 0), stop=(k == 7)))
                        for k in range(8)], reads=[h2TB, wrB], writes=[p3B])
        yield
        LG = rs[:, 8:44]
        sy.op("dve", lambda: DVE.tensor_tensor(out=LG, in0=p3[:, 0:36], in1=brb, op=ALU.add), reads=[p3B, brbB], writes=[rsB])
        yield
        R = lambda *a, **k_: sy.op("dve", *a, reads=[rsB], writes=[rsB], **k_)
        R(lambda: DVE.tensor_reduce(out=col(5), in_=rs[:, 8:12], axis=AX.X, op=ALU.max))
        yield
        R(lambda: DVE.tensor_scalar(out=col(6), in0=col(5), scalar1=-1.0, scalar2=None, op0=ALU.mult))
        yield
        sy.op("act", lambda: ACT.activation(out=rs[:, 48:52], in_=rs[:, 8:12], func=AF.Exp, bias=col(6), accum_out=col(7)),
              reads=[rsB], writes=[rsB])
        yield
        R(lambda: DVE.reciprocal(out=col(52), in_=col(7)))
        yield
        R(lambda: DVE.tensor_scalar(out=rs[:, 56:60], in0=rs[:, 8:12], scalar1=col(5), scalar2=None, op0=ALU.is_equal))
        yield
        R(lambda: DVE.tensor_scalar(out=rs[:, 64:72], in0=rs[:, 12:20], scalar1=col(56), scalar2=None, op0=ALU.mult))
        yield
        for gi in range(1, 4):
            R(lambda gi=gi: DVE.scalar_tensor_tensor(out=rs[:, 64:72], in0=rs[:, 12 + 8 * gi:20 + 8 * gi], scalar=col(56 + gi),
                                                     in1=rs[:, 64:72], op0=ALU.mult, op1=ALU.add))
            yield
        R(lambda: DVE.max(out=rs[:, 72:80], in_=rs[:, 64:72]))
        yield
        R(lambda: DVE.tensor_tensor(out=col(80), in0=col(73), in1=col(72), op=ALU.subtract))
        yield
        sy.op("act", lambda: ACT.activation(out=col(81), in_=col(80), func=AF.Exp), reads=[rsB], writes=[rsB])
        yield
        R(lambda: DVE.tensor_scalar(out=col(82), in0=col(81), scalar1=1.0, scalar2=None, op0=ALU.add))
        yield
        R(lambda: DVE.reciprocal(out=col(83), in_=col(82)))
        yield
        R(lambda: DVE.tensor_tensor(out=col(84), in0=col(83), in1=col(52), op=ALU.mult))
        yield
        R(lambda: DVE.tensor_tensor(out=col(85), in0=col(52), in1=col(84), op=ALU.subtract))
        yield
        R(lambda: DVE.tensor_scalar(out=rs[:, 88:96], in0=rs[:, 64:72], scalar1=col(72), scalar2=None, op0=ALU.is_equal))
        yield
        R(lambda: DVE.tensor_scalar(out=rs[:, 96:104], in0=rs[:, 64:72], scalar1=col(73), scalar2=None, op0=ALU.is_equal))
        yield
        for gi in range(4):
            R(lambda gi=gi: DVE.tensor_scalar(out=rs[:, 128 + 8 * gi:136 + 8 * gi], in0=rs[:, 88:96], scalar1=col(56 + gi),
                                              scalar2=None, op0=ALU.mult))
            yield
            R(lambda gi=gi: DVE.tensor_scalar(out=rs[:, 160 + 8 * gi:168 + 8 * gi], in0=rs[:, 96:104], scalar1=col(56 + gi),
                                              scalar2=None, op0=ALU.mult))
            yield
        sy.op("dve", lambda: DVE.tensor_tensor(out=Abf, in0=rs[:, 128:160], in1=rs[:, 160:192], op=ALU.add), reads=[rsB], writes=[AbfB])
        yield
        p4, p4B = rotPP.next()
        sy.group("pe", [lambda: PE.matmul(p4[:, 0:32], lhsT=ltri, rhs=Abf, start=True, stop=True),
                        lambda: PE.matmul(p4[:, 32:64], lhsT=ones_bf, rhs=Abf, start=True, stop=True)],
                 reads=[AbfB, ltriB, onesB], writes=[p4B])
        yield
        sy.op("dve", lambda: DVE.tensor_tensor(out=rs[:, 192:224], in0=p4[:, 0:32], in1=base_bc, op=ALU.add),
              reads=[p4B, baseB, rsB], writes=[rsB])
        sy.op("dve", lambda: DVE.tensor_tensor(out=base_bc, in0=p4[:, 32:64], in1=base_bc, op=ALU.add), reads=[p4B, baseB], writes=[baseB])
        yield
        for kx, s0 in ((0, 128), (1, 160)):
            R(lambda s0=s0: DVE.tensor_tensor(out=rs[:, 224:256], in0=rs[:, s0:s0 + 32], in1=rs[:, 192:224], op=ALU.mult))
            yield
            R(lambda kx=kx: DVE.tensor_reduce(out=col(256 + kx), in_=rs[:, 224:256], axis=AX.X, op=ALU.add))
            yield
            sy.op("dve", lambda s0=s0: DVE.tensor_tensor(out=rs[:, 224:256], in0=rs[:, s0:s0 + 32], in1=ecap, op=ALU.mult),
                  reads=[rsB, ecapB], writes=[rsB])
            yield
            R(lambda kx=kx: DVE.tensor_reduce(out=col(258 + kx), in_=rs[:, 224:256], axis=AX.X, op=ALU.add))
            yield
            R(lambda kx=kx: DVE.tensor_scalar(out=col(260 + kx), in0=col(256 + kx), scalar1=float(CAP), scalar2=None, op0=ALU.is_lt))
            yield
            R(lambda kx=kx: DVE.tensor_tensor(out=col(262 + kx), in0=col(256 + kx), in1=col(258 + kx), op=ALU.add))
            yield
            R(lambda kx=kx: DVE.tensor_scalar(out=col(262 + kx), in0=col(262 + kx), scalar1=-float(TRASH), scalar2=None, op0=ALU.add))
            yield
            R(lambda kx=kx: DVE.tensor_tensor(out=col(262 + kx), in0=col(262 + kx), in1=col(260 + kx), op=ALU.mult))
            yield
            R(lambda kx=kx: DVE.tensor_scalar(out=col(262 + kx), in0=col(262 + kx), scalar1=float(TRASH), scalar2=None, op0=ALU.add))
            yield
            sy.op("dve", lambda kx=kx: DVE.tensor_copy(out=SLOT[:, tt, kx:kx + 1], in_=col(262 + kx)), reads=[rsB], writes=[SLOTBs[tt]])
            yield
            sy.op("dve", lambda kx=kx: DVE.tensor_tensor(out=WGT[:, tt, kx:kx + 1], in0=col(84 + kx), in1=col(260 + kx), op=ALU.mult),
                  reads=[rsB], writes=[WGTBs[tt]])
            yield
        for kx in range(2):
            sy.dma_fn("pool", lambda kx=kx: POOL.indirect_dma_start(
                out=xbuf_d[:, :], out_offset=bass.IndirectOffsetOnAxis(ap=SLOT[:, tt, kx:kx + 1].bitcast(U32), axis=0),
                in_=h2tm, in_offset=None), h2tmB, reads=[h2tmB, SLOTBs[tt], xbufB])
            yield

    for b in range(nseq):
        bcast_rows_from(gs2, gs2B, GS2b, GS2bB, b)
        bcast_row(SH2b, SH2bB, 24, b)
        for ti in range(0, NT, 4):
            gens = [m1_tile(b, ti + q_, q_) for q_ in range(4)]
            alive = [True] * 4
            while any(alive):
                for gi_ in range(4):
                    if alive[gi_]:
                        try:
                            next(gens[gi_])
                        except StopIteration:
                            alive[gi_] = False
    if stage == "m1":
        sy.finish()
        return nc, sy, locals()

    sy.barrier()
    arena.off = arena_mark
    WEXP = [(A([128, 8, 512], BF16), A([128, 8, 512], BF16), A([128, 4, D], BF16)) for _ in range(2)]
    WEXPB = [(Buf("wg%d" % i), Buf("wu%d" % i), Buf("wd%d" % i)) for i in range(2)]
    XT = A([128, 8, CAP], BF16); XTB = Buf("XT")
    HT = A([128, 4, CAP], BF16); HTB = Buf("HT")
    xrow = [A([128, D], BF16) for _ in range(5)]; xrowB = [Buf("xrow%d" % i) for i in range(5)]
    yst = [A([128, D], BF16) for _ in range(3)]; ystB = [Buf("yst%d" % i) for i in range(3)]
    sgb = [A([128, 384], F32) for _ in range(3)]; sgbB = [Buf("sg%d" % i) for i in range(3)]
    tmpf = A([128, D], F32); tmpfB = Buf("tmpf3")
    Yg = [A([128, D], BF16) for _ in range(4)]; YgB = [Buf("yg%d" % i) for i in range(4)]
    NST = CAP // 128
    NCC = 3
    rotGU = Rot([0, 1, 2, 3, 4, 7])
    CW = CAP // NCC
    xr = [0]

    def load_expert(e):
        i_ = e % 2
        (wg, wu, wd_), (wgB, wuB, wdB) = WEXP[i_], WEXPB[i_]
        sy.dma("pool", wg, wg_d[e].rearrange("(k p) n -> p k n", p=128), wgB, writes=[wgB])
        sy.dma("pool", wu, wu_d[e].rearrange("(k p) n -> p k n", p=128), wuB, writes=[wuB])
        sy.dma("pool", wd_, wd_d[e].rearrange("(k p) n -> p k n", p=128), wdB, writes=[wdB])

    def x_part(e):
        for s_ in range(NST):
            xw, xwB = xrow[xr[0] % 5], xrowB[xr[0] % 5]; xr[0] += 1
            r0 = e * CAP + s_ * 128
            sy.dma("sp", xw, xbuf_d[r0:r0 + 128, :], xwB, writes=[xwB])
            p2, p2B = rotP.next()
            p2b = p2[:].bitcast(BF16)
            sy.group("pe", [(lambda k=k: PE.transpose(p2b[:, k * 128:(k + 1) * 128], xw[:, k * 128:(k + 1) * 128], ident[:]))
                            for k in range(8)], reads=[xwB, identB], writes=[p2B])
            sy.op("act" if s_ % 2 == 0 else "dve",
                  (lambda: ACT.activation(out=XT[:, :, s_ * 128:(s_ + 1) * 128], in_=p2b[:, :].rearrange("p (k t) -> p k t", k=8), func=AF.Copy))
                  if s_ % 2 == 0 else
                  (lambda: DVE.tensor_copy(out=XT[:, :, s_ * 128:(s_ + 1) * 128], in_=p2b[:, :].rearrange("p (k t) -> p k t", k=8))),
                  reads=[p2B], writes=[XTB])

    def gu_part(e):
        (wg, wu, wd_), (wgB, wuB, wdB) = WEXP[e % 2], WEXPB[e % 2]
        for m in range(4):
            for cc in range(NCC):
                cs = slice(cc * CW, (cc + 1) * CW)
                pg, pgB = rotGU.next()
                sy.group("pe", [(lambda k=k: PE.matmul(pg[:, 0:CW], lhsT=wg[:, k, m * 128:(m + 1) * 128], rhs=XT[:, k, cs],
                                                       start=(k == 0), stop=(k == 7))) for k in range(8)], reads=[wgB, XTB], writes=[pgB])
                pu, puB = rotGU.next()
                sy.group("pe", [(lambda k=k: PE.matmul(pu[:, 0:CW], lhsT=wu[:, k, m * 128:(m + 1) * 128], rhs=XT[:, k, cs],
                                                       start=(k == 0), stop=(k == 7))) for k in range(8)], reads=[wuB, XTB], writes=[puB])
                sg, sgB = sgb[(m * NCC + cc) % 3], sgbB[(m * NCC + cc) % 3]
                sy.op("act", lambda: ACT.activation(out=sg[:, 0:CW], in_=pg[:, 0:CW], func=AF.Silu), reads=[pgB], writes=[sgB])
                sy.op("dve", lambda: DVE.tensor_tensor(out=HT[:, m, cs], in0=pu[:, 0:CW], in1=sg[:, 0:CW], op=ALU.mult),
                      reads=[puB, sgB], writes=[HTB])

    def dn_part(e):
        (wg, wu, wd_), (wgB, wuB, wdB) = WEXP[e % 2], WEXPB[e % 2]
        for s_ in range(NST):
            ys, ysB = yst[s_ % 3], ystB[s_ % 3]
            for half in range(2):
                po, poB = rotO.next()
                sy.group("pe", [(lambda m=m: PE.matmul(po[:, :], lhsT=HT[:, m, s_ * 128:(s_ + 1) * 128], rhs=wd_[:, m, half * 512:(half + 1) * 512],
                                                       start=(m == 0), stop=(m == 3))) for m in range(4)], reads=[HTB, wdB], writes=[poB])
                if half == 0:
                    sy.op("act", lambda: ACT.activation(out=ys[:, 0:512], in_=po[:, :], func=AF.Copy), reads=[poB], writes=[ysB])
                else:
                    sy.op("dve", lambda: DVE.tensor_copy(out=ys[:, 512:1024], in_=po[:, :]), reads=[poB], writes=[ysB])
            r0 = e * CAP + s_ * 128
            sy.dma("sp", ybuf_d[r0:r0 + 128, :], ys, ysB, reads=[ysB])

    load_expert(0)
    x_part(0)
    for e in range(NEXP):
        if e + 1 < NEXP:
            load_expert(e + 1)
        gu_part(e)
        if e + 1 < NEXP:
            x_part(e + 1)
        dn_part(e)
    if stage == "m2":
        sy.finish()
        return nc, sy, locals()

    sy.barrier()
    yi = [0]
    for b in range(nseq):
        bcast_row(G2b, G2bB, 40, b)
        for ti in range(NT):
            tt = b * NT + ti
            xt, xB = stg[xi[0] % 2], stgB[xi[0] % 2]; xi[0] += 1
            sy.dma("sp", xt[:], x1_d[b, ti * 128:(ti + 1) * 128, :], xB, writes=[xB])
            ys_ = []
            for kx in range(2):
                y_, yB_ = Yg[yi[0] % 4], YgB[yi[0] % 4]; yi[0] += 1
                sy.dma_fn("pool", lambda kx=kx, y_=y_: POOL.indirect_dma_start(
                    out=y_, out_offset=None, in_=ybuf_d[:, :],
                    in_offset=bass.IndirectOffsetOnAxis(ap=SLOT[:, tt, kx:kx + 1].bitcast(U32), axis=0)), yB_, reads=[SLOTBs[tt]], writes=[yB_])
                ys_.append((y_, yB_))
            sy.op("act", lambda: ACT.activation(out=tmpf, in_=ys_[0][0], func=AF.Identity, scale=WGT[:, tt, 0:1]),
                  reads=[ys_[0][1], WGTBs[tt]], writes=[tmpfB])
            sy.op("dve", lambda: DVE.scalar_tensor_tensor(out=tmpf, in0=ys_[1][0], scalar=WGT[:, tt, 1:2], in1=tmpf, op0=ALU.mult, op1=ALU.add),
                  reads=[ys_[1][1], WGTBs[tt], tmpfB], writes=[tmpfB])
            sy.op("dve", lambda: DVE.tensor_tensor(out=tmpf, in0=tmpf, in1=G2b, op=ALU.mult), reads=[tmpfB, G2bB], writes=[tmpfB])
            sy.op("dve", lambda: DVE.tensor_tensor(out=xt[:], in0=xt[:], in1=tmpf, op=ALU.add), reads=[tmpfB, xB], writes=[xB])
            sy.dma("sp", out_d[b, ti * 128:(ti + 1) * 128, :], xt[:], xB, reads=[xB])
    sy.finish()
    return nc, sy, locals()


def _prep_shared(inp):
    f = lambda a: np.ascontiguousarray(np.asarray(a, dtype=np.float32))
    hc = _host_consts()
    sh = {}
    sh["w_ada"] = f(inp["w_ada"][0])
    sh["b_ada_fm"] = _fm(f(inp["b_ada"][0]), 48)
    sh["b_ada_row"] = f(inp["b_ada"][0]).reshape(1, -1)
    sh["gmix_fm"] = _fm(f(inp["g_norm_mix"][0]), 8)
    sh["gffn_fm"] = _fm(f(inp["g_norm_ffn"][0]), 8)
    sh["w_inp"] = _win_cols(f(inp["w_in"][0]))
    gq = f(inp["g_nsa_q"][0]); gk = f(inp["g_nsa_k"][0])
    gdq = f(inp["g_diff_q"][0]); gdk = f(inp["g_diff_k"][0])
    sh["rowgain"] = np.ascontiguousarray(np.stack([np.tile(gq, 2), np.tile(gk[1], 2), np.tile(gk[2], 2),
                                                   np.tile(gdq, 4), np.tile(gdk, 4)], axis=1))
    sh["gk0_row"] = np.tile(gk[0], 2).reshape(1, 128)
    sh["go_row"] = f(inp["g_diff_out"][0]).reshape(1, 64)
    sh["lamv"] = np.concatenate([f(inp["lam_q1"][0]), f(inp["lam_k1"][0]), f(inp["lam_q2"][0]), f(inp["lam_k2"][0])]).reshape(1, 128)
    pe = f(inp["pe_cmp"][0])
    pefm = np.zeros((128, 32), np.float32)
    for kv in range(2):
        for j in range(16):
            pefm[0:64, kv * 16 + j] = pe[kv, 2 * j]
            pefm[64:128, kv * 16 + j] = pe[kv, 2 * j + 1]
    sh["pe_fm"] = pefm
    sh["w_cmp1"] = f(inp["w_cmp1"][0])
    w2 = f(inp["w_cmp2"][0])
    sh["w_cmp2k"] = np.ascontiguousarray(np.concatenate([w2[0], w2[0]], axis=1))
    sh["w_cmp2v"] = np.ascontiguousarray(w2[1])
    sh["w_out"] = f(inp["w_out"][0])
    for k in ("cn", "sn", "cd", "sd"):
        sh[k] = hc[k]
    for k in ("blk64", "blk32", "perm64", "perm32", "ident", "negc", "negw", "negcmp", "esel", "ov", "forced", "invalid", "ltri"):
        sh["m_" + k] = hc[k]
    sh["ecap"] = np.ascontiguousarray(np.tile((np.arange(32, dtype=np.float32) * CAP)[None, :], (128, 1)))
    sh["w_r"] = np.ascontiguousarray(np.concatenate([f(inp["w_router_group"][0]), f(inp["w_router_expert"][0])], axis=1))
    sh["b_r"] = np.concatenate([f(inp["b_router_group"][0]), f(inp["b_router_expert"][0])]).reshape(1, 36)
    sh["w_g"] = f(inp["w_exp_gate"][0])
    sh["w_u"] = f(inp["w_exp_up"][0])
    sh["w_d"] = f(inp["w_exp_down"][0])
    return sh


def _prep_core(inp, b0, nseq):
    x = np.ascontiguousarray(np.asarray(inp["x"][b0:b0 + nseq], dtype=np.float32))
    c = np.asarray(inp["c"][b0:b0 + nseq], dtype=np.float32)
    cT = np.ascontiguousarray(c.T.reshape(8, 128, nseq).transpose(1, 0, 2))
    return {"x": x, "cT": cT}


def run(inputs, nseq=4, ncores=8, stage="full", trace=False):
    nc, sy, L = build(nseq, stage=stage)
    used = set(L["used_inputs"])
    sh = {k: v for k, v in _prep_shared(inputs).items() if k in used}
    in_maps = []
    for ci in range(ncores):
        m = dict(sh)
        m.update(_prep_core(inputs, ci * nseq, nseq))
        in_maps.append(m)
    res = run_bass_kernel_spmd(nc, in_maps, core_ids=list(range(ncores)), trace=trace)
    outs = [r["out"] for r in res.results]
    return np.concatenate(outs, axis=0), res


def kernel(**inputs):
    out, _ = run(inputs, nseq=4, ncores=8, stage="full")
    return out.astype(np.float32)
```
